# Optimizing a Trainium2 kernel written in Bass

```python
import jax, jax.numpy as jnp
from jax import lax
import numpy as np

D_MODEL = 2048
BATCH = 4
SEQ = 4096
DEPTH = 2

NORM_EPS = 1e-6
MIX_WIDTH = D_MODEL
CONV_CH = MIX_WIDTH // 2
CONV_K = 3
GLA_HEADS = 4
GLA_DV = (MIX_WIDTH // 2) // GLA_HEADS
GLA_DK = GLA_DV // 2
GLA_GATE_RANK = 16
GLA_GATE_TEMP = 16.0
GLA_CHUNK = 64
ATTN_HEADS = 16
ATTN_HEAD_DIM = D_MODEL // ATTN_HEADS
DILATED_GROUPS = ((128, 1), (512, 4), (2048, 16))
ATTN_BLOCK = 128
D_FF = 4 * D_MODEL
N_EVEN = (DEPTH + 1) // 2
N_ODD = DEPTH // 2
IN_SIZES = (CONV_CH, CONV_CH, CONV_CH, GLA_HEADS * GLA_DK, GLA_HEADS * GLA_DK,
            GLA_HEADS * GLA_DV, GLA_HEADS * GLA_DV, GLA_GATE_RANK)
IN_OFFSETS = tuple(int(v) for v in np.cumsum(IN_SIZES)[:-1])
IN_COLS = sum(IN_SIZES)

kernel_name = "hybrid_conv_gla_dilated_trunk"


def rmsnorm(x, g):
    xf = x.astype(jnp.float32)
    xf = xf * lax.rsqrt(jnp.mean(xf * xf, axis=-1, keepdims=True) + NORM_EPS)
    return (xf * g.astype(jnp.float32)).astype(x.dtype)


def causal_short_conv(u, w):
    return lax.conv_general_dilated(
        u, w[:, None, :], window_strides=(1,), padding=[(CONV_K - 1, 0)],
        dimension_numbers=('NWC', 'WIO', 'NWC'), feature_group_count=u.shape[-1])


def gla_chunked(q, k, v, log_a):
    b, t, h, dk = q.shape
    dv = v.shape[-1]
    n = t // GLA_CHUNK

    def chunks(a):
        return a.astype(jnp.float32).reshape(b, n, GLA_CHUNK, h, a.shape[-1]).transpose(1, 0, 3, 2, 4)

    qc, kc, vc = chunks(q), chunks(k), chunks(v)
    gc = jnp.cumsum(chunks(log_a), axis=3)
    causal = jnp.tril(jnp.ones((GLA_CHUNK, GLA_CHUNK), dtype=bool))

    def step(state, inp):
        qi, ki, vi, gi = inp
        o_inter = jnp.einsum('bhck,bhkv->bhcv', qi * jnp.exp(gi), state)
        rel = gi[:, :, :, None, :] - gi[:, :, None, :, :]
        decay = jnp.exp(jnp.where(causal[:, :, None], rel, -jnp.inf))
        scores = jnp.einsum('bhik,bhijk,bhjk->bhij', qi, decay, ki)
        o_intra = jnp.einsum('bhij,bhjv->bhiv', scores, vi)
        g_last = gi[:, :, -1]
        k_dec = ki * jnp.exp(g_last[:, :, None] - gi)
        state = jnp.exp(g_last)[..., None] * state + jnp.einsum('bhjk,bhjv->bhkv', k_dec, vi)
        return state, o_inter + o_intra

    s0 = jnp.zeros((b, h, dk, dv), jnp.float32)
    _, o = lax.scan(step, s0, (qc, kc, vc, gc))
    return o.transpose(1, 0, 3, 2, 4).reshape(b, t, h, dv).astype(q.dtype)


def conv_gla_mixer(h, w_in, conv_w, w_gate2, b_gate, gla_norm_g, w_out):
    b, t, _ = h.shape
    a_b, a_c, a_x, q, k, v, r, g_low = jnp.split(h @ w_in, IN_OFFSETS, axis=-1)
    y_conv = a_b * causal_short_conv(a_c * a_x, conv_w)
    q = q.reshape(b, t, GLA_HEADS, GLA_DK) * (GLA_DK ** -0.5)
    k = k.reshape(b, t, GLA_HEADS, GLA_DK)
    v = v.reshape(b, t, GLA_HEADS, GLA_DV)
    log_a = jax.nn.log_sigmoid((g_low @ w_gate2 + b_gate).astype(jnp.float32)) / GLA_GATE_TEMP
    o = gla_chunked(q, k, v, log_a.reshape(b, t, GLA_HEADS, GLA_DK))
    o = rmsnorm(o, gla_norm_g) * jax.nn.silu(r.reshape(b, t, GLA_HEADS, GLA_DV))
    y = jnp.concatenate([y_conv, o.reshape(b, t, GLA_HEADS * GLA_DV)], axis=-1)
    return y @ w_out


def dilated_branch(q, k, v, slopes, window, dilation):
    b, t, h, dh = q.shape
    steps = window // dilation
    assert steps <= ATTN_BLOCK
    span = dilation * ATTN_BLOCK
    tp = -(-t // span) * span
    n_sub = tp // dilation
    nb = n_sub // ATTN_BLOCK

    def blocks(a):
        a = jnp.pad(a, ((0, 0), (0, tp - t), (0, 0), (0, 0)))
        a = a.reshape(b, n_sub, dilation, h, dh).transpose(0, 2, 3, 1, 4)
        return a.reshape(b, dilation, h, nb, ATTN_BLOCK, dh)

    def with_prev(a):
        prev = jnp.pad(a, ((0, 0), (0, 0), (0, 0), (1, 0), (0, 0), (0, 0)))[:, :, :, :-1]
        return jnp.concatenate([prev, a], axis=4)

    qb = blocks(q)
    kw, vw = with_prev(blocks(k)), with_prev(blocks(v))
    s = jnp.einsum('brhnqc,brhnkc->brhnqk', qb, kw).astype(jnp.float32) * (dh ** -0.5)
    qi = jnp.arange(ATTN_BLOCK)[:, None]
    ki = jnp.arange(2 * ATTN_BLOCK)[None, :]
    diff = ATTN_BLOCK + qi - ki
    band = (diff >= 0) & (diff <= steps)
    after_start = (jnp.arange(nb)[:, None, None] > 0) | (ki[None] >= ATTN_BLOCK)
    mask = band[None] & after_start
    alibi = -slopes[:, None, None] * (dilation * diff).astype(jnp.float32)[None]
    s = jnp.where(mask[None, None, None], s + alibi[None, None, :, None], -jnp.inf)
    m = jnp.max(s, axis=-1, keepdims=True)
    p = jnp.exp(s - m)
    z = jnp.sum(p, axis=-1, keepdims=True)
    o = jnp.einsum('brhnqk,brhnkc->brhnqc', p, vw.astype(jnp.float32)) / z
    lse = (m + jnp.log(z))[..., 0]
    o = o.reshape(b, dilation, h, n_sub, dh).transpose(0, 3, 1, 2, 4).reshape(b, tp, h, dh)[:, :t]
    lse = lse.reshape(b, dilation, h, n_sub).transpose(0, 3, 1, 2).reshape(b, tp, h)[:, :t]
    return o, lse


def dilated_attention(h, w_qkv, w_o):
    b, t, _ = h.shape
    qkv = (h @ w_qkv).reshape(b, t, 3, ATTN_HEADS, ATTN_HEAD_DIM)
    q, k, v = qkv[:, :, 0], qkv[:, :, 1], qkv[:, :, 2]
    slopes = jnp.exp2(-8.0 * jnp.arange(1, ATTN_HEADS + 1, dtype=jnp.float32) / ATTN_HEADS)
    outs, lses = zip(*[dilated_branch(q, k, v, slopes, win, dil) for win, dil in DILATED_GROUPS])
    wts = jax.nn.softmax(jnp.stack(lses), axis=0)
    o = jnp.sum(wts[..., None] * jnp.stack(outs), axis=0)
    return o.reshape(b, t, D_MODEL).astype(h.dtype) @ w_o


def squared_relu_mlp(h, w1, w2):
    return jnp.square(jax.nn.relu(h @ w1)) @ w2


def setup_inputs(seed: int = 0) -> dict:
    key = jax.random.key(seed)
    ks = jax.random.split(key, 14)
    nrm = jax.random.normal
    f32 = jnp.float32
    return {
        'x': nrm(ks[0], (BATCH, SEQ, D_MODEL), f32),
        'norm_mix_g': 1.0 + 0.01 * nrm(ks[1], (DEPTH, D_MODEL), f32),
        'norm_mlp_g': 1.0 + 0.01 * nrm(ks[2], (DEPTH, D_MODEL), f32),
        'final_norm_g': 1.0 + 0.01 * nrm(ks[3], (D_MODEL,), f32),
        'hyb_w_in': nrm(ks[4], (N_EVEN, D_MODEL, IN_COLS), f32) * D_MODEL ** -0.5,
        'conv_w': nrm(ks[5], (N_EVEN, CONV_K, CONV_CH), f32) * CONV_K ** -0.5,
        'gla_w_gate2': nrm(ks[6], (N_EVEN, GLA_GATE_RANK, GLA_HEADS * GLA_DK), f32) * GLA_GATE_RANK ** -0.5,
        'gla_b_gate': 0.01 * nrm(ks[7], (N_EVEN, GLA_HEADS * GLA_DK), f32),
        'gla_norm_g': 1.0 + 0.01 * nrm(ks[8], (N_EVEN, GLA_DV), f32),
        'hyb_w_out': nrm(ks[9], (N_EVEN, MIX_WIDTH, D_MODEL), f32) * MIX_WIDTH ** -0.5,
        'attn_w_qkv': nrm(ks[10], (N_ODD, D_MODEL, 3 * D_MODEL), f32) * D_MODEL ** -0.5,
        'attn_w_o': nrm(ks[11], (N_ODD, D_MODEL, D_MODEL), f32) * D_MODEL ** -0.5,
        'mlp_w1': nrm(ks[12], (DEPTH, D_MODEL, D_FF), f32) * D_MODEL ** -0.5,
        'mlp_w2': nrm(ks[13], (DEPTH, D_FF, D_MODEL), f32) * D_FF ** -0.5,
    }


def reference(x, norm_mix_g, norm_mlp_g, final_norm_g, hyb_w_in, conv_w, gla_w_gate2,
              gla_b_gate, gla_norm_g, hyb_w_out, attn_w_qkv, attn_w_o, mlp_w1, mlp_w2):
    for layer in range(DEPTH):
        h = rmsnorm(x, norm_mix_g[layer])
        if layer % 2 == 0:
            e = layer // 2
            mix = conv_gla_mixer(h, hyb_w_in[e], conv_w[e], gla_w_gate2[e], gla_b_gate[e],
                                 gla_norm_g[e], hyb_w_out[e])
        else:
            o = layer // 2
            mix = dilated_attention(h, attn_w_qkv[o], attn_w_o[o])
        x = x + mix
        x = x + squared_relu_mlp(rmsnorm(x, norm_mlp_g[layer]), mlp_w1[layer], mlp_w2[layer])
    return rmsnorm(x, final_norm_g)
```

```python
import numpy as np
import concourse.bass as bass
import concourse.mybir as mybir
from concourse.bass_utils import run_bass_kernel_spmd

F32 = mybir.dt.float32
BF16 = mybir.dt.bfloat16
AF = mybir.ActivationFunctionType
ALU = mybir.AluOpType
AX = mybir.AxisListType

D = 2048
NOWN = 2048
TT = 1024
NT = NOWN // TT
ST = 512
DFF = 8192
INCOLS = 6160
EPS = 1e-6
NHEAD_ATT = 16
DH = 128


class Buf:
    def __init__(self, name):
        self.name = name
        self.wt = None
        self.rts = []
        self.dsem = None
        self.dcount = 0


class Sched:
    def __init__(self, nc):
        self.nc = nc
        self.eng = {'pe': nc.tensor, 'act': nc.scalar, 'dve': nc.vector, 'pool': nc.gpsimd, 'sp': nc.sync}
        self.sem = {k: nc.alloc_semaphore(name="prog_" + k) for k in self.eng}
        self.cnt = {k: 0 for k in self.eng}
        self.waited = {k: {} for k in self.eng}
        self.nwaits = 0
        self.nops = 0
        self.allsems = {}

    def _deps(self, e, reads, writes):
        need = {}

        def add(t):
            if t is None:
                return
            s, v = t
            k = id(s)
            if k not in need or need[k][1] < v:
                need[k] = (s, v)
        for r in reads:
            add(r.wt)
        for w in writes:
            add(w.wt)
            for t in w.rts:
                add(t)
        wd = self.waited[e]
        for k, (s, v) in need.items():
            if wd.get(k, 0) >= v:
                continue
            if e == 'pe' and s is self.sem['pe']:
                continue
            self.eng[e].wait_ge(s, v)
            wd[k] = v
            self.nwaits += 1

    def _done(self, t, reads, writes):
        for r in reads:
            r.rts = [x for x in r.rts if x[0] is not t[0]] + [t]
        for w in writes:
            w.wt = t
            w.rts = []

    def op(self, e, fn, reads=(), writes=()):
        self._deps(e, reads, writes)
        ins = fn(self.eng[e])
        self.cnt[e] += 1
        ins.then_inc(self.sem[e], 1)
        t = (self.sem[e], self.cnt[e])
        self._done(t, reads, writes)
        self.nops += 1
        return t

    def mm(self, fn, reads=(), writes=(), sig=True):
        self._deps('pe', reads, writes)
        ins = fn(self.eng['pe'])
        self.nops += 1
        if sig:
            self.cnt['pe'] += 1
            ins.then_inc(self.sem['pe'], 1)
        t = (self.sem['pe'], self.cnt['pe'] + (0 if sig else 1))
        self._done(t, reads, writes)
        return t

    def dma(self, q, out_ap, in_ap, semb, reads=(), writes=(), **kw):
        self._deps(q, reads, writes)
        if semb.dsem is None:
            semb.dsem = self.nc.alloc_semaphore(name="d_" + semb.name)
            self.allsems[id(semb.dsem)] = semb
        ins = self.eng[q].dma_start(out=out_ap, in_=in_ap, **kw)
        semb.dcount += 16
        ins.then_inc(semb.dsem, 16)
        t = (semb.dsem, semb.dcount)
        self._done(t, reads, writes)
        self.nops += 1
        return t

    def wait_bufs(self, e, bufs):
        self._deps(e, bufs, bufs)

    def barrier(self, extra_bufs=()):
        for e in self.eng:
            wd = self.waited[e]
            for e2 in self.eng:
                v = self.cnt[e2]
                if v > 0 and wd.get(id(self.sem[e2]), 0) < v:
                    self.eng[e].wait_ge(self.sem[e2], v)
                    wd[id(self.sem[e2])] = v
            for k, b in self.allsems.items():
                if b.dcount > 0 and wd.get(k, 0) < b.dcount:
                    self.eng[e].wait_ge(b.dsem, b.dcount)
                    wd[k] = b.dcount


class K:
    pass


def bc_last(ap, n):
    return ap.unsqueeze(2).broadcast_to([ap.shape[0], ap.shape[1], n])


def build(mode, ncores=8):
    nc = bass.Bass("TRN2", target_bir_lowering=False)
    S = Sched(nc)
    k = K()
    k.nc, k.S, k.mode = nc, S, mode
    k.ncores = ncores
    doA = mode in ('A', 'F')
    doB = mode in ('B', 'F')

    def dram_in(name, shape, dt=F32):
        return nc.dram_tensor(name, shape, dt, kind="ExternalInput").ap()

    def dram_out(name, shape, dt=F32):
        return nc.dram_tensor(name, shape, dt, kind="ExternalOutput").ap()

    def dram_int(name, shape, dt=F32):
        return nc.dram_tensor(name, shape, dt).ap()

    if doA:
        k.x_own = dram_in("x_own", [NOWN, D])
        k.x_prev = dram_in("x_prev", [NOWN, D])
        k.norm_mix_g = dram_in("norm_mix_g", [2, D])
        k.norm_mlp_g = dram_in("norm_mlp_g", [2, D])
        k.w_in = dram_in("hyb_w_in", [D, INCOLS])
        k.conv_w = dram_in("conv_w", [3, 1024])
        k.wg2 = dram_in("gla_w_gate2", [16, 512])
        k.bg = dram_in("gla_b_gate", [1, 512])
        k.gng = dram_in("gla_norm_g", [1, 256])
        k.w_out = dram_in("hyb_w_out", [D, D])
        k.w_qkv = dram_in("attn_w_qkv", [D, 3 * D])
        k.w1_0 = dram_in("mlp_w1_0", [D, DFF])
        k.w2_0 = dram_in("mlp_w2_0", [DFF, D])
    if mode == 'A':
        k.X1 = dram_out("X1", [NOWN, D])
        k.QT = dram_out("QT", [D, NOWN], BF16)
        k.KT = dram_out("KT", [D, NOWN], BF16)
        k.V = dram_out("V", [NOWN, D], BF16)
    elif mode == 'B':
        k.X1 = dram_in("X1", [NOWN, D])
        k.QT = dram_in("QT", [D, NOWN], BF16)
        k.KT = dram_in("KT", [D, NOWN], BF16)
        k.V = dram_in("V", [NOWN, D], BF16)
        k.KTp = dram_in("KTp", [D, NOWN], BF16)
        k.Vp = dram_in("Vp", [NOWN, D], BF16)
    else:
        k.X1 = dram_int("X1", [NOWN, D])
        k.QT = dram_int("QT", [D, NOWN], BF16)
        k.KT = dram_int("KT", [D, NOWN], BF16)
        k.V = dram_int("V", [NOWN, D], BF16)
        k.KTp = dram_int("KTp", [D, NOWN], BF16)
        k.Vp = dram_int("Vp", [NOWN, D], BF16)
    if doB:
        k.flag = dram_in("flag", [128, 1])
        k.norm_mlp_g1 = k.norm_mlp_g if doA else dram_in("norm_mlp_g", [2, D])
        k.final_g = dram_in("final_norm_g", [1, D])
        k.w_o = dram_in("attn_w_o", [D, D])
        k.w1_1 = dram_in("mlp_w1_1", [D, DFF])
        k.w2_1 = dram_in("mlp_w2_1", [DFF, D])
        k.OT = dram_int("OT", [D, NOWN], BF16)
        k.out = dram_out("out", [NOWN, D])

    k.banks = [(nc.alloc_psum_tensor("bank%d" % i, [128, 512], F32), Buf("bank%d" % i)) for i in range(8)]
    k.bank_i = 0

    def bank():
        b = k.banks[k.bank_i % 8]
        k.bank_i += 1
        return b
    k.bank = bank

    k.ident = nc.alloc_sbuf_tensor("ident", [128, 128], BF16)
    k.b_const = Buf("consts")
    cb = k.b_const
    S.op('pool', lambda e: e.memset(k.ident[:, :], 1.0), writes=[cb])
    S.op('pool', lambda e: e.affine_select(k.ident[:, :], k.ident[:, :], [[-1, 128]], ALU.is_equal, 0.0,
                                           base=0, channel_multiplier=1), reads=[cb], writes=[cb])

    k.ident32 = nc.alloc_sbuf_tensor("ident32", [16, 16], F32)
    S.op('pool', lambda e: e.memset(k.ident32[:, :], 1.0), writes=[cb])
    S.op('pool', lambda e: e.affine_select(k.ident32[:, :], k.ident32[:, :], [[-1, 16]], ALU.is_equal, 0.0,
                                           base=0, channel_multiplier=1), reads=[cb], writes=[cb])
    k.gstage = (nc.alloc_sbuf_tensor("gstage", [16, 128], F32), Buf("gstage"))
    k.epsc = nc.alloc_sbuf_tensor("epsc", [128, 2], F32)
    S.op('pool', lambda e: e.memset(k.epsc[:, 0:1], EPS), writes=[cb])
    S.op('pool', lambda e: e.memset(k.epsc[:, 1:2], 1.0), writes=[cb])
    if doA:
        phaseA(k)
    if doB:
        S.barrier()
        phaseB(k)
    S.barrier()
    print("built mode", mode, "ops", S.nops, "waits", S.nwaits)
    return nc


def load_gT(k, name, src_row_ap, nchunk=16, dst=None):
    nc, S = k.nc, k.S
    b = k.b_const
    if dst is None:
        dst = nc.alloc_sbuf_tensor(name, [128, nchunk], F32)[:, :]
    st, bst = k.gstage
    S.dma('sp', st[0:nchunk, :], src_row_ap.rearrange("o (c p) -> (o c) p", p=128), bst, writes=[bst])
    pt, bpt = k.bank()
    S.mm(lambda e: e.matmul(pt[:, 0:nchunk], st[0:nchunk, :], k.ident32[0:nchunk, 0:nchunk], start=True, stop=True),
         reads=[bst, b], writes=[bpt])
    S.op('dve', lambda e: e.tensor_copy(dst, pt[:, 0:nchunk]), reads=[bpt], writes=[b])
    return dst


def norm_transpose(k, xres, bx, gT, hT, bhT, nsub):
    nc, S = k.nc, k.S
    for s in range(nsub):
        hb, bhb = k.hb[s % 2]
        S.op('act', lambda e: e.activation(hb[:, :], xres[:, s, :], AF.Square, accum_out=k.stat[:, s:s + 1]),
             reads=[bx], writes=[bhb, k.bstat])
    S.op('act', lambda e: e.activation(k.stat[:, 8:8 + nsub], k.stat[:, 0:nsub], AF.Ln, scale=1.0 / D, bias=k.epsc[:, 0:1]),
         reads=[k.bstat, k.b_const], writes=[k.bstat])
    S.op('act', lambda e: e.activation(k.stat[:, 16:16 + nsub], k.stat[:, 8:8 + nsub], AF.Exp, scale=-0.5),
         reads=[k.bstat], writes=[k.bstat])
    for s in range(nsub):
        hb, bhb = k.hb[s % 2]
        rstd = k.stat[:, 16 + s:17 + s]
        if s % 2 == 0:
            S.op('act', lambda e: e.activation(hb[:, :], xres[:, s, :], AF.Copy, scale=rstd),
                 reads=[bx, k.bstat], writes=[bhb])
        else:
            S.op('dve', lambda e: e.tensor_scalar(hb[:, :], xres[:, s, :], rstd, None, ALU.mult),
                 reads=[bx, k.bstat], writes=[bhb])
        for cg in range(2):
            pt, bpt = k.bank()
            pv = pt[:, :].bitcast(BF16)
            for c in range(8):
                cc = cg * 8 + c
                S.mm(lambda e: e.transpose(pv[:, c * 128:(c + 1) * 128], hb[:, cc * 128:(cc + 1) * 128], k.ident[:, :]),
                     reads=[bhb, k.b_const], writes=[bpt], sig=(c == 7))
            pv3 = pv.rearrange("p (c t) -> p c t", c=8)
            S.op('dve', lambda e: e.tensor_tensor(hT[:, cg * 8:(cg + 1) * 8, s * 128:(s + 1) * 128], pv3,
                                                  bc_last(gT[:, cg * 8:(cg + 1) * 8], 128), ALU.mult),
                 reads=[bpt, k.b_const], writes=[bhT])


def wblock_loader(k, W, r0, segs, nk=16):
    S = k.S
    ncols = sum(n for _, n in segs)

    def load(slot):
        wb, bwb = slot
        v = wb[:, 0:nk * ncols].rearrange("p (k c) -> p k c", k=nk)
        off = 0
        for (c0, n) in segs:
            src = W[r0:r0 + nk * 128, c0:c0 + n].rearrange("(kc p) c -> p kc c", p=128)
            S.dma('pool', v[:, :, off:off + n], src, bwb, writes=[bwb])
            off += n
        return v
    return load


def gemm_f(k, wv, bwb, chunks, rhsT, brhs, halves, evac):
    S = k.S
    nk = wv.shape[1]
    for ci, (off, width) in enumerate(chunks):
        for hi, (t0, n) in enumerate(halves):
            pt, bpt = k.bank()
            for kk in range(nk):
                S.mm(lambda e: e.matmul(pt[0:width, 0:n], wv[:, kk, off:off + width], rhsT[:, kk, t0:t0 + n],
                                        start=(kk == 0), stop=(kk == nk - 1)),
                     reads=[bwb, brhs], writes=[bpt], sig=(kk == nk - 1))
            evac(ci, hi, pt[0:width, 0:n], bpt)
            bg_tick(k)


def gemm_t(k, wv, bwb, ncols, lhsT, blhs, subtiles, evac, t_base=0):
    S = k.S
    nk = wv.shape[1]
    for m in subtiles:
        pt, bpt = k.bank()
        t0 = t_base + m * 128
        for kk in range(nk):
            S.mm(lambda e: e.matmul(pt[:, 0:ncols], lhsT[:, kk, t0:t0 + 128], wv[:, kk, 0:ncols],
                                    start=(kk == 0), stop=(kk == nk - 1)),
                 reads=[bwb, blhs[kk] if isinstance(blhs, list) else blhs], writes=[bpt], sig=(kk == nk - 1))
        evac(m, pt[:, 0:ncols], bpt)


def bg_tick(k):
    g = getattr(k, 'bgen', None)
    if g is not None:
        try:
            next(g)
        except StopIteration:
            k.bgen = None


def bg_drain(k):
    while getattr(k, 'bgen', None) is not None:
        bg_tick(k)


class Pipe:
    def __init__(self, k):
        self.k = k
        self.steps = []

    def add(self, loader, compute):
        self.steps.append((loader, compute))

    def run(self):
        k = self.k
        slots = k.wslots
        ns = len(slots)
        views = {}
        li = 0
        wi = 0
        import os
        sk = os.environ.get("KSKIP")
        if sk:
            rngs = [[int(v) for v in r.split(":")] for r in sk.split(",")]
            self.steps = [st for i, st in enumerate(self.steps) if not any(a <= i < b for a, b in rngs)]
        order = [i for i, (l, c) in enumerate(self.steps) if l is not None]
        slot_of = {i: slots[j % ns] for j, i in enumerate(order)}
        pos = 0

        def issue_next():
            nonlocal pos
            if pos < len(order):
                i = order[pos]
                views[i] = self.steps[i][0](slot_of[i])
                pos += 1
        for _ in range(ns - 1):
            issue_next()
        import os
        lim = int(os.environ.get("KSTEPS", "1000000"))
        for i, (l, c) in enumerate(self.steps):
            if i >= lim:
                break
            if l is not None:
                while i not in views:
                    issue_next()
                c(views[i], slot_of[i][1])
                issue_next()
                del views[i]
            else:
                c(None, None)
        self.steps = []


def phaseA(k):
    from contextlib import ExitStack
    with ExitStack() as es:
        _phaseA(k, es)
        k.S.barrier()


def _phaseA(k, es):
    nc, S = k.nc, k.S
    cb = k.b_const
    A = lambda name, shape, dt: es.enter_context(nc.sbuf_tensor(name, shape, dt))
    k.xres = A("xres", [128, 8, D], F32); k.bx = Buf("xres")
    k.hT = A("hT", [128, 16, TT], BF16); k.bhT = Buf("hT")
    k.actT = A("actT", [128, 16, TT], BF16); k.bactT = [Buf("actT%d" % c) for c in range(16)]
    k.wslots = [(A("wb%d" % i, [128, 4096], BF16), Buf("wb%d" % i)) for i in range(2)]
    k.hb = [(A("hb%d" % i, [128, D], BF16), Buf("hb%d" % i)) for i in range(2)]
    k.stat = A("stat", [128, 32], F32); k.bstat = Buf("stat")
    k.gmix0 = load_gT(k, "gmix0", k.norm_mix_g[0:1, :], dst=A("gmix0", [128, 16], F32)[:, :])
    k.gmix1 = load_gT(k, "gmix1", k.norm_mix_g[1:2, :], dst=A("gmix1", [128, 16], F32)[:, :])
    k.gmlp0 = load_gT(k, "gmlp0", k.norm_mlp_g[0:1, :], dst=A("gmlp0", [128, 16], F32)[:, :])
    k.convw = A("convw", [128, 8, 3], F32)
    for kk in range(3):
        load_gT(k, None, k.conv_w[kk:kk + 1, :], nchunk=8, dst=k.convw[:, :, kk])
    k.wg2e = A("wg2e", [32, 512], BF16)
    k.b_wg2 = Buf("wg2e")
    S.dma('pool', k.wg2e[0:16, :], k.wg2[:, :], k.b_wg2, writes=[k.b_wg2])
    S.dma('pool', k.wg2e[16:17, :], k.bg[:, :], k.b_wg2, writes=[k.b_wg2])
    k.gngB = A("gngB", [128, 256], F32)
    S.dma('sp', k.gngB[:, :], k.gng[0:1, :].partition_broadcast(128), cb, writes=[cb])
    k.triG = A("triG", [128, 128], BF16)
    k.triD = A("triD", [128, 128], BF16)
    k.cmask = A("cmask", [128, 128], F32)
    S.op('pool', lambda e: e.memset(k.triG[:, :], -1.0 / 16), writes=[cb])
    S.op('pool', lambda e: e.affine_select(k.triG[:, :], k.triG[:, :], [[1, 128]], ALU.is_ge, 0.0, base=0,
                                           channel_multiplier=-1), reads=[cb], writes=[cb])
    S.op('pool', lambda e: e.memset(k.triD[:, :], -1.0 / 16), writes=[cb])
    S.op('pool', lambda e: e.affine_select(k.triD[:, :], k.triD[:, :], [[-1, 128]], ALU.is_gt, 0.0, base=0,
                                           channel_multiplier=1), reads=[cb], writes=[cb])
    S.op('pool', lambda e: e.memset(k.cmask[:, :], 1.0), writes=[cb])
    S.op('pool', lambda e: e.affine_select(k.cmask[:, :], k.cmask[:, :], [[1, 128]], ALU.is_ge, 0.0, base=0,
                                           channel_multiplier=-1), reads=[cb], writes=[cb])
    k.arena = A("arena", [128, 8192], BF16)
    ar = k.arena
    k.convst = ar[:, 0:4096].rearrange("p (c t) -> p c t", c=8); k.bconvst = [Buf("convst%d" % c) for c in range(8)]
    k.U = ar[:, 4096:5632].bitcast(F32)[:, 0:ST + 2]; k.bU = Buf("U")
    k.actmp = ar[:, 5632:6656].bitcast(F32); k.bactmp = Buf("actmp")
    k.t1 = k.actmp; k.bt1 = k.bactmp
    k.halo = A("halo", [128, 8, 2], F32); k.bhalo = Buf("halo")
    k.glowT = A("glowT", [32, ST], BF16); k.bglow = Buf("glowT")
    k.sp_tok = A("sp_tok", [128, 4, 512], BF16); k.bsp = Buf("sp_tok")
    k.qT = A("qT", [128, ST], BF16); k.bqT = Buf("qT")
    k.kT = A("kT", [128, ST], BF16); k.bkT = Buf("kT")
    k.k_tok = A("k_tok", [128, 4, 128], BF16); k.bktok = Buf("k_tok")
    k.v_tok = A("v_tok", [128, 4, 256], BF16); k.bvtok = Buf("v_tok")
    k.rg = A("rg", [128, 4, 256], F32); k.brg = Buf("rg")
    k.Sst = A("Sst", [128, 4, 256], F32); k.bSst = [Buf("Sst%d" % h) for h in range(4)]
    k.Sbf = A("Sbf", [128, 4, 256], BF16); k.bSbf = [Buf("Sbf%d" % h) for h in range(4)]
    k.ch = []
    for i in range(2):
        d = {}
        for nm, shp, dt in [("Eg", [128, 128], F32), ("Eng", [128, 128], F32), ("Dd", [128, 128], F32),
                            ("qg", [128, 128], BF16), ("kg", [128, 128], BF16), ("kd", [128, 128], BF16),
                            ("sm", [128, 128], BF16), ("of", [128, 256], BF16), ("osq", [128, 256], BF16),
                            ("cst", [128, 4], F32)]:
            d[nm] = (A("%s%d" % (nm, i), shp, dt), Buf("%s%d" % (nm, i)))
        k.ch.append(d)
    k.chi = 0
    k.rtmp = [(A("rtmp%d" % i, [128, 512], F32), Buf("rtmp%d" % i)) for i in range(2)]
    k.rti = 0
    k.sptmp, k.bsptmp = k.rtmp[0]
    k.qstage = [(ar[:, i * 2048:(i + 1) * 2048].rearrange("p (c t) -> p c t", c=2), Buf("qstage%d" % i)) for i in range(2)]
    k.vstage = [(ar[:, 4096 + i * 2048:4096 + (i + 1) * 2048].rearrange("p (m c) -> p m c", m=8), Buf("vstage%d" % i)) for i in range(2)]
    k.sti = 0
    k.dX1 = Buf("dX1"); k.dQT = Buf("dQT"); k.dKT = Buf("dKT"); k.dV = Buf("dV")

    import os
    if os.environ.get("KDEBUG_INIT"):
        for c in range(16):
            S.op('dve', lambda e: e.memset(k.actT[:, c, :], 0.0), writes=[k.bactT[c]])
    for h in range(4):
        S.op('dve', lambda e: e.memset(k.Sst[:, h, :], 0.0), writes=[k.bSst[h]])
        S.op('dve', lambda e: e.memset(k.Sbf[:, h, :], 0.0), writes=[k.bSbf[h]])
    S.op('dve', lambda e: e.memset(k.halo[:, :, :], 0.0), writes=[k.bhalo])
    S.op('dve', lambda e: e.memset(k.glowT[:, :], 1.0), writes=[k.bglow])

    pipe = Pipe(k)
    if k.mode == 'F':
        for t in range(NT):
            mixer_tile(k, pipe, k.x_prev, t, state_only=False, last=False)
            mlp_tile(k, pipe, k.gmlp0, k.w1_0, k.w2_0)
            qkv_tile(k, pipe, t, prev=True)
    else:
        for t in range(NT):
            mixer_tile(k, pipe, k.x_prev, t, state_only=True, last=(t == NT - 1))
    for t in range(NT):
        mixer_tile(k, pipe, k.x_own, t, state_only=False, last=False)
        mlp_tile(k, pipe, k.gmlp0, k.w1_0, k.w2_0)
        qkv_tile(k, pipe, t)
    pipe.run()


def load_x_tile(k, src, t):
    S = k.S
    v = src[t * TT:(t + 1) * TT, :].rearrange("(s p) d -> p s d", p=128)
    for s0 in range(0, 8, 4):
        S.dma('sp', k.xres[:, s0:s0 + 4, :], v[:, s0:s0 + 4, :], k.bx, writes=[k.bx])


def mixer_tile(k, pipe, xsrc, t, state_only, last):
    nc, S = k.nc, k.S
    cb = k.b_const
    W = k.w_in

    def c_load(wv, bwb):
        load_x_tile(k, xsrc, t)
        norm_transpose(k, k.xres, k.bx, k.gmix0, k.hT, k.bhT, 8)
    pipe.add(None, c_load)

    def c_bar(wv, bwb):
        S.barrier()
    pipe.add(None, c_bar)
    for sub in range(TT // ST):
        mixer_sub(k, pipe, sub, state_only, last)

    if not state_only:
        for n in range(8):
            def wout(wv, bwb, n=n):
                def evac_t(m, ps, bps):
                    S.op('dve', lambda e: e.tensor_tensor(k.xres[:, m, n * 256:(n + 1) * 256], ps,
                                                          k.xres[:, m, n * 256:(n + 1) * 256], ALU.add),
                         reads=[bps, k.bx], writes=[k.bx])
                gemm_t(k, wv, bwb, 256, k.actT, k.bactT, range(8), evac_t)
            pipe.add(wblock_loader(k, k.w_out, 0, [(n * 256, 256)]), wout)


def mixer_sub(k, pipe, sub, state_only, last):
    nc, S = k.nc, k.S
    cb = k.b_const
    W = k.w_in
    tb = sub * ST
    hTs = k.hT
    halves = [(tb, ST)]

    conv_steps = []
    if not state_only:
        def mk_conv_c(c):
            def conv_c(wv, bwb):
                def evac(ci, hi, ps, bps):
                    if ci == 0:
                        S.op('act', lambda e: e.copy(k.actmp[:, :], ps), reads=[bps], writes=[k.bactmp])
                    else:
                        S.op('dve', lambda e: e.tensor_copy(k.U[:, 0:2], k.halo[:, c, :]), reads=[k.bhalo], writes=[k.bU])
                        S.op('dve', lambda e: e.tensor_tensor(k.U[:, 2:2 + ST], ps, k.actmp[:, :], ALU.mult),
                             reads=[bps, k.bactmp], writes=[k.bU])
                        S.op('dve', lambda e: e.tensor_copy(k.halo[:, c, :], k.U[:, ST:ST + 2]), reads=[k.bU], writes=[k.bhalo])
                        S.op('act', lambda e: e.activation(k.t1[:, :], k.U[:, 2:2 + ST], AF.Copy, scale=k.convw[:, c, 2:3]),
                             reads=[k.bU, cb], writes=[k.bt1])
                        S.op('dve', lambda e: e.scalar_tensor_tensor(k.t1[:, :], k.U[:, 1:1 + ST], k.convw[:, c, 1:2], k.t1[:, :],
                                                                     ALU.mult, ALU.add),
                             reads=[k.bU, k.bt1, cb], writes=[k.bt1])
                        S.op('dve', lambda e: e.scalar_tensor_tensor(k.convst[:, c, :], k.U[:, 0:ST], k.convw[:, c, 0:1], k.t1[:, :],
                                                                     ALU.mult, ALU.add),
                             reads=[k.bU, k.bt1, cb], writes=[k.bconvst[c]])
                gemm_f(k, wv, bwb, [(0, 128), (128, 128)], hTs, k.bhT, halves, evac)
            return (wblock_loader(k, W, 0, [(1024 + c * 128, 128), (2048 + c * 128, 128)]), conv_c)

        def mk_convb(cp):
            def convb(wv, bwb):
                def evac(ci, hi, ps, bps):
                    c = 2 * cp + ci
                    S.op('dve', lambda e: e.tensor_tensor(k.actT[:, c, tb:tb + ST], ps, k.convst[:, c, :], ALU.mult),
                         reads=[bps, k.bconvst[c]], writes=[k.bactT[c]])
                gemm_f(k, wv, bwb, [(0, 128), (128, 128)], hTs, k.bhT, halves, evac)
                bg_drain(k)
            return (wblock_loader(k, W, 0, [(cp * 256, 256)]), convb)
        for cp in range(4):
            conv_steps.append([mk_conv_c(2 * cp), mk_conv_c(2 * cp + 1), mk_convb(cp)])
    elif last and sub == TT // ST - 1:
        for c in range(8):
            def halo_c(wv, bwb, c=c):
                def evac(ci, hi, ps, bps):
                    if ci == 0:
                        S.op('act', lambda e: e.copy(k.actmp[:, 0:2], ps), reads=[bps], writes=[k.bactmp])
                    else:
                        S.op('dve', lambda e: e.tensor_tensor(k.halo[:, c, :], ps, k.actmp[:, 0:2], ALU.mult),
                             reads=[bps, k.bactmp], writes=[k.bhalo])
                gemm_f(k, wv, bwb, [(0, 128), (128, 128)], hTs, k.bhT, [(TT - 2, 2)], evac)
            pipe.add(wblock_loader(k, W, 0, [(1024 + c * 128, 128), (2048 + c * 128, 128)]), halo_c)

    def gate(wv, bwb):
        def evac(ci, hi, ps, bps):
            S.op('act', lambda e: e.copy(k.glowT[0:16, :], ps), reads=[bps], writes=[k.bglow])
        gemm_f(k, wv, bwb, [(0, 16)], hTs, k.bhT, halves, evac)
        for m in range(4):
            pt, bpt = k.bank()
            S.mm(lambda e: e.matmul(pt[:, 0:512], k.glowT[0:17, m * 128:(m + 1) * 128], k.wg2e[0:17, :],
                                    start=True, stop=True), reads=[k.bglow, k.b_wg2], writes=[bpt])
            S.op('act', lambda e: e.activation(k.sptmp[:, :], pt[:, 0:512], AF.Exp, scale=-1.0),
                 reads=[bpt], writes=[k.bsptmp])
            S.op('act', lambda e: e.activation(k.sp_tok[:, m, :], k.sptmp[:, :], AF.Ln, bias=k.epsc[:, 1:2]),
                 reads=[k.bsptmp, cb], writes=[k.bsp])
    pipe.add(wblock_loader(k, W, 0, [(6144, 16)]), gate)

    for h in range(4):
        if not state_only:
            def qk(wv, bwb, h=h):
                def evac(ci, hi, ps, bps):
                    if ci == 0:
                        S.op('act', lambda e: e.activation(k.qT[:, :], ps, AF.Copy, scale=float(128 ** -0.5)),
                             reads=[bps], writes=[k.bqT])
                    else:
                        S.op('act', lambda e: e.copy(k.kT[:, :], ps), reads=[bps], writes=[k.bkT])
                gemm_f(k, wv, bwb, [(0, 128), (128, 128)], hTs, k.bhT, halves, evac)

                def evac_t(m, ps, bps):
                    S.op('dve', lambda e: e.tensor_copy(k.k_tok[:, m, :], ps), reads=[bps], writes=[k.bktok])
                gemm_t(k, wv[:, :, 128:256], bwb, 128, hTs, k.bhT, range(4), evac_t, t_base=tb)
            pipe.add(wblock_loader(k, W, 0, [(3072 + h * 128, 128), (3584 + h * 128, 128)]), qk)
        else:
            def konly(wv, bwb, h=h):
                def evac_t(m, ps, bps):
                    S.op('dve', lambda e: e.tensor_copy(k.k_tok[:, m, :], ps), reads=[bps], writes=[k.bktok])
                gemm_t(k, wv, bwb, 128, hTs, k.bhT, range(4), evac_t, t_base=tb)
            pipe.add(wblock_loader(k, W, 0, [(3584 + h * 128, 128)]), konly)

        def vproj(wv, bwb, h=h):
            def evac_t(m, ps, bps):
                S.op('act', lambda e: e.copy(k.v_tok[:, m, :], ps), reads=[bps], writes=[k.bvtok])
            gemm_t(k, wv, bwb, 256, hTs, k.bhT, range(4), evac_t, t_base=tb)
            if state_only:
                k.bgen = gla_gen(k, h, tb, state_only=True)
                bg_drain(k)
        pipe.add(wblock_loader(k, W, 0, [(4096 + h * 256, 256)]), vproj)

        if not state_only:
            def rproj(wv, bwb, h=h):
                def evac_t(m, ps, bps):
                    rt, brt = k.rtmp[k.rti % 2]; k.rti += 1
                    S.op('act', lambda e: e.activation(rt[:, 0:256], ps, AF.Silu), reads=[bps], writes=[brt])
                    S.op('dve', lambda e: e.tensor_tensor(k.rg[:, m, :], rt[:, 0:256], k.gngB[:, :], ALU.mult),
                         reads=[brt, cb], writes=[k.brg])
                gemm_t(k, wv, bwb, 256, hTs, k.bhT, range(4), evac_t, t_base=tb)
                bg_drain(k)
                k.bgen = gla_gen(k, h, tb, state_only=False)
            pipe.add(wblock_loader(k, W, 0, [(5120 + h * 256, 256)]), rproj)
            for st in conv_steps[h]:
                pipe.add(*st)


def gla_gen(k, h, tb, state_only):
    nc, S = k.nc, k.S
    cb = k.b_const
    hs = slice(h * 128, (h + 1) * 128)
    Ts = {}

    def front(m):
        T = k.ch[k.chi % 2]; k.chi += 1
        Ts[m] = T
        Eg, bEg = T["Eg"]; Eng, bEng = T["Eng"]; Dd, bDd = T["Dd"]
        qg, bqg = T["qg"]; kg, bkg = T["kg"]; kd, bkd = T["kd"]
        sm, bsm = T["sm"]; cst, bcst = T["cst"]
        ms = slice(m * 128, (m + 1) * 128)
        p2, bp2 = k.bank()
        S.mm(lambda e: e.matmul(p2[:, 0:128], k.triD[:, :], k.sp_tok[:, m, hs], start=True, stop=True),
             reads=[cb, k.bsp], writes=[bp2])
        if not state_only:
            p1, bp1 = k.bank()
            S.mm(lambda e: e.matmul(p1[:, 0:128], k.sp_tok[:, m, hs], k.triG[:, :], start=True, stop=True),
                 reads=[cb, k.bsp], writes=[bp1])
            S.op('act', lambda e: e.activation(Eg[:, :], p1[:, 0:128], AF.Exp), reads=[bp1], writes=[bEg])
            S.op('act', lambda e: e.activation(Eng[:, :], p1[:, 0:128], AF.Exp, scale=-1.0), reads=[bp1], writes=[bEng])
            S.op('dve', lambda e: e.tensor_tensor(qg[:, :], k.qT[:, ms], Eg[:, :], ALU.mult),
                 reads=[k.bqT, bEg], writes=[bqg])
            S.op('dve', lambda e: e.tensor_tensor(kg[:, :], k.kT[:, ms], Eng[:, :], ALU.mult),
                 reads=[k.bkT, bEng], writes=[bkg])
            p3, bp3 = k.bank()
            S.mm(lambda e: e.matmul(p3[:, 0:128], kg[:, :], qg[:, :], start=True, stop=True),
                 reads=[bkg, bqg], writes=[bp3])
            S.op('dve', lambda e: e.tensor_tensor(sm[:, :], p3[:, 0:128], k.cmask[:, :], ALU.mult),
                 reads=[bp3, cb], writes=[bsm])
        else:
            p1, bp1 = k.bank()
            S.mm(lambda e: e.matmul(p1[:, 0:1], k.sp_tok[:, m, hs], k.triG[:, 127:128], start=True, stop=True),
                 reads=[cb, k.bsp], writes=[bp1])
            S.op('act', lambda e: e.activation(cst[:, 2:3], p1[:, 0:1], AF.Exp), reads=[bp1], writes=[bcst])
        S.op('act', lambda e: e.activation(Dd[:, :], p2[:, 0:128], AF.Exp), reads=[bp2], writes=[bDd])
        S.op('dve', lambda e: e.tensor_tensor(kd[:, :], k.k_tok[:, m, :], Dd[:, :], ALU.mult),
             reads=[k.bktok, bDd], writes=[bkd])

    def back(m):
        T = Ts[m]
        Eg, bEg = T["Eg"]
        qg, bqg = T["qg"]; kd, bkd = T["kd"]
        sm, bsm = T["sm"]; of, bof = T["of"]; osq, bosq = T["osq"]; cst, bcst = T["cst"]
        if not state_only:
            egl, begl = Eg[:, 127:128], bEg
            p4, bp4 = k.bank()
            S.mm(lambda e: e.matmul(p4[:, 0:256], sm[:, :], k.v_tok[:, m, :], start=True, stop=False),
                 reads=[bsm, k.bvtok], writes=[bp4], sig=False)
            S.mm(lambda e: e.matmul(p4[:, 0:256], qg[:, :], k.Sbf[:, h, :], start=False, stop=True),
                 reads=[bqg, k.bSbf[h]], writes=[bp4])
        else:
            egl, begl = cst[:, 2:3], bcst
        p6, bp6 = k.bank()
        S.mm(lambda e: e.matmul(p6[:, 0:256], kd[:, :], k.v_tok[:, m, :], start=True, stop=True),
             reads=[bkd, k.bvtok], writes=[bp6])
        S.op('dve', lambda e: e.scalar_tensor_tensor(k.Sst[:, h, :], k.Sst[:, h, :], egl, p6[:, 0:256], ALU.mult, ALU.add),
             reads=[k.bSst[h], begl, bp6], writes=[k.bSst[h]])
        S.op('act', lambda e: e.copy(k.Sbf[:, h, :], k.Sst[:, h, :]), reads=[k.bSst[h]], writes=[k.bSbf[h]])
        if not state_only:
            S.op('act', lambda e: e.activation(osq[:, :], p4[:, 0:256], AF.Square, accum_out=cst[:, 0:1]),
                 reads=[bp4], writes=[bosq, bcst])
            S.op('act', lambda e: e.activation(cst[:, 3:4], cst[:, 0:1], AF.Ln, scale=1.0 / 256, bias=k.epsc[:, 0:1]),
                 reads=[bcst, cb], writes=[bcst])
            S.op('act', lambda e: e.activation(cst[:, 1:2], cst[:, 3:4], AF.Exp, scale=-0.5),
                 reads=[bcst], writes=[bcst])
            S.op('dve', lambda e: e.scalar_tensor_tensor(of[:, :], p4[:, 0:256], cst[:, 1:2], k.rg[:, m, :], ALU.mult, ALU.mult),
                 reads=[bp4, bcst, k.brg], writes=[bof])
            p5, bp5 = k.bank()
            pv = p5[:, :].bitcast(BF16)
            for j in range(2):
                S.mm(lambda e: e.transpose(pv[:, j * 128:(j + 1) * 128], of[:, j * 128:(j + 1) * 128], k.ident[:, :]),
                     reads=[bof, cb], writes=[bp5], sig=(j == 1))
            S.op('act', lambda e: e.copy(k.actT[:, 8 + 2 * h:10 + 2 * h, tb + m * 128:tb + (m + 1) * 128],
                                         pv[:, 0:256].rearrange("p (c t) -> p c t", c=2)),
                 reads=[bp5], writes=[k.bactT[8 + 2 * h], k.bactT[9 + 2 * h]])

    front(0)
    yield
    for m in range(4):
        if m + 1 < 4:
            front(m + 1)
            yield
        back(m)
        if m < 3:
            yield


def tbs(ms):
    return ms


def mlp_tile(k, pipe, gT, W1, W2, final=None):
    S = k.S

    def c_norm(wv, bwb):
        norm_transpose(k, k.xres, k.bx, gT, k.hT, k.bhT, 8)
    pipe.add(None, c_norm)
    halves = [(0, 512), (512, 512)]
    for q in range(4):
        for j in range(8):
            def w1(wv, bwb, j=j):
                def evac(ci, hi, ps, bps):
                    rt, brt = k.rtmp[k.rti % 2]; k.rti += 1
                    S.op('dve', lambda e: e.tensor_scalar(rt[:, :], ps, 0.0, None, ALU.max), reads=[bps], writes=[brt])
                    S.op('act', lambda e: e.activation(k.actT[:, 2 * j + ci, hi * 512:(hi + 1) * 512], rt[:, :], AF.Square),
                         reads=[brt], writes=[k.bactT[2 * j + ci]])
                gemm_f(k, wv, bwb, [(0, 128), (128, 128)], k.hT, k.bhT, halves, evac)
            pipe.add(wblock_loader(k, W1, 0, [(q * 2048 + j * 256, 256)]), w1)
        for n in range(8):
            def w2(wv, bwb, n=n):
                def evac_t(m, ps, bps):
                    S.op('dve', lambda e: e.tensor_tensor(k.xres[:, m, n * 256:(n + 1) * 256], ps,
                                                          k.xres[:, m, n * 256:(n + 1) * 256], ALU.add),
                         reads=[bps, k.bx], writes=[k.bx])
                gemm_t(k, wv, bwb, 256, k.actT, k.bactT, range(8), evac_t)
            pipe.add(wblock_loader(k, W2, q * 2048, [(n * 256, 256)]), w2)


def qkv_tile(k, pipe, t, prev=False):
    S = k.S
    W = k.w_qkv

    def c_store(wv, bwb):
        S.barrier()
        if not prev:
            v = k.X1[t * TT:(t + 1) * TT, :].rearrange("(s p) d -> p s d", p=128)
            for s0 in range(0, 8, 4):
                S.dma('sp', v[:, s0:s0 + 4, :], k.xres[:, s0:s0 + 4, :], k.dX1, reads=[k.bx], writes=[k.dX1])
        norm_transpose(k, k.xres, k.bx, k.gmix1, k.hT, k.bhT, 8)
    pipe.add(None, c_store)
    halves = [(0, 512), (512, 512)]
    Vdst = k.Vp if prev else k.V
    for which, dst, dbuf in ((0, k.QT, k.dQT), (1, k.KTp if prev else k.KT, k.dKT)):
        if prev and which == 0:
            continue
        for j in range(8):
            def qk(wv, bwb, j=j, dst=dst, dbuf=dbuf):
                st, bst = k.qstage[k.sti % 2]; k.sti += 1

                def evac(ci, hi, ps, bps):
                    if hi == 0:
                        S.op('act', lambda e: e.copy(st[:, ci, hi * 512:(hi + 1) * 512], ps), reads=[bps], writes=[bst])
                    else:
                        S.op('dve', lambda e: e.tensor_copy(st[:, ci, hi * 512:(hi + 1) * 512], ps), reads=[bps], writes=[bst])
                gemm_f(k, wv, bwb, [(0, 128), (128, 128)], k.hT, k.bhT, halves, evac)
                for ci in range(2):
                    r0 = (2 * j + ci) * 128
                    S.dma('sp', dst[r0:r0 + 128, t * TT:(t + 1) * TT], st[:, ci, :], bst, reads=[bst], writes=[dbuf])
            pipe.add(wblock_loader(k, W, 0, [(which * D + j * 256, 256)]), qk)
    for n in range(8):
        def vp(wv, bwb, n=n):
            st, bst = k.vstage[k.sti % 2]; k.sti += 1

            def evac_t(m, ps, bps):
                if m % 2 == 0:
                    S.op('act', lambda e: e.copy(st[:, m, :], ps), reads=[bps], writes=[bst])
                else:
                    S.op('dve', lambda e: e.tensor_copy(st[:, m, :], ps), reads=[bps], writes=[bst])
            gemm_t(k, wv, bwb, 256, k.hT, k.bhT, range(8), evac_t)
            dv = Vdst[t * TT:(t + 1) * TT, n * 256:(n + 1) * 256].rearrange("(m p) c -> p m c", p=128)
            S.dma('sp', dv, st[:, :, :], bst, reads=[bst], writes=[k.dV])
        pipe.add(wblock_loader(k, W, 0, [(2 * D + n * 256, 256)]), vp)


def exchange(k):
    nc, S = k.nc, k.S
    S.barrier()
    groups = [[2 * i, 2 * i + 1] for i in range(k.ncores // 2)]
    bkv = Buf("kvall")
    S._deps('pool', [], [bkv])
    ins = nc.gpsimd.collective_compute("AllGather", ALU.bypass, groups,
                                       [k.KV.rearrange("a t d -> (a t) d")],
                                       [k.KVall.rearrange("r a t d -> (r a t) d")])
    bkv.dsem = nc.alloc_semaphore(name="d_kvall")
    S.allsems[id(bkv.dsem)] = bkv
    bkv.dcount = 16
    ins.then_inc(bkv.dsem, 16)


def slopes():
    return [2.0 ** (-8.0 * (h + 1) / NHEAD_ATT) for h in range(NHEAD_ATT)]


DILS = (1, 4, 16)


def phaseB(k):
    from contextlib import ExitStack
    nc, S = k.nc, k.S
    cb = k.b_const
    with ExitStack() as es:
        A = lambda name, shape, dt: es.enter_context(nc.sbuf_tensor(name, shape, dt))
        flag = A("flag_sb", [128, 1], F32)
        S.dma('sp', flag[:, :], k.flag[:, :], cb, writes=[cb])
        ones = A("ones_bf", [128, 128], BF16)
        S.op('pool', lambda e: e.memset(ones[:, :], 1.0), writes=[cb])
        Dm = A("Dm", [128, 256], F32)
        S.op('pool', lambda e: e.iota(Dm[:, :], [[1, 256]], base=0, channel_multiplier=-1, allow_small_or_imprecise_dtypes=True), writes=[cb])
        Dc = A("Dc", [128, 256], F32)
        S.op('dve', lambda e: e.tensor_scalar(Dc[:, :], Dm[:, :], 0.0, 128.0, ALU.max, ALU.min), reads=[cb], writes=[cb])
        EB = A("EB", [128, 48, 256], BF16)
        EBf = A("EBf", [128, 48, 128], BF16)
        M01 = A("M01", [128, 256], F32)
        S.op('pool', lambda e: e.memset(M01[:, :], 1.0), writes=[cb])
        S.op('pool', lambda e: e.affine_select(M01[:, :], M01[:, :], [[1, 256]], ALU.is_ge, 0.0, base=0,
                                               channel_multiplier=-1), reads=[cb], writes=[cb])
        S.op('pool', lambda e: e.affine_select(M01[:, :], M01[:, :], [[-1, 256]], ALU.is_ge, 0.0, base=128,
                                               channel_multiplier=1), reads=[cb], writes=[cb])
        M01f = A("M01f", [128, 128], F32)
        S.op('dve', lambda e: e.tensor_scalar(M01f[:, :], M01[:, 128:256], flag[:, 0:1], None, ALU.mult), reads=[cb], writes=[cb])
        ebts = [(A("ebt%d" % i, [128, 256], F32), Buf("ebt%d" % i)) for i in range(2)]
        sl = slopes()
        for h in range(NHEAD_ATT):
            for di, d in enumerate(DILS):
                idx = h * 3 + di
                ebt, bebt = ebts[idx % 2]
                S.op('act', lambda e: e.activation(ebt[:, :], Dc[:, :], AF.Exp, scale=-float(sl[h] * d)),
                     reads=[cb], writes=[bebt])
                S.op('dve', lambda e: e.tensor_tensor(EB[:, idx, :], ebt[:, :], M01[:, :], ALU.mult), reads=[bebt, cb], writes=[cb])
                S.op('pool', lambda e: e.tensor_tensor(EBf[:, idx, :], ebt[:, 128:256], M01f[:, :], ALU.mult),
                     reads=[bebt, cb], writes=[cb])
        HG = 2
        NG = NHEAD_ATT // HG
        LA = 2
        qks = [(A("qTa%d" % i, [128, HG, NOWN], BF16), Buf("qTa%d" % i),
                A("kTa%d" % i, [128, HG, 2 * NOWN], BF16), Buf("kTa%d" % i)) for i in range(2)]
        vts = [(A("vt%d" % i, [128, 32, HG * 128], BF16), Buf("vt%d" % i)) for i in range(3)]
        acc = A("acc", [128, HG, 2, NOWN], F32); bacc = [Buf("acc%d" % i) for i in range(HG)]
        NPB = LA + 2
        ptmp = [(A("ptmp%d" % i, [128, 256], F32), Buf("ptmp%d" % i)) for i in range(NPB)]
        pbf = [(A("pbf%d" % i, [128, 256], BF16), Buf("pbf%d" % i)) for i in range(NPB)]
        ostg = [(A("ostg%d" % i, [128, NOWN], BF16), Buf("ostg%d" % i)) for i in range(2)]
        rz = A("rz", [128, NOWN], F32); brz = Buf("rz")
        dOT = Buf("dOT")
        scale = float(DH ** -0.5)

        def load_qk(g):
            qT, bq, kT, bk = qks[g % 2]
            for hh in range(HG):
                r0 = (g * HG + hh) * 128
                S.dma('sp', qT[:, hh, :], k.QT[r0:r0 + 128, :], bq, writes=[bq])
                S.dma('sp', kT[:, hh, 0:NOWN], k.KTp[r0:r0 + 128, :], bk, writes=[bk])
                S.dma('sp', kT[:, hh, NOWN:2 * NOWN], k.KT[r0:r0 + 128, :], bk, writes=[bk])

        def load_v(g, di):
            d = DILS[di]
            vt, bvt = vts[di]
            nbh = 16 // d
            vt4 = vt[:, :, :].rearrange("p (r b) c -> p r b c", r=d)
            for half, src in enumerate((k.Vp, k.V)):
                sv = src[:, g * HG * 128:(g + 1) * HG * 128].rearrange("(b p r) c -> p r b c", p=128, r=d)
                for r in range(d):
                    S.dma('sp', vt4[:, r, half * nbh:(half + 1) * nbh, :], sv[:, r, :, :], bvt, writes=[bvt])

        def st_qk(b):
            qT, bq, kT, bk = qks[b['g'] % 2]
            d, hh, r, qb, nbh = b['d'], b['hh'], b['r'], b['qb'], b['nbh']
            kbq = nbh + qb
            pt, bpt = k.bank()
            b['pt'], b['bpt'] = pt, bpt
            q0 = r + d * 128 * qb
            b['q0'] = q0
            qsl = qT[:, hh, q0:q0 + d * 127 + 1:d]
            for ci, kb in enumerate((kbq, kbq - 1)):
                f0 = r + d * 128 * kb
                S.mm(lambda e: e.matmul(pt[:, ci * 128:(ci + 1) * 128], kT[:, hh, f0:f0 + d * 127 + 1:d], qsl,
                                        start=True, stop=True), reads=[bk, bq], writes=[bpt], sig=(ci == 1))
            pm, bpm = ptmp[b['i'] % NPB]; pb, bpb = pbf[b['i'] % NPB]
            b['pb'], b['bpb'] = pb, bpb
            idx = b['idx']
            S.op('act', lambda e: e.activation(pm[:, :], pt[:, 0:256], AF.Exp, scale=scale), reads=[bpt], writes=[bpm])
            me = 'dve'
            if qb == 0:
                S.op(me, lambda e: e.tensor_tensor(pb[:, 0:128], pm[:, 0:128], EB[:, idx, 0:128], ALU.mult),
                     reads=[bpm, cb], writes=[bpb])
                S.op(me, lambda e: e.tensor_tensor(pb[:, 128:256], pm[:, 128:256], EBf[:, idx, :], ALU.mult),
                     reads=[bpm, cb], writes=[bpb])
            else:
                S.op(me, lambda e: e.tensor_tensor(pb[:, :], pm[:, :], EB[:, idx, :], ALU.mult),
                     reads=[bpm, cb], writes=[bpb])

        def st_pv(b):
            d, hh, r, qb, nbh, di = b['d'], b['hh'], b['r'], b['qb'], b['nbh'], b['di']
            vt, bvt = vts[di]
            pb, bpb = b['pb'], b['bpb']
            kbq = nbh + qb
            po, bpo = k.bank()
            for ci, kb in enumerate((kbq, kbq - 1)):
                tile = r * (2 * nbh) + kb
                S.mm(lambda e: e.matmul(po[:, 0:128], vt[:, tile, hh * 128:(hh + 1) * 128], pb[:, ci * 128:(ci + 1) * 128],
                                        start=(ci == 0), stop=(ci == 1)), reads=[bvt, bpb], writes=[bpo], sig=False)
            for ci in range(2):
                S.mm(lambda e: e.matmul(po[:, 128:256], ones[:, :], pb[:, ci * 128:(ci + 1) * 128],
                                        start=(ci == 0), stop=(ci == 1)), reads=[cb, bpb], writes=[bpo], sig=(ci == 1))
            q0 = b['q0']
            asl = acc[:, hh, :, q0:q0 + d * 127 + 1:d]
            pv = po[:, 0:256].rearrange("p (a t) -> p a t", a=2)
            if di == 0:
                S.op('act', lambda e: e.copy(asl, pv), reads=[bpo], writes=[bacc[hh]])
            else:
                S.op('dve', lambda e: e.tensor_tensor(asl, pv, asl, ALU.add), reads=[bpo, bacc[hh]], writes=[bacc[hh]])

        load_qk(0)
        for di in range(3):
            load_v(0, di)
        bi = 0
        for g in range(NG):
            blocks = []
            marks = {}
            for di, d in enumerate(DILS):
                nbh = 16 // d
                for hh in range(HG):
                    for r in range(d):
                        for qb in range(nbh):
                            blocks.append(dict(g=g, di=di, d=d, hh=hh, r=r, qb=qb, nbh=nbh, i=bi,
                                               idx=(g * HG + hh) * 3 + di))
                            bi += 1
                marks[len(blocks) - 1] = di
            n = len(blocks)
            for i in range(n + LA):
                if i < n:
                    st_qk(blocks[i])
                if i - LA >= 0:
                    st_pv(blocks[i - LA])
                    bd = blocks[i - LA]
                    if bd['di'] == 2 and bd['r'] == 15 and bd['qb'] == bd['nbh'] - 1:
                        hh = bd['hh']
                        h = g * HG + hh
                        og, bog = ostg[h % 2]
                        S.op('pool', lambda e: e.memset(rz[:, :], -1.0), writes=[brz])
                        S.op('pool', lambda e: e.tensor_tensor(rz[:, :], acc[:, hh, 1, :], rz[:, :], ALU.pow),
                             reads=[bacc[hh], brz], writes=[brz])
                        S.op('pool', lambda e: e.tensor_tensor(og[:, :], acc[:, hh, 0, :], rz[:, :], ALU.mult),
                             reads=[bacc[hh], brz], writes=[bog])
                        S.dma('sp', k.OT[h * 128:(h + 1) * 128, :], og[:, :], bog, reads=[bog], writes=[dOT])
                    if (i - LA) in marks and g + 1 < NG:
                        di_done = marks[i - LA]
                        if di_done == 0:
                            load_qk(g + 1)
                        load_v(g + 1, di_done)
        S.barrier()
    with ExitStack() as es:
        A = lambda name, shape, dt: es.enter_context(nc.sbuf_tensor(name, shape, dt))
        k.xres = A("xres3", [128, 8, D], F32); k.bx = Buf("xres3")
        k.hT = A("hT3", [128, 16, TT], BF16); k.bhT = Buf("hT3")
        k.actT = A("actT3", [128, 16, TT], BF16); k.bactT = [Buf("actT3_%d" % c) for c in range(16)]
        k.wslots = [(A("wc%d" % i, [128, 4096], BF16), Buf("wc%d" % i)) for i in range(2)]
        k.hb = [(A("hc%d" % i, [128, D], BF16), Buf("hc%d" % i)) for i in range(2)]
        k.stat = A("stat3", [128, 32], F32); k.bstat = Buf("stat3")
        k.rtmp = [(A("rtmq%d" % i, [128, 512], F32), Buf("rtmq%d" % i)) for i in range(2)]
        k.rti = 0
        gfB = A("gfB", [128, D], F32)
        S.dma('sp', gfB[:, :], k.final_g[0:1, :].partition_broadcast(128), cb, writes=[cb])
        k.gmlp1 = load_gT(k, "gmlp1", k.norm_mlp_g1[1:2, :], dst=A("gmlp1", [128, 16], F32)[:, :])
        ost = [(A("ost%d" % i, [128, D], F32), Buf("ost%d" % i)) for i in range(2)]
        dout = Buf("dout")
        pipe = Pipe(k)
        for t in range(NT):
            def c_load(wv, bwb, t=t):
                v = k.X1[t * TT:(t + 1) * TT, :].rearrange("(s p) d -> p s d", p=128)
                for s0 in range(0, 8, 4):
                    S.dma('sp', k.xres[:, s0:s0 + 4, :], v[:, s0:s0 + 4, :], k.bx, writes=[k.bx])
                S.dma('sp', k.actT[:, :, :], k.OT[:, t * TT:(t + 1) * TT].rearrange("(c p) t -> p c t", p=128), k.bactT[0],
                      writes=k.bactT)
            pipe.add(None, c_load)
            for n in range(8):
                def wo(wv, bwb, n=n):
                    def evac_t(m, ps, bps):
                        S.op('dve', lambda e: e.tensor_tensor(k.xres[:, m, n * 256:(n + 1) * 256], ps,
                                                              k.xres[:, m, n * 256:(n + 1) * 256], ALU.add),
                             reads=[bps, k.bx], writes=[k.bx])
                    gemm_t(k, wv, bwb, 256, k.actT, k.bactT, range(8), evac_t)
                pipe.add(wblock_loader(k, k.w_o, 0, [(n * 256, 256)]), wo)
            mlp_tile(k, pipe, k.gmlp1, k.w1_1, k.w2_1)

            def c_final(wv, bwb, t=t):
                for s in range(8):
                    hb, bhb = k.hb[s % 2]
                    S.op('act', lambda e: e.activation(hb[:, :], k.xres[:, s, :], AF.Square, accum_out=k.stat[:, s:s + 1]),
                         reads=[k.bx], writes=[bhb, k.bstat])
                S.op('act', lambda e: e.activation(k.stat[:, 8:16], k.stat[:, 0:8], AF.Ln, scale=1.0 / D, bias=k.epsc[:, 0:1]),
                     reads=[k.bstat, cb], writes=[k.bstat])
                S.op('act', lambda e: e.activation(k.stat[:, 16:24], k.stat[:, 8:16], AF.Exp, scale=-0.5),
                     reads=[k.bstat], writes=[k.bstat])
                for s in range(8):
                    o, bo = ost[s % 2]
                    S.op('dve', lambda e: e.scalar_tensor_tensor(o[:, :], k.xres[:, s, :], k.stat[:, 16 + s:17 + s], gfB[:, :],
                                                                 ALU.mult, ALU.mult), reads=[k.bx, k.bstat, cb], writes=[bo])
                    r0 = t * TT + s * 128
                    S.dma('sp', k.out[r0:r0 + 128, :], o[:, :], bo, reads=[bo], writes=[dout])
            pipe.add(None, c_final)
        pipe.run()
        S.barrier()


FUSED = True
_CACHE = {}


def _get(mode, ncores=8):
    key = (mode, ncores)
    if key not in _CACHE:
        _CACHE[key] = build(mode, ncores)
    return _CACHE[key]


def _maps_A(inp, cores):
    x = inp['x']
    maps = []
    zeros = np.zeros((NOWN, D), np.float32)
    for c in cores:
        b, half = c // 2, c % 2
        maps.append({
            'x_own': np.ascontiguousarray(x[b, half * NOWN:(half + 1) * NOWN]),
            'x_prev': np.ascontiguousarray(x[b, 0:NOWN]) if half == 1 else zeros,
            'norm_mix_g': inp['norm_mix_g'], 'norm_mlp_g': inp['norm_mlp_g'],
            'hyb_w_in': inp['hyb_w_in'][0], 'conv_w': inp['conv_w'][0],
            'gla_w_gate2': inp['gla_w_gate2'][0], 'gla_b_gate': inp['gla_b_gate'],
            'gla_norm_g': inp['gla_norm_g'], 'hyb_w_out': inp['hyb_w_out'][0],
            'attn_w_qkv': inp['attn_w_qkv'][0],
            'mlp_w1_0': inp['mlp_w1'][0], 'mlp_w2_0': inp['mlp_w2'][0],
        })
    return maps


def _maps_B_extra(inp, cores):
    maps = []
    for c in cores:
        half = c % 2
        maps.append({
            'flag': np.full((128, 1), float(half), np.float32),
            'final_norm_g': inp['final_norm_g'].reshape(1, D),
            'attn_w_o': inp['attn_w_o'][0],
            'mlp_w1_1': inp['mlp_w1'][1], 'mlp_w2_1': inp['mlp_w2'][1],
        })
    return maps


def kernel(**inputs):
    inp = {k_: np.ascontiguousarray(np.asarray(v)) for k_, v in inputs.items()}
    B = inp['x'].shape[0]
    ncores = 2 * B
    cores = list(range(ncores))
    if FUSED:
        nc = _get('F', ncores)
        mA = _maps_A(inp, cores)
        mB = _maps_B_extra(inp, cores)
        maps = [dict(a, **b) for a, b in zip(mA, mB)]
        res = run_bass_kernel_spmd(nc, maps, core_ids=cores)
        outs = [np.asarray(r['out']) for r in res.results]
    else:
        ncA = _get('A', ncores)
        resA = run_bass_kernel_spmd(ncA, _maps_A(inp, cores), core_ids=cores).results
        ncB = _get('B', ncores)
        mB = _maps_B_extra(inp, cores)
        zk = None
        for c in cores:
            m = mB[c]
            m['norm_mlp_g'] = inp['norm_mlp_g']
            for nm in ('X1', 'QT', 'KT', 'V'):
                m[nm] = np.asarray(resA[c][nm])
            if c % 2 == 1:
                m['KTp'] = np.asarray(resA[c - 1]['KT'])
                m['Vp'] = np.asarray(resA[c - 1]['V'])
            else:
                if zk is None:
                    zk = np.zeros_like(np.asarray(resA[c]['KT']))
                m['KTp'] = zk
                m['Vp'] = zk
        res = run_bass_kernel_spmd(ncB, mB, core_ids=cores)
        outs = [np.asarray(r['out']) for r in res.results]
    out = np.stack([np.concatenate([outs[2 * b], outs[2 * b + 1]], axis=0) for b in range(B)], axis=0)
    return out.astype(np.float32)
```

```python
import numpy as np
import concourse.bass as bass
import concourse.mybir as mybir
from concourse.bass_utils import run_bass_kernel_spmd

F32 = mybir.dt.float32
BF16 = mybir.dt.bfloat16
AF = mybir.ActivationFunctionType
ALU = mybir.AluOpType
AX = mybir.AxisListType

D = 2048
NOWN = 2048
TT = 1024
NT = NOWN // TT
ST = 512
DFF = 8192
INCOLS = 6160
EPS = 1e-6
NHEAD_ATT = 16
DH = 128


class Buf:
    def __init__(self, name):
        self.name = name
        self.wt = None
        self.rts = []
        self.dsem = None
        self.dcount = 0


class Sched:
    def __init__(self, nc):
        self.nc = nc
        self.eng = {'pe': nc.tensor, 'act': nc.scalar, 'dve': nc.vector, 'pool': nc.gpsimd, 'sp': nc.sync}
        self.sem = {k: nc.alloc_semaphore(name="prog_" + k) for k in self.eng}
        self.cnt = {k: 0 for k in self.eng}
        self.waited = {k: {} for k in self.eng}
        self.nwaits = 0
        self.nops = 0
        self.allsems = {}

    def _deps(self, e, reads, writes):
        need = {}

        def add(t):
            if t is None:
                return
            s, v = t
            k = id(s)
            if k not in need or need[k][1] < v:
                need[k] = (s, v)
        for r in reads:
            add(r.wt)
        for w in writes:
            add(w.wt)
            for t in w.rts:
                add(t)
        wd = self.waited[e]
        for k, (s, v) in need.items():
            if wd.get(k, 0) >= v:
                continue
            if e == 'pe' and s is self.sem['pe']:
                continue
            self.eng[e].wait_ge(s, v)
            wd[k] = v
            self.nwaits += 1

    def _done(self, t, reads, writes):
        for r in reads:
            r.rts = [x for x in r.rts if x[0] is not t[0]] + [t]
        for w in writes:
            w.wt = t
            w.rts = []

    def op(self, e, fn, reads=(), writes=()):
        self._deps(e, reads, writes)
        ins = fn(self.eng[e])
        self.cnt[e] += 1
        ins.then_inc(self.sem[e], 1)
        t = (self.sem[e], self.cnt[e])
        self._done(t, reads, writes)
        self.nops += 1
        return t

    def mm(self, fn, reads=(), writes=(), sig=True):
        self._deps('pe', reads, writes)
        ins = fn(self.eng['pe'])
        self.nops += 1
        if sig:
            self.cnt['pe'] += 1
            ins.then_inc(self.sem['pe'], 1)
        t = (self.sem['pe'], self.cnt['pe'] + (0 if sig else 1))
        self._done(t, reads, writes)
        return t

    def dma(self, q, out_ap, in_ap, semb, reads=(), writes=(), **kw):
        self._deps(q, reads, writes)
        if semb.dsem is None:
            semb.dsem = self.nc.alloc_semaphore(name="d_" + semb.name)
            self.allsems[id(semb.dsem)] = semb
        ins = self.eng[q].dma_start(out=out_ap, in_=in_ap, **kw)
        semb.dcount += 16
        ins.then_inc(semb.dsem, 16)
        t = (semb.dsem, semb.dcount)
        self._done(t, reads, writes)
        self.nops += 1
        return t

    def wait_bufs(self, e, bufs):
        self._deps(e, bufs, bufs)

    def barrier(self, extra_bufs=()):
        for e in self.eng:
            wd = self.waited[e]
            for e2 in self.eng:
                v = self.cnt[e2]
                if v > 0 and wd.get(id(self.sem[e2]), 0) < v:
                    self.eng[e].wait_ge(self.sem[e2], v)
                    wd[id(self.sem[e2])] = v
            for k, b in self.allsems.items():
                if b.dcount > 0 and wd.get(k, 0) < b.dcount:
                    self.eng[e].wait_ge(b.dsem, b.dcount)
                    wd[k] = b.dcount


class K:
    pass


def bc_last(ap, n):
    return ap.unsqueeze(2).broadcast_to([ap.shape[0], ap.shape[1], n])


def build(mode, ncores=8):
    nc = bass.Bass("TRN2", target_bir_lowering=False)
    S = Sched(nc)
    k = K()
    k.nc, k.S, k.mode = nc, S, mode
    k.ncores = ncores
    doA = mode in ('A', 'F')
    doB = mode in ('B', 'F')

    def dram_in(name, shape, dt=F32):
        return nc.dram_tensor(name, shape, dt, kind="ExternalInput").ap()

    def dram_out(name, shape, dt=F32):
        return nc.dram_tensor(name, shape, dt, kind="ExternalOutput").ap()

    def dram_int(name, shape, dt=F32):
        return nc.dram_tensor(name, shape, dt).ap()

    if doA:
        k.x_own = dram_in("x_own", [NOWN, D])
        k.x_prev = dram_in("x_prev", [NOWN, D])
        k.norm_mix_g = dram_in("norm_mix_g", [2, D])
        k.norm_mlp_g = dram_in("norm_mlp_g", [2, D])
        k.w_in = dram_in("hyb_w_in", [D, INCOLS])
        k.conv_w = dram_in("conv_w", [3, 1024])
        k.wg2 = dram_in("gla_w_gate2", [16, 512])
        k.bg = dram_in("gla_b_gate", [1, 512])
        k.gng = dram_in("gla_norm_g", [1, 256])
        k.w_out = dram_in("hyb_w_out", [D, D])
        k.w_qkv = dram_in("attn_w_qkv", [D, 3 * D])
        k.w1_0 = dram_in("mlp_w1_0", [D, DFF])
        k.w2_0 = dram_in("mlp_w2_0", [DFF, D])
    if mode == 'A':
        k.X1 = dram_out("X1", [NOWN, D])
        k.QT = dram_out("QT", [D, NOWN], BF16)
        k.KT = dram_out("KT", [D, NOWN], BF16)
        k.V = dram_out("V", [NOWN, D], BF16)
    elif mode == 'B':
        k.X1 = dram_in("X1", [NOWN, D])
        k.QT = dram_in("QT", [D, NOWN], BF16)
        k.KT = dram_in("KT", [D, NOWN], BF16)
        k.V = dram_in("V", [NOWN, D], BF16)
        k.KTp = dram_in("KTp", [D, NOWN], BF16)
        k.Vp = dram_in("Vp", [NOWN, D], BF16)
    else:
        k.X1 = dram_int("X1", [NOWN, D])
        k.QT = dram_int("QT", [D, NOWN], BF16)
        k.KT = dram_int("KT", [D, NOWN], BF16)
        k.V = dram_int("V", [NOWN, D], BF16)
        k.KTp = dram_int("KTp", [D, NOWN], BF16)
        k.Vp = dram_int("Vp", [NOWN, D], BF16)
    if doB:
        k.flag = dram_in("flag", [128, 1])
        k.norm_mlp_g1 = k.norm_mlp_g if doA else dram_in("norm_mlp_g", [2, D])
        k.final_g = dram_in("final_norm_g", [1, D])
        k.w_o = dram_in("attn_w_o", [D, D])
        k.w1_1 = dram_in("mlp_w1_1", [D, DFF])
        k.w2_1 = dram_in("mlp_w2_1", [DFF, D])
        k.OT = dram_int("OT", [D, NOWN], BF16)
        k.out = dram_out("out", [NOWN, D])

    k.banks = [(nc.alloc_psum_tensor("bank%d" % i, [128, 512], F32), Buf("bank%d" % i)) for i in range(8)]
    k.bank_i = 0

    def bank():
        b = k.banks[k.bank_i % 8]
        k.bank_i += 1
        return b
    k.bank = bank

    k.ident = nc.alloc_sbuf_tensor("ident", [128, 128], BF16)
    k.b_const = Buf("consts")
    cb = k.b_const
    S.op('pool', lambda e: e.memset(k.ident[:, :], 1.0), writes=[cb])
    S.op('pool', lambda e: e.affine_select(k.ident[:, :], k.ident[:, :], [[-1, 128]], ALU.is_equal, 0.0,
                                           base=0, channel_multiplier=1), reads=[cb], writes=[cb])

    k.ident32 = nc.alloc_sbuf_tensor("ident32", [16, 16], F32)
    S.op('pool', lambda e: e.memset(k.ident32[:, :], 1.0), writes=[cb])
    S.op('pool', lambda e: e.affine_select(k.ident32[:, :], k.ident32[:, :], [[-1, 16]], ALU.is_equal, 0.0,
                                           base=0, channel_multiplier=1), reads=[cb], writes=[cb])
    k.gstage = (nc.alloc_sbuf_tensor("gstage", [16, 128], F32), Buf("gstage"))
    k.epsc = nc.alloc_sbuf_tensor("epsc", [128, 2], F32)
    S.op('pool', lambda e: e.memset(k.epsc[:, 0:1], EPS), writes=[cb])
    S.op('pool', lambda e: e.memset(k.epsc[:, 1:2], 1.0), writes=[cb])
    if doA:
        phaseA(k)
    if doB:
        S.barrier()
        phaseB(k)
    S.barrier()
    print("built mode", mode, "ops", S.nops, "waits", S.nwaits)
    return nc


def load_gT(k, name, src_row_ap, nchunk=16, dst=None):
    nc, S = k.nc, k.S
    b = k.b_const
    if dst is None:
        dst = nc.alloc_sbuf_tensor(name, [128, nchunk], F32)[:, :]
    st, bst = k.gstage
    S.dma('sp', st[0:nchunk, :], src_row_ap.rearrange("o (c p) -> (o c) p", p=128), bst, writes=[bst])
    pt, bpt = k.bank()
    S.mm(lambda e: e.matmul(pt[:, 0:nchunk], st[0:nchunk, :], k.ident32[0:nchunk, 0:nchunk], start=True, stop=True),
         reads=[bst, b], writes=[bpt])
    S.op('dve', lambda e: e.tensor_copy(dst, pt[:, 0:nchunk]), reads=[bpt], writes=[b])
    return dst


def norm_transpose(k, xres, bx, gT, hT, bhT, nsub):
    nc, S = k.nc, k.S
    for s in range(nsub):
        hb, bhb = k.hb[s % 2]
        S.op('act', lambda e: e.activation(hb[:, :], xres[:, s, :], AF.Square, accum_out=k.stat[:, s:s + 1]),
             reads=[bx], writes=[bhb, k.bstat])
    S.op('act', lambda e: e.activation(k.stat[:, 8:8 + nsub], k.stat[:, 0:nsub], AF.Ln, scale=1.0 / D, bias=k.epsc[:, 0:1]),
         reads=[k.bstat, k.b_const], writes=[k.bstat])
    S.op('act', lambda e: e.activation(k.stat[:, 16:16 + nsub], k.stat[:, 8:8 + nsub], AF.Exp, scale=-0.5),
         reads=[k.bstat], writes=[k.bstat])
    for s in range(nsub):
        hb, bhb = k.hb[s % 2]
        rstd = k.stat[:, 16 + s:17 + s]
        if s % 2 == 0:
            S.op('act', lambda e: e.activation(hb[:, :], xres[:, s, :], AF.Copy, scale=rstd),
                 reads=[bx, k.bstat], writes=[bhb])
        else:
            S.op('dve', lambda e: e.tensor_scalar(hb[:, :], xres[:, s, :], rstd, None, ALU.mult),
                 reads=[bx, k.bstat], writes=[bhb])
        for cg in range(2):
            pt, bpt = k.bank()
            pv = pt[:, :].bitcast(BF16)
            for c in range(8):
                cc = cg * 8 + c
                S.mm(lambda e: e.transpose(pv[:, c * 128:(c + 1) * 128], hb[:, cc * 128:(cc + 1) * 128], k.ident[:, :]),
                     reads=[bhb, k.b_const], writes=[bpt], sig=(c == 7))
            pv3 = pv.rearrange("p (c t) -> p c t", c=8)
            S.op('dve', lambda e: e.tensor_tensor(hT[:, cg * 8:(cg + 1) * 8, s * 128:(s + 1) * 128], pv3,
                                                  bc_last(gT[:, cg * 8:(cg + 1) * 8], 128), ALU.mult),
                 reads=[bpt, k.b_const], writes=[bhT])


def wblock_loader(k, W, r0, segs, nk=16):
    S = k.S
    ncols = sum(n for _, n in segs)

    def load(slot):
        wb, bwb = slot
        v = wb[:, 0:nk * ncols].rearrange("p (k c) -> p k c", k=nk)
        off = 0
        for (c0, n) in segs:
            src = W[r0:r0 + nk * 128, c0:c0 + n].rearrange("(kc p) c -> p kc c", p=128)
            S.dma('pool', v[:, :, off:off + n], src, bwb, writes=[bwb])
            off += n
        return v
    return load


def gemm_f(k, wv, bwb, chunks, rhsT, brhs, halves, evac):
    S = k.S
    nk = wv.shape[1]
    for ci, (off, width) in enumerate(chunks):
        for hi, (t0, n) in enumerate(halves):
            pt, bpt = k.bank()
            for kk in range(nk):
                S.mm(lambda e: e.matmul(pt[0:width, 0:n], wv[:, kk, off:off + width], rhsT[:, kk, t0:t0 + n],
                                        start=(kk == 0), stop=(kk == nk - 1)),
                     reads=[bwb, brhs], writes=[bpt], sig=(kk == nk - 1))
            evac(ci, hi, pt[0:width, 0:n], bpt)
            bg_tick(k)


def gemm_t(k, wv, bwb, ncols, lhsT, blhs, subtiles, evac, t_base=0):
    S = k.S
    nk = wv.shape[1]
    for m in subtiles:
        pt, bpt = k.bank()
        t0 = t_base + m * 128
        for kk in range(nk):
            S.mm(lambda e: e.matmul(pt[:, 0:ncols], lhsT[:, kk, t0:t0 + 128], wv[:, kk, 0:ncols],
                                    start=(kk == 0), stop=(kk == nk - 1)),
                 reads=[bwb, blhs[kk] if isinstance(blhs, list) else blhs], writes=[bpt], sig=(kk == nk - 1))
        evac(m, pt[:, 0:ncols], bpt)


def bg_tick(k):
    g = getattr(k, 'bgen', None)
    if g is not None:
        try:
            next(g)
        except StopIteration:
            k.bgen = None


def bg_drain(k):
    while getattr(k, 'bgen', None) is not None:
        bg_tick(k)


class Pipe:
    def __init__(self, k):
        self.k = k
        self.steps = []

    def add(self, loader, compute):
        self.steps.append((loader, compute))

    def run(self):
        k = self.k
        slots = k.wslots
        ns = len(slots)
        views = {}
        li = 0
        wi = 0
        import os
        sk = os.environ.get("KSKIP")
        if sk:
            rngs = [[int(v) for v in r.split(":")] for r in sk.split(",")]
            self.steps = [st for i, st in enumerate(self.steps) if not any(a <= i < b for a, b in rngs)]
        order = [i for i, (l, c) in enumerate(self.steps) if l is not None]
        slot_of = {i: slots[j % ns] for j, i in enumerate(order)}
        pos = 0

        def issue_next():
            nonlocal pos
            if pos < len(order):
                i = order[pos]
                views[i] = self.steps[i][0](slot_of[i])
                pos += 1
        for _ in range(ns - 1):
            issue_next()
        import os
        lim = int(os.environ.get("KSTEPS", "1000000"))
        for i, (l, c) in enumerate(self.steps):
            if i >= lim:
                break
            if l is not None:
                while i not in views:
                    issue_next()
                c(views[i], slot_of[i][1])
                issue_next()
                del views[i]
            else:
                c(None, None)
        self.steps = []


def phaseA(k):
    from contextlib import ExitStack
    with ExitStack() as es:
        _phaseA(k, es)
        k.S.barrier()


def _phaseA(k, es):
    nc, S = k.nc, k.S
    cb = k.b_const
    A = lambda name, shape, dt: es.enter_context(nc.sbuf_tensor(name, shape, dt))
    k.xres = A("xres", [128, 8, D], F32); k.bx = Buf("xres")
    k.hT = A("hT", [128, 16, TT], BF16); k.bhT = Buf("hT")
    k.actT = A("actT", [128, 16, TT], BF16); k.bactT = [Buf("actT%d" % c) for c in range(16)]
    k.wslots = [(A("wb%d" % i, [128, 4096], BF16), Buf("wb%d" % i)) for i in range(2)]
    k.hb = [(A("hb%d" % i, [128, D], BF16), Buf("hb%d" % i)) for i in range(2)]
    k.stat = A("stat", [128, 32], F32); k.bstat = Buf("stat")
    k.gmix0 = load_gT(k, "gmix0", k.norm_mix_g[0:1, :], dst=A("gmix0", [128, 16], F32)[:, :])
    k.gmix1 = load_gT(k, "gmix1", k.norm_mix_g[1:2, :], dst=A("gmix1", [128, 16], F32)[:, :])
    k.gmlp0 = load_gT(k, "gmlp0", k.norm_mlp_g[0:1, :], dst=A("gmlp0", [128, 16], F32)[:, :])
    k.convw = A("convw", [128, 8, 3], F32)
    for kk in range(3):
        load_gT(k, None, k.conv_w[kk:kk + 1, :], nchunk=8, dst=k.convw[:, :, kk])
    k.wg2e = A("wg2e", [32, 512], BF16)
    k.b_wg2 = Buf("wg2e")
    S.dma('pool', k.wg2e[0:16, :], k.wg2[:, :], k.b_wg2, writes=[k.b_wg2])
    S.dma('pool', k.wg2e[16:17, :], k.bg[:, :], k.b_wg2, writes=[k.b_wg2])
    k.gngB = A("gngB", [128, 256], F32)
    S.dma('sp', k.gngB[:, :], k.gng[0:1, :].partition_broadcast(128), cb, writes=[cb])
    k.triG = A("triG", [128, 128], BF16)
    k.triD = A("triD", [128, 128], BF16)
    k.cmask = A("cmask", [128, 128], F32)
    S.op('pool', lambda e: e.memset(k.triG[:, :], -1.0 / 16), writes=[cb])
    S.op('pool', lambda e: e.affine_select(k.triG[:, :], k.triG[:, :], [[1, 128]], ALU.is_ge, 0.0, base=0,
                                           channel_multiplier=-1), reads=[cb], writes=[cb])
    S.op('pool', lambda e: e.memset(k.triD[:, :], -1.0 / 16), writes=[cb])
    S.op('pool', lambda e: e.affine_select(k.triD[:, :], k.triD[:, :], [[-1, 128]], ALU.is_gt, 0.0, base=0,
                                           channel_multiplier=1), reads=[cb], writes=[cb])
    S.op('pool', lambda e: e.memset(k.cmask[:, :], 1.0), writes=[cb])
    S.op('pool', lambda e: e.affine_select(k.cmask[:, :], k.cmask[:, :], [[1, 128]], ALU.is_ge, 0.0, base=0,
                                           channel_multiplier=-1), reads=[cb], writes=[cb])
    k.arena = A("arena", [128, 8192], BF16)
    ar = k.arena
    k.convst = ar[:, 0:4096].rearrange("p (c t) -> p c t", c=8); k.bconvst = [Buf("convst%d" % c) for c in range(8)]
    k.U = ar[:, 4096:5632].bitcast(F32)[:, 0:ST + 2]; k.bU = Buf("U")
    k.actmp = ar[:, 5632:6656].bitcast(F32); k.bactmp = Buf("actmp")
    k.t1 = k.actmp; k.bt1 = k.bactmp
    k.halo = A("halo", [128, 8, 2], F32); k.bhalo = Buf("halo")
    k.glowT = A("glowT", [32, ST], BF16); k.bglow = Buf("glowT")
    k.sp_tok = A("sp_tok", [128, 4, 512], BF16); k.bsp = Buf("sp_tok")
    k.qT = A("qT", [128, ST], BF16); k.bqT = Buf("qT")
    k.kT = A("kT", [128, ST], BF16); k.bkT = Buf("kT")
    k.k_tok = A("k_tok", [128, 4, 128], BF16); k.bktok = Buf("k_tok")
    k.v_tok = A("v_tok", [128, 4, 256], BF16); k.bvtok = Buf("v_tok")
    k.rg = A("rg", [128, 4, 256], F32); k.brg = Buf("rg")
    k.Sst = A("Sst", [128, 4, 256], F32); k.bSst = [Buf("Sst%d" % h) for h in range(4)]
    k.Sbf = A("Sbf", [128, 4, 256], BF16); k.bSbf = [Buf("Sbf%d" % h) for h in range(4)]
    k.ch = []
    for i in range(2):
        d = {}
        for nm, shp, dt in [("Eg", [128, 128], F32), ("Eng", [128, 128], F32), ("Dd", [128, 128], F32),
                            ("qg", [128, 128], BF16), ("kg", [128, 128], BF16), ("kd", [128, 128], BF16),
                            ("sm", [128, 128], BF16), ("of", [128, 256], BF16), ("osq", [128, 256], BF16),
                            ("cst", [128, 4], F32)]:
            d[nm] = (A("%s%d" % (nm, i), shp, dt), Buf("%s%d" % (nm, i)))
        k.ch.append(d)
    k.chi = 0
    k.rtmp = [(A("rtmp%d" % i, [128, 512], F32), Buf("rtmp%d" % i)) for i in range(2)]
    k.rti = 0
    k.sptmp, k.bsptmp = k.rtmp[0]
    k.qstage = [(ar[:, i * 2048:(i + 1) * 2048].rearrange("p (c t) -> p c t", c=2), Buf("qstage%d" % i)) for i in range(2)]
    k.vstage = [(ar[:, 4096 + i * 2048:4096 + (i + 1) * 2048].rearrange("p (m c) -> p m c", m=8), Buf("vstage%d" % i)) for i in range(2)]
    k.sti = 0
    k.dX1 = Buf("dX1"); k.dQT = Buf("dQT"); k.dKT = Buf("dKT"); k.dV = Buf("dV")

    import os
    if os.environ.get("KDEBUG_INIT"):
        for c in range(16):
            S.op('dve', lambda e: e.memset(k.actT[:, c, :], 0.0), writes=[k.bactT[c]])
    for h in range(4):
        S.op('dve', lambda e: e.memset(k.Sst[:, h, :], 0.0), writes=[k.bSst[h]])
        S.op('dve', lambda e: e.memset(k.Sbf[:, h, :], 0.0), writes=[k.bSbf[h]])
    S.op('dve', lambda e: e.memset(k.halo[:, :, :], 0.0), writes=[k.bhalo])
    S.op('dve', lambda e: e.memset(k.glowT[:, :], 1.0), writes=[k.bglow])

    pipe = Pipe(k)
    if k.mode == 'F':
        for t in range(NT):
            mixer_tile(k, pipe, k.x_prev, t, state_only=False, last=False)
            mlp_tile(k, pipe, k.gmlp0, k.w1_0, k.w2_0)
            qkv_tile(k, pipe, t, prev=True)
    else:
        for t in range(NT):
            mixer_tile(k, pipe, k.x_prev, t, state_only=True, last=(t == NT - 1))
    for t in range(NT):
        mixer_tile(k, pipe, k.x_own, t, state_only=False, last=False)
        mlp_tile(k, pipe, k.gmlp0, k.w1_0, k.w2_0)
        qkv_tile(k, pipe, t)
    pipe.run()


def load_x_tile(k, src, t):
    S = k.S
    v = src[t * TT:(t + 1) * TT, :].rearrange("(s p) d -> p s d", p=128)
    for s0 in range(0, 8, 4):
        S.dma('sp', k.xres[:, s0:s0 + 4, :], v[:, s0:s0 + 4, :], k.bx, writes=[k.bx])


def mixer_tile(k, pipe, xsrc, t, state_only, last):
    nc, S = k.nc, k.S
    cb = k.b_const
    W = k.w_in

    def c_load(wv, bwb):
        load_x_tile(k, xsrc, t)
        norm_transpose(k, k.xres, k.bx, k.gmix0, k.hT, k.bhT, 8)
    pipe.add(None, c_load)

    def c_bar(wv, bwb):
        S.barrier()
    pipe.add(None, c_bar)
    for sub in range(TT // ST):
        mixer_sub(k, pipe, sub, state_only, last)

    if not state_only:
        for n in range(8):
            def wout(wv, bwb, n=n):
                def evac_t(m, ps, bps):
                    S.op('dve', lambda e: e.tensor_tensor(k.xres[:, m, n * 256:(n + 1) * 256], ps,
                                                          k.xres[:, m, n * 256:(n + 1) * 256], ALU.add),
                         reads=[bps, k.bx], writes=[k.bx])
                gemm_t(k, wv, bwb, 256, k.actT, k.bactT, range(8), evac_t)
            pipe.add(wblock_loader(k, k.w_out, 0, [(n * 256, 256)]), wout)


def mixer_sub(k, pipe, sub, state_only, last):
    nc, S = k.nc, k.S
    cb = k.b_const
    W = k.w_in
    tb = sub * ST
    hTs = k.hT
    halves = [(tb, ST)]

    conv_steps = []
    if not state_only:
        def mk_conv_c(c):
            def conv_c(wv, bwb):
                def evac(ci, hi, ps, bps):
                    if ci == 0:
                        S.op('act', lambda e: e.copy(k.actmp[:, :], ps), reads=[bps], writes=[k.bactmp])
                    else:
                        S.op('dve', lambda e: e.tensor_copy(k.U[:, 0:2], k.halo[:, c, :]), reads=[k.bhalo], writes=[k.bU])
                        S.op('dve', lambda e: e.tensor_tensor(k.U[:, 2:2 + ST], ps, k.actmp[:, :], ALU.mult),
                             reads=[bps, k.bactmp], writes=[k.bU])
                        S.op('dve', lambda e: e.tensor_copy(k.halo[:, c, :], k.U[:, ST:ST + 2]), reads=[k.bU], writes=[k.bhalo])
                        S.op('act', lambda e: e.activation(k.t1[:, :], k.U[:, 2:2 + ST], AF.Copy, scale=k.convw[:, c, 2:3]),
                             reads=[k.bU, cb], writes=[k.bt1])
                        S.op('dve', lambda e: e.scalar_tensor_tensor(k.t1[:, :], k.U[:, 1:1 + ST], k.convw[:, c, 1:2], k.t1[:, :],
                                                                     ALU.mult, ALU.add),
                             reads=[k.bU, k.bt1, cb], writes=[k.bt1])
                        S.op('dve', lambda e: e.scalar_tensor_tensor(k.convst[:, c, :], k.U[:, 0:ST], k.convw[:, c, 0:1], k.t1[:, :],
                                                                     ALU.mult, ALU.add),
                             reads=[k.bU, k.bt1, cb], writes=[k.bconvst[c]])
                gemm_f(k, wv, bwb, [(0, 128), (128, 128)], hTs, k.bhT, halves, evac)
            return (wblock_loader(k, W, 0, [(1024 + c * 128, 128), (2048 + c * 128, 128)]), conv_c)

        def mk_convb(cp):
            def convb(wv, bwb):
                def evac(ci, hi, ps, bps):
                    c = 2 * cp + ci
                    S.op('dve', lambda e: e.tensor_tensor(k.actT[:, c, tb:tb + ST], ps, k.convst[:, c, :], ALU.mult),
                         reads=[bps, k.bconvst[c]], writes=[k.bactT[c]])
                gemm_f(k, wv, bwb, [(0, 128), (128, 128)], hTs, k.bhT, halves, evac)
                bg_drain(k)
            return (wblock_loader(k, W, 0, [(cp * 256, 256)]), convb)
        for cp in range(4):
            conv_steps.append([mk_conv_c(2 * cp), mk_conv_c(2 * cp + 1), mk_convb(cp)])
    elif last and sub == TT // ST - 1:
        for c in range(8):
            def halo_c(wv, bwb, c=c):
                def evac(ci, hi, ps, bps):
                    if ci == 0:
                        S.op('act', lambda e: e.copy(k.actmp[:, 0:2], ps), reads=[bps], writes=[k.bactmp])
                    else:
                        S.op('dve', lambda e: e.tensor_tensor(k.halo[:, c, :], ps, k.actmp[:, 0:2], ALU.mult),
                             reads=[bps, k.bactmp], writes=[k.bhalo])
                gemm_f(k, wv, bwb, [(0, 128), (128, 128)], hTs, k.bhT, [(TT - 2, 2)], evac)
            pipe.add(wblock_loader(k, W, 0, [(1024 + c * 128, 128), (2048 + c * 128, 128)]), halo_c)

    def gate(wv, bwb):
        def evac(ci, hi, ps, bps):
            S.op('act', lambda e: e.copy(k.glowT[0:16, :], ps), reads=[bps], writes=[k.bglow])
        gemm_f(k, wv, bwb, [(0, 16)], hTs, k.bhT, halves, evac)
        for m in range(4):
            pt, bpt = k.bank()
            S.mm(lambda e: e.matmul(pt[:, 0:512], k.glowT[0:17, m * 128:(m + 1) * 128], k.wg2e[0:17, :],
                                    start=True, stop=True), reads=[k.bglow, k.b_wg2], writes=[bpt])
            S.op('act', lambda e: e.activation(k.sptmp[:, :], pt[:, 0:512], AF.Exp, scale=-1.0),
                 reads=[bpt], writes=[k.bsptmp])
            S.op('act', lambda e: e.activation(k.sp_tok[:, m, :], k.sptmp[:, :], AF.Ln, bias=k.epsc[:, 1:2]),
                 reads=[k.bsptmp, cb], writes=[k.bsp])
    pipe.add(wblock_loader(k, W, 0, [(6144, 16)]), gate)

    for h in range(4):
        if not state_only:
            def qk(wv, bwb, h=h):
                def evac(ci, hi, ps, bps):
                    if ci == 0:
                        S.op('act', lambda e: e.activation(k.qT[:, :], ps, AF.Copy, scale=float(128 ** -0.5)),
                             reads=[bps], writes=[k.bqT])
                    else:
                        S.op('act', lambda e: e.copy(k.kT[:, :], ps), reads=[bps], writes=[k.bkT])
                gemm_f(k, wv, bwb, [(0, 128), (128, 128)], hTs, k.bhT, halves, evac)

                def evac_t(m, ps, bps):
                    S.op('dve', lambda e: e.tensor_copy(k.k_tok[:, m, :], ps), reads=[bps], writes=[k.bktok])
                gemm_t(k, wv[:, :, 128:256], bwb, 128, hTs, k.bhT, range(4), evac_t, t_base=tb)
            pipe.add(wblock_loader(k, W, 0, [(3072 + h * 128, 128), (3584 + h * 128, 128)]), qk)
        else:
            def konly(wv, bwb, h=h):
                def evac_t(m, ps, bps):
                    S.op('dve', lambda e: e.tensor_copy(k.k_tok[:, m, :], ps), reads=[bps], writes=[k.bktok])
                gemm_t(k, wv, bwb, 128, hTs, k.bhT, range(4), evac_t, t_base=tb)
            pipe.add(wblock_loader(k, W, 0, [(3584 + h * 128, 128)]), konly)

        def vproj(wv, bwb, h=h):
            def evac_t(m, ps, bps):
                S.op('act', lambda e: e.copy(k.v_tok[:, m, :], ps), reads=[bps], writes=[k.bvtok])
            gemm_t(k, wv, bwb, 256, hTs, k.bhT, range(4), evac_t, t_base=tb)
            if state_only:
                k.bgen = gla_gen(k, h, tb, state_only=True)
                bg_drain(k)
        pipe.add(wblock_loader(k, W, 0, [(4096 + h * 256, 256)]), vproj)

        if not state_only:
            def rproj(wv, bwb, h=h):
                def evac_t(m, ps, bps):
                    rt, brt = k.rtmp[k.rti % 2]; k.rti += 1
                    S.op('act', lambda e: e.activation(rt[:, 0:256], ps, AF.Silu), reads=[bps], writes=[brt])
                    S.op('dve', lambda e: e.tensor_tensor(k.rg[:, m, :], rt[:, 0:256], k.gngB[:, :], ALU.mult),
                         reads=[brt, cb], writes=[k.brg])
                gemm_t(k, wv, bwb, 256, hTs, k.bhT, range(4), evac_t, t_base=tb)
                bg_drain(k)
                k.bgen = gla_gen(k, h, tb, state_only=False)
            pipe.add(wblock_loader(k, W, 0, [(5120 + h * 256, 256)]), rproj)
            for st in conv_steps[h]:
                pipe.add(*st)


def gla_gen(k, h, tb, state_only):
    nc, S = k.nc, k.S
    cb = k.b_const
    hs = slice(h * 128, (h + 1) * 128)
    Ts = {}

    def front(m):
        T = k.ch[k.chi % 2]; k.chi += 1
        Ts[m] = T
        Eg, bEg = T["Eg"]; Eng, bEng = T["Eng"]; Dd, bDd = T["Dd"]
        qg, bqg = T["qg"]; kg, bkg = T["kg"]; kd, bkd = T["kd"]
        sm, bsm = T["sm"]; cst, bcst = T["cst"]
        ms = slice(m * 128, (m + 1) * 128)
        p2, bp2 = k.bank()
        S.mm(lambda e: e.matmul(p2[:, 0:128], k.triD[:, :], k.sp_tok[:, m, hs], start=True, stop=True),
             reads=[cb, k.bsp], writes=[bp2])
        if not state_only:
            p1, bp1 = k.bank()
            S.mm(lambda e: e.matmul(p1[:, 0:128], k.sp_tok[:, m, hs], k.triG[:, :], start=True, stop=True),
                 reads=[cb, k.bsp], writes=[bp1])
            S.op('act', lambda e: e.activation(Eg[:, :], p1[:, 0:128], AF.Exp), reads=[bp1], writes=[bEg])
            S.op('act', lambda e: e.activation(Eng[:, :], p1[:, 0:128], AF.Exp, scale=-1.0), reads=[bp1], writes=[bEng])
            S.op('dve', lambda e: e.tensor_tensor(qg[:, :], k.qT[:, ms], Eg[:, :], ALU.mult),
                 reads=[k.bqT, bEg], writes=[bqg])
            S.op('dve', lambda e: e.tensor_tensor(kg[:, :], k.kT[:, ms], Eng[:, :], ALU.mult),
                 reads=[k.bkT, bEng], writes=[bkg])
            p3, bp3 = k.bank()
            S.mm(lambda e: e.matmul(p3[:, 0:128], kg[:, :], qg[:, :], start=True, stop=True),
                 reads=[bkg, bqg], writes=[bp3])
            S.op('dve', lambda e: e.tensor_tensor(sm[:, :], p3[:, 0:128], k.cmask[:, :], ALU.mult),
                 reads=[bp3, cb], writes=[bsm])
        else:
            p1, bp1 = k.bank()
            S.mm(lambda e: e.matmul(p1[:, 0:1], k.sp_tok[:, m, hs], k.triG[:, 127:128], start=True, stop=True),
                 reads=[cb, k.bsp], writes=[bp1])
            S.op('act', lambda e: e.activation(cst[:, 2:3], p1[:, 0:1], AF.Exp), reads=[bp1], writes=[bcst])
        S.op('act', lambda e: e.activation(Dd[:, :], p2[:, 0:128], AF.Exp), reads=[bp2], writes=[bDd])
        S.op('dve', lambda e: e.tensor_tensor(kd[:, :], k.k_tok[:, m, :], Dd[:, :], ALU.mult),
             reads=[k.bktok, bDd], writes=[bkd])

    def back(m):
        T = Ts[m]
        Eg, bEg = T["Eg"]
        qg, bqg = T["qg"]; kd, bkd = T["kd"]
        sm, bsm = T["sm"]; of, bof = T["of"]; osq, bosq = T["osq"]; cst, bcst = T["cst"]
        if not state_only:
            egl, begl = Eg[:, 127:128], bEg
            p4, bp4 = k.bank()
            S.mm(lambda e: e.matmul(p4[:, 0:256], sm[:, :], k.v_tok[:, m, :], start=True, stop=False),
                 reads=[bsm, k.bvtok], writes=[bp4], sig=False)
            S.mm(lambda e: e.matmul(p4[:, 0:256], qg[:, :], k.Sbf[:, h, :], start=False, stop=True),
                 reads=[bqg, k.bSbf[h]], writes=[bp4])
        else:
            egl, begl = cst[:, 2:3], bcst
        p6, bp6 = k.bank()
        S.mm(lambda e: e.matmul(p6[:, 0:256], kd[:, :], k.v_tok[:, m, :], start=True, stop=True),
             reads=[bkd, k.bvtok], writes=[bp6])
        S.op('dve', lambda e: e.scalar_tensor_tensor(k.Sst[:, h, :], k.Sst[:, h, :], egl, p6[:, 0:256], ALU.mult, ALU.add),
             reads=[k.bSst[h], begl, bp6], writes=[k.bSst[h]])
        S.op('act', lambda e: e.copy(k.Sbf[:, h, :], k.Sst[:, h, :]), reads=[k.bSst[h]], writes=[k.bSbf[h]])
        if not state_only:
            S.op('act', lambda e: e.activation(osq[:, :], p4[:, 0:256], AF.Square, accum_out=cst[:, 0:1]),
                 reads=[bp4], writes=[bosq, bcst])
            S.op('act', lambda e: e.activation(cst[:, 3:4], cst[:, 0:1], AF.Ln, scale=1.0 / 256, bias=k.epsc[:, 0:1]),
                 reads=[bcst, cb], writes=[bcst])
            S.op('act', lambda e: e.activation(cst[:, 1:2], cst[:, 3:4], AF.Exp, scale=-0.5),
                 reads=[bcst], writes=[bcst])
            S.op('dve', lambda e: e.scalar_tensor_tensor(of[:, :], p4[:, 0:256], cst[:, 1:2], k.rg[:, m, :], ALU.mult, ALU.mult),
                 reads=[bp4, bcst, k.brg], writes=[bof])
            p5, bp5 = k.bank()
            pv = p5[:, :].bitcast(BF16)
            for j in range(2):
                S.mm(lambda e: e.transpose(pv[:, j * 128:(j + 1) * 128], of[:, j * 128:(j + 1) * 128], k.ident[:, :]),
                     reads=[bof, cb], writes=[bp5], sig=(j == 1))
            S.op('act', lambda e: e.copy(k.actT[:, 8 + 2 * h:10 + 2 * h, tb + m * 128:tb + (m + 1) * 128],
                                         pv[:, 0:256].rearrange("p (c t) -> p c t", c=2)),
                 reads=[bp5], writes=[k.bactT[8 + 2 * h], k.bactT[9 + 2 * h]])

    front(0)
    yield
    for m in range(4):
        if m + 1 < 4:
            front(m + 1)
            yield
        back(m)
        if m < 3:
            yield


def tbs(ms):
    return ms


def mlp_tile(k, pipe, gT, W1, W2, final=None):
    S = k.S

    def c_norm(wv, bwb):
        norm_transpose(k, k.xres, k.bx, gT, k.hT, k.bhT, 8)
    pipe.add(None, c_norm)
    halves = [(0, 512), (512, 512)]
    for q in range(4):
        for j in range(8):
            def w1(wv, bwb, j=j):
                def evac(ci, hi, ps, bps):
                    rt, brt = k.rtmp[k.rti % 2]; k.rti += 1
                    S.op('dve', lambda e: e.tensor_scalar(rt[:, :], ps, 0.0, None, ALU.max), reads=[bps], writes=[brt])
                    S.op('act', lambda e: e.activation(k.actT[:, 2 * j + ci, hi * 512:(hi + 1) * 512], rt[:, :], AF.Square),
                         reads=[brt], writes=[k.bactT[2 * j + ci]])
                gemm_f(k, wv, bwb, [(0, 128), (128, 128)], k.hT, k.bhT, halves, evac)
            pipe.add(wblock_loader(k, W1, 0, [(q * 2048 + j * 256, 256)]), w1)
        for n in range(8):
            def w2(wv, bwb, n=n):
                def evac_t(m, ps, bps):
                    S.op('dve', lambda e: e.tensor_tensor(k.xres[:, m, n * 256:(n + 1) * 256], ps,
                                                          k.xres[:, m, n * 256:(n + 1) * 256], ALU.add),
                         reads=[bps, k.bx], writes=[k.bx])
                gemm_t(k, wv, bwb, 256, k.actT, k.bactT, range(8), evac_t)
            pipe.add(wblock_loader(k, W2, q * 2048, [(n * 256, 256)]), w2)


def qkv_tile(k, pipe, t, prev=False):
    S = k.S
    W = k.w_qkv

    def c_store(wv, bwb):
        S.barrier()
        if not prev:
            v = k.X1[t * TT:(t + 1) * TT, :].rearrange("(s p) d -> p s d", p=128)
            for s0 in range(0, 8, 4):
                S.dma('sp', v[:, s0:s0 + 4, :], k.xres[:, s0:s0 + 4, :], k.dX1, reads=[k.bx], writes=[k.dX1])
        norm_transpose(k, k.xres, k.bx, k.gmix1, k.hT, k.bhT, 8)
    pipe.add(None, c_store)
    halves = [(0, 512), (512, 512)]
    Vdst = k.Vp if prev else k.V
    for which, dst, dbuf in ((0, k.QT, k.dQT), (1, k.KTp if prev else k.KT, k.dKT)):
        if prev and which == 0:
            continue
        for j in range(8):
            def qk(wv, bwb, j=j, dst=dst, dbuf=dbuf):
                st, bst = k.qstage[k.sti % 2]; k.sti += 1

                def evac(ci, hi, ps, bps):
                    if hi == 0:
                        S.op('act', lambda e: e.copy(st[:, ci, hi * 512:(hi + 1) * 512], ps), reads=[bps], writes=[bst])
                    else:
                        S.op('dve', lambda e: e.tensor_copy(st[:, ci, hi * 512:(hi + 1) * 512], ps), reads=[bps], writes=[bst])
                gemm_f(k, wv, bwb, [(0, 128), (128, 128)], k.hT, k.bhT, halves, evac)
                for ci in range(2):
                    r0 = (2 * j + ci) * 128
                    S.dma('sp', dst[r0:r0 + 128, t * TT:(t + 1) * TT], st[:, ci, :], bst, reads=[bst], writes=[dbuf])
            pipe.add(wblock_loader(k, W, 0, [(which * D + j * 256, 256)]), qk)
    for n in range(8):
        def vp(wv, bwb, n=n):
            st, bst = k.vstage[k.sti % 2]; k.sti += 1

            def evac_t(m, ps, bps):
                if m % 2 == 0:
                    S.op('act', lambda e: e.copy(st[:, m, :], ps), reads=[bps], writes=[bst])
                else:
                    S.op('dve', lambda e: e.tensor_copy(st[:, m, :], ps), reads=[bps], writes=[bst])
            gemm_t(k, wv, bwb, 256, k.hT, k.bhT, range(8), evac_t)
            dv = Vdst[t * TT:(t + 1) * TT, n * 256:(n + 1) * 256].rearrange("(m p) c -> p m c", p=128)
            S.dma('sp', dv, st[:, :, :], bst, reads=[bst], writes=[k.dV])
        pipe.add(wblock_loader(k, W, 0, [(2 * D + n * 256, 256)]), vp)


def exchange(k):
    nc, S = k.nc, k.S
    S.barrier()
    groups = [[2 * i, 2 * i + 1] for i in range(k.ncores // 2)]
    bkv = Buf("kvall")
    S._deps('pool', [], [bkv])
    ins = nc.gpsimd.collective_compute("AllGather", ALU.bypass, groups,
                                       [k.KV.rearrange("a t d -> (a t) d")],
                                       [k.KVall.rearrange("r a t d -> (r a t) d")])
    bkv.dsem = nc.alloc_semaphore(name="d_kvall")
    S.allsems[id(bkv.dsem)] = bkv
    bkv.dcount = 16
    ins.then_inc(bkv.dsem, 16)


def slopes():
    return [2.0 ** (-8.0 * (h + 1) / NHEAD_ATT) for h in range(NHEAD_ATT)]


DILS = (1, 4, 16)


def phaseB(k):
    from contextlib import ExitStack
    nc, S = k.nc, k.S
    cb = k.b_const
    with ExitStack() as es:
        A = lambda name, shape, dt: es.enter_context(nc.sbuf_tensor(name, shape, dt))
        flag = A("flag_sb", [128, 1], F32)
        S.dma('sp', flag[:, :], k.flag[:, :], cb, writes=[cb])
        ones = A("ones_bf", [128, 128], BF16)
        S.op('pool', lambda e: e.memset(ones[:, :], 1.0), writes=[cb])
        Dm = A("Dm", [128, 256], F32)
        S.op('pool', lambda e: e.iota(Dm[:, :], [[1, 256]], base=0, channel_multiplier=-1, allow_small_or_imprecise_dtypes=True), writes=[cb])
        Dc = A("Dc", [128, 256], F32)
        S.op('dve', lambda e: e.tensor_scalar(Dc[:, :], Dm[:, :], 0.0, 128.0, ALU.max, ALU.min), reads=[cb], writes=[cb])
        EB = A("EB", [128, 48, 256], BF16)
        EBf = A("EBf", [128, 48, 128], BF16)
        M01 = A("M01", [128, 256], F32)
        S.op('pool', lambda e: e.memset(M01[:, :], 1.0), writes=[cb])
        S.op('pool', lambda e: e.affine_select(M01[:, :], M01[:, :], [[1, 256]], ALU.is_ge, 0.0, base=0,
                                               channel_multiplier=-1), reads=[cb], writes=[cb])
        S.op('pool', lambda e: e.affine_select(M01[:, :], M01[:, :], [[-1, 256]], ALU.is_ge, 0.0, base=128,
                                               channel_multiplier=1), reads=[cb], writes=[cb])
        M01f = A("M01f", [128, 128], F32)
        S.op('dve', lambda e: e.tensor_scalar(M01f[:, :], M01[:, 128:256], flag[:, 0:1], None, ALU.mult), reads=[cb], writes=[cb])
        ebts = [(A("ebt%d" % i, [128, 256], F32), Buf("ebt%d" % i)) for i in range(2)]
        sl = slopes()
        for h in range(NHEAD_ATT):
            for di, d in enumerate(DILS):
                idx = h * 3 + di
                ebt, bebt = ebts[idx % 2]
                S.op('act', lambda e: e.activation(ebt[:, :], Dc[:, :], AF.Exp, scale=-float(sl[h] * d)),
                     reads=[cb], writes=[bebt])
                S.op('dve', lambda e: e.tensor_tensor(EB[:, idx, :], ebt[:, :], M01[:, :], ALU.mult), reads=[bebt, cb], writes=[cb])
                S.op('pool', lambda e: e.tensor_tensor(EBf[:, idx, :], ebt[:, 128:256], M01f[:, :], ALU.mult),
                     reads=[bebt, cb], writes=[cb])
        HG = 2
        NG = NHEAD_ATT // HG
        LA = 2
        qks = [(A("qTa%d" % i, [128, HG, NOWN], BF16), Buf("qTa%d" % i),
                A("kTa%d" % i, [128, HG, 2 * NOWN], BF16), Buf("kTa%d" % i)) for i in range(2)]
        vts = [(A("vt%d" % i, [128, 32, HG * 128], BF16), Buf("vt%d" % i)) for i in range(3)]
        acc = A("acc", [128, HG, 2, NOWN], F32); bacc = [Buf("acc%d" % i) for i in range(HG)]
        NPB = LA + 2
        ptmp = [(A("ptmp%d" % i, [128, 256], F32), Buf("ptmp%d" % i)) for i in range(NPB)]
        pbf = [(A("pbf%d" % i, [128, 256], BF16), Buf("pbf%d" % i)) for i in range(NPB)]
        ostg = [(A("ostg%d" % i, [128, NOWN], BF16), Buf("ostg%d" % i)) for i in range(2)]
        rz = A("rz", [128, NOWN], F32); brz = Buf("rz")
        dOT = Buf("dOT")
        scale = float(DH ** -0.5)

        def load_qk(g):
            qT, bq, kT, bk = qks[g % 2]
            for hh in range(HG):
                r0 = (g * HG + hh) * 128
                S.dma('sp', qT[:, hh, :], k.QT[r0:r0 + 128, :], bq, writes=[bq])
                S.dma('sp', kT[:, hh, 0:NOWN], k.KTp[r0:r0 + 128, :], bk, writes=[bk])
                S.dma('sp', kT[:, hh, NOWN:2 * NOWN], k.KT[r0:r0 + 128, :], bk, writes=[bk])

        def load_v(g, di):
            d = DILS[di]
            vt, bvt = vts[di]
            nbh = 16 // d
            vt4 = vt[:, :, :].rearrange("p (r b) c -> p r b c", r=d)
            for half, src in enumerate((k.Vp, k.V)):
                sv = src[:, g * HG * 128:(g + 1) * HG * 128].rearrange("(b p r) c -> p r b c", p=128, r=d)
                for r in range(d):
                    S.dma('sp', vt4[:, r, half * nbh:(half + 1) * nbh, :], sv[:, r, :, :], bvt, writes=[bvt])

        def st_qk(b):
            qT, bq, kT, bk = qks[b['g'] % 2]
            d, hh, r, qb, nbh = b['d'], b['hh'], b['r'], b['qb'], b['nbh']
            kbq = nbh + qb
            pt, bpt = k.bank()
            b['pt'], b['bpt'] = pt, bpt
            q0 = r + d * 128 * qb
            b['q0'] = q0
            qsl = qT[:, hh, q0:q0 + d * 127 + 1:d]
            for ci, kb in enumerate((kbq, kbq - 1)):
                f0 = r + d * 128 * kb
                S.mm(lambda e: e.matmul(pt[:, ci * 128:(ci + 1) * 128], kT[:, hh, f0:f0 + d * 127 + 1:d], qsl,
                                        start=True, stop=True), reads=[bk, bq], writes=[bpt], sig=(ci == 1))
            pm, bpm = ptmp[b['i'] % NPB]; pb, bpb = pbf[b['i'] % NPB]
            b['pb'], b['bpb'] = pb, bpb
            idx = b['idx']
            S.op('act', lambda e: e.activation(pm[:, :], pt[:, 0:256], AF.Exp, scale=scale), reads=[bpt], writes=[bpm])
            me = 'dve'
            if qb == 0:
                S.op(me, lambda e: e.tensor_tensor(pb[:, 0:128], pm[:, 0:128], EB[:, idx, 0:128], ALU.mult),
                     reads=[bpm, cb], writes=[bpb])
                S.op(me, lambda e: e.tensor_tensor(pb[:, 128:256], pm[:, 128:256], EBf[:, idx, :], ALU.mult),
                     reads=[bpm, cb], writes=[bpb])
            else:
                S.op(me, lambda e: e.tensor_tensor(pb[:, :], pm[:, :], EB[:, idx, :], ALU.mult),
                     reads=[bpm, cb], writes=[bpb])

        def st_pv(b):
            d, hh, r, qb, nbh, di = b['d'], b['hh'], b['r'], b['qb'], b['nbh'], b['di']
            vt, bvt = vts[di]
            pb, bpb = b['pb'], b['bpb']
            kbq = nbh + qb
            po, bpo = k.bank()
            for ci, kb in enumerate((kbq, kbq - 1)):
                tile = r * (2 * nbh) + kb
                S.mm(lambda e: e.matmul(po[:, 0:128], vt[:, tile, hh * 128:(hh + 1) * 128], pb[:, ci * 128:(ci + 1) * 128],
                                        start=(ci == 0), stop=(ci == 1)), reads=[bvt, bpb], writes=[bpo], sig=False)
            for ci in range(2):
                S.mm(lambda e: e.matmul(po[:, 128:256], ones[:, :], pb[:, ci * 128:(ci + 1) * 128],
                                        start=(ci == 0), stop=(ci == 1)), reads=[cb, bpb], writes=[bpo], sig=(ci == 1))
            q0 = b['q0']
            asl = acc[:, hh, :, q0:q0 + d * 127 + 1:d]
            pv = po[:, 0:256].rearrange("p (a t) -> p a t", a=2)
            if di == 0:
                S.op('act', lambda e: e.copy(asl, pv), reads=[bpo], writes=[bacc[hh]])
            else:
                S.op('dve', lambda e: e.tensor_tensor(asl, pv, asl, ALU.add), reads=[bpo, bacc[hh]], writes=[bacc[hh]])

        load_qk(0)
        for di in range(3):
            load_v(0, di)
        bi = 0
        for g in range(NG):
            blocks = []
            marks = {}
            for di, d in enumerate(DILS):
                nbh = 16 // d
                for hh in range(HG):
                    for r in range(d):
                        for qb in range(nbh):
                            blocks.append(dict(g=g, di=di, d=d, hh=hh, r=r, qb=qb, nbh=nbh, i=bi,
                                               idx=(g * HG + hh) * 3 + di))
                            bi += 1
                marks[len(blocks) - 1] = di
            n = len(blocks)
            for i in range(n + LA):
                if i < n:
                    st_qk(blocks[i])
                if i - LA >= 0:
                    st_pv(blocks[i - LA])
                    bd = blocks[i - LA]
                    if bd['di'] == 2 and bd['r'] == 15 and bd['qb'] == bd['nbh'] - 1:
                        hh = bd['hh']
                        h = g * HG + hh
                        og, bog = ostg[h % 2]
                        S.op('act', lambda e: e.activation(rz[:, :], acc[:, hh, 1, :], AF.Ln), reads=[bacc[hh]], writes=[brz])
                        S.op('act', lambda e: e.activation(rz[:, :], rz[:, :], AF.Exp, scale=-1.0), reads=[brz], writes=[brz])
                        S.op('pool', lambda e: e.tensor_tensor(og[:, :], acc[:, hh, 0, :], rz[:, :], ALU.mult),
                             reads=[bacc[hh], brz], writes=[bog])
                        S.dma('sp', k.OT[h * 128:(h + 1) * 128, :], og[:, :], bog, reads=[bog], writes=[dOT])
                    if (i - LA) in marks and g + 1 < NG:
                        di_done = marks[i - LA]
                        if di_done == 0:
                            load_qk(g + 1)
                        load_v(g + 1, di_done)
        S.barrier()
    with ExitStack() as es:
        A = lambda name, shape, dt: es.enter_context(nc.sbuf_tensor(name, shape, dt))
        k.xres = A("xres3", [128, 8, D], F32); k.bx = Buf("xres3")
        k.hT = A("hT3", [128, 16, TT], BF16); k.bhT = Buf("hT3")
        k.actT = A("actT3", [128, 16, TT], BF16); k.bactT = [Buf("actT3_%d" % c) for c in range(16)]
        k.wslots = [(A("wc%d" % i, [128, 4096], BF16), Buf("wc%d" % i)) for i in range(2)]
        k.hb = [(A("hc%d" % i, [128, D], BF16), Buf("hc%d" % i)) for i in range(2)]
        k.stat = A("stat3", [128, 32], F32); k.bstat = Buf("stat3")
        k.rtmp = [(A("rtmq%d" % i, [128, 512], F32), Buf("rtmq%d" % i)) for i in range(2)]
        k.rti = 0
        gfB = A("gfB", [128, D], F32)
        S.dma('sp', gfB[:, :], k.final_g[0:1, :].partition_broadcast(128), cb, writes=[cb])
        k.gmlp1 = load_gT(k, "gmlp1", k.norm_mlp_g1[1:2, :], dst=A("gmlp1", [128, 16], F32)[:, :])
        ost = [(A("ost%d" % i, [128, D], F32), Buf("ost%d" % i)) for i in range(2)]
        dout = Buf("dout")
        pipe = Pipe(k)
        for t in range(NT):
            def c_load(wv, bwb, t=t):
                v = k.X1[t * TT:(t + 1) * TT, :].rearrange("(s p) d -> p s d", p=128)
                for s0 in range(0, 8, 4):
                    S.dma('sp', k.xres[:, s0:s0 + 4, :], v[:, s0:s0 + 4, :], k.bx, writes=[k.bx])
                S.dma('sp', k.actT[:, :, :], k.OT[:, t * TT:(t + 1) * TT].rearrange("(c p) t -> p c t", p=128), k.bactT[0],
                      writes=k.bactT)
            pipe.add(None, c_load)
            for n in range(8):
                def wo(wv, bwb, n=n):
                    def evac_t(m, ps, bps):
                        S.op('dve', lambda e: e.tensor_tensor(k.xres[:, m, n * 256:(n + 1) * 256], ps,
                                                              k.xres[:, m, n * 256:(n + 1) * 256], ALU.add),
                             reads=[bps, k.bx], writes=[k.bx])
                    gemm_t(k, wv, bwb, 256, k.actT, k.bactT, range(8), evac_t)
                pipe.add(wblock_loader(k, k.w_o, 0, [(n * 256, 256)]), wo)
            mlp_tile(k, pipe, k.gmlp1, k.w1_1, k.w2_1)

            def c_final(wv, bwb, t=t):
                for s in range(8):
                    hb, bhb = k.hb[s % 2]
                    S.op('act', lambda e: e.activation(hb[:, :], k.xres[:, s, :], AF.Square, accum_out=k.stat[:, s:s + 1]),
                         reads=[k.bx], writes=[bhb, k.bstat])
                S.op('act', lambda e: e.activation(k.stat[:, 8:16], k.stat[:, 0:8], AF.Ln, scale=1.0 / D, bias=k.epsc[:, 0:1]),
                     reads=[k.bstat, cb], writes=[k.bstat])
                S.op('act', lambda e: e.activation(k.stat[:, 16:24], k.stat[:, 8:16], AF.Exp, scale=-0.5),
                     reads=[k.bstat], writes=[k.bstat])
                for s in range(8):
                    o, bo = ost[s % 2]
                    S.op('dve', lambda e: e.scalar_tensor_tensor(o[:, :], k.xres[:, s, :], k.stat[:, 16 + s:17 + s], gfB[:, :],
                                                                 ALU.mult, ALU.mult), reads=[k.bx, k.bstat, cb], writes=[bo])
                    r0 = t * TT + s * 128
                    S.dma('sp', k.out[r0:r0 + 128, :], o[:, :], bo, reads=[bo], writes=[dout])
            pipe.add(None, c_final)
        pipe.run()
        S.barrier()


FUSED = True
_CACHE = {}


def _get(mode, ncores=8):
    key = (mode, ncores)
    if key not in _CACHE:
        _CACHE[key] = build(mode, ncores)
    return _CACHE[key]


def _maps_A(inp, cores):
    x = inp['x']
    maps = []
    zeros = np.zeros((NOWN, D), np.float32)
    for c in cores:
        b, half = c // 2, c % 2
        maps.append({
            'x_own': np.ascontiguousarray(x[b, half * NOWN:(half + 1) * NOWN]),
            'x_prev': np.ascontiguousarray(x[b, 0:NOWN]) if half == 1 else zeros,
            'norm_mix_g': inp['norm_mix_g'], 'norm_mlp_g': inp['norm_mlp_g'],
            'hyb_w_in': inp['hyb_w_in'][0], 'conv_w': inp['conv_w'][0],
            'gla_w_gate2': inp['gla_w_gate2'][0], 'gla_b_gate': inp['gla_b_gate'],
            'gla_norm_g': inp['gla_norm_g'], 'hyb_w_out': inp['hyb_w_out'][0],
            'attn_w_qkv': inp['attn_w_qkv'][0],
            'mlp_w1_0': inp['mlp_w1'][0], 'mlp_w2_0': inp['mlp_w2'][0],
        })
    return maps


def _maps_B_extra(inp, cores):
    maps = []
    for c in cores:
        half = c % 2
        maps.append({
            'flag': np.full((128, 1), float(half), np.float32),
            'final_norm_g': inp['final_norm_g'].reshape(1, D),
            'attn_w_o': inp['attn_w_o'][0],
            'mlp_w1_1': inp['mlp_w1'][1], 'mlp_w2_1': inp['mlp_w2'][1],
        })
    return maps


def kernel(**inputs):
    inp = {k_: np.ascontiguousarray(np.asarray(v)) for k_, v in inputs.items()}
    B = inp['x'].shape[0]
    ncores = 2 * B
    cores = list(range(ncores))
    if FUSED:
        nc = _get('F', ncores)
        mA = _maps_A(inp, cores)
        mB = _maps_B_extra(inp, cores)
        maps = [dict(a, **b) for a, b in zip(mA, mB)]
        res = run_bass_kernel_spmd(nc, maps, core_ids=cores)
        outs = [np.asarray(r['out']) for r in res.results]
    else:
        ncA = _get('A', ncores)
        resA = run_bass_kernel_spmd(ncA, _maps_A(inp, cores), core_ids=cores).results
        ncB = _get('B', ncores)
        mB = _maps_B_extra(inp, cores)
        zk = None
        for c in cores:
            m = mB[c]
            m['norm_mlp_g'] = inp['norm_mlp_g']
            for nm in ('X1', 'QT', 'KT', 'V'):
                m[nm] = np.asarray(resA[c][nm])
            if c % 2 == 1:
                m['KTp'] = np.asarray(resA[c - 1]['KT'])
                m['Vp'] = np.asarray(resA[c - 1]['V'])
            else:
                if zk is None:
                    zk = np.zeros_like(np.asarray(resA[c]['KT']))
                m['KTp'] = zk
                m['Vp'] = zk
        res = run_bass_kernel_spmd(ncB, mB, core_ids=cores)
        outs = [np.asarray(r['out']) for r in res.results]
    out = np.stack([np.concatenate([outs[2 * b], outs[2 * b + 1]], axis=0) for b in range(B)], axis=0)
    return out.astype(np.float32)
```

```python
import numpy as np
import concourse.bass as bass
import concourse.mybir as mybir
from concourse.bass_utils import run_bass_kernel_spmd

F32 = mybir.dt.float32
BF16 = mybir.dt.bfloat16
AF = mybir.ActivationFunctionType
ALU = mybir.AluOpType
AX = mybir.AxisListType

D = 2048
NOWN = 2048
TT = 1024
NT = NOWN // TT
ST = 512
DFF = 8192
INCOLS = 6160
EPS = 1e-6
NHEAD_ATT = 16
DH = 128


class Buf:
    def __init__(self, name):
        self.name = name
        self.wt = None
        self.rts = []
        self.dsem = None
        self.dcount = 0


class Sched:
    def __init__(self, nc):
        self.nc = nc
        self.eng = {'pe': nc.tensor, 'act': nc.scalar, 'dve': nc.vector, 'pool': nc.gpsimd, 'sp': nc.sync}
        self.sem = {k: nc.alloc_semaphore(name="prog_" + k) for k in self.eng}
        self.cnt = {k: 0 for k in self.eng}
        self.waited = {k: {} for k in self.eng}
        self.nwaits = 0
        self.nops = 0
        self.allsems = {}

    def _deps(self, e, reads, writes):
        need = {}

        def add(t):
            if t is None:
                return
            s, v = t
            k = id(s)
            if k not in need or need[k][1] < v:
                need[k] = (s, v)
        for r in reads:
            add(r.wt)
        for w in writes:
            add(w.wt)
            for t in w.rts:
                add(t)
        wd = self.waited[e]
        for k, (s, v) in need.items():
            if wd.get(k, 0) >= v:
                continue
            if e == 'pe' and s is self.sem['pe']:
                continue
            self.eng[e].wait_ge(s, v)
            wd[k] = v
            self.nwaits += 1

    def _done(self, t, reads, writes):
        for r in reads:
            r.rts = [x for x in r.rts if x[0] is not t[0]] + [t]
        for w in writes:
            w.wt = t
            w.rts = []

    def op(self, e, fn, reads=(), writes=()):
        self._deps(e, reads, writes)
        ins = fn(self.eng[e])
        self.cnt[e] += 1
        ins.then_inc(self.sem[e], 1)
        t = (self.sem[e], self.cnt[e])
        self._done(t, reads, writes)
        self.nops += 1
        return t

    def mm(self, fn, reads=(), writes=(), sig=True):
        self._deps('pe', reads, writes)
        ins = fn(self.eng['pe'])
        self.nops += 1
        if sig:
            self.cnt['pe'] += 1
            ins.then_inc(self.sem['pe'], 1)
        t = (self.sem['pe'], self.cnt['pe'] + (0 if sig else 1))
        self._done(t, reads, writes)
        return t

    def dma(self, q, out_ap, in_ap, semb, reads=(), writes=(), **kw):
        self._deps(q, reads, writes)
        if semb.dsem is None:
            semb.dsem = self.nc.alloc_semaphore(name="d_" + semb.name)
            self.allsems[id(semb.dsem)] = semb
        ins = self.eng[q].dma_start(out=out_ap, in_=in_ap, **kw)
        semb.dcount += 16
        ins.then_inc(semb.dsem, 16)
        t = (semb.dsem, semb.dcount)
        self._done(t, reads, writes)
        self.nops += 1
        return t

    def wait_bufs(self, e, bufs):
        self._deps(e, bufs, bufs)

    def barrier(self, extra_bufs=()):
        for e in self.eng:
            wd = self.waited[e]
            for e2 in self.eng:
                v = self.cnt[e2]
                if v > 0 and wd.get(id(self.sem[e2]), 0) < v:
                    self.eng[e].wait_ge(self.sem[e2], v)
                    wd[id(self.sem[e2])] = v
            for k, b in self.allsems.items():
                if b.dcount > 0 and wd.get(k, 0) < b.dcount:
                    self.eng[e].wait_ge(b.dsem, b.dcount)
                    wd[k] = b.dcount


class K:
    pass


def bc_last(ap, n):
    return ap.unsqueeze(2).broadcast_to([ap.shape[0], ap.shape[1], n])


def build(mode, ncores=8):
    nc = bass.Bass("TRN2", target_bir_lowering=False)
    S = Sched(nc)
    k = K()
    k.nc, k.S, k.mode = nc, S, mode
    k.ncores = ncores
    doA = mode in ('A', 'F')
    doB = mode in ('B', 'F')

    def dram_in(name, shape, dt=F32):
        return nc.dram_tensor(name, shape, dt, kind="ExternalInput").ap()

    def dram_out(name, shape, dt=F32):
        return nc.dram_tensor(name, shape, dt, kind="ExternalOutput").ap()

    def dram_int(name, shape, dt=F32):
        return nc.dram_tensor(name, shape, dt).ap()

    if doA:
        k.x_own = dram_in("x_own", [NOWN, D])
        k.x_prev = dram_in("x_prev", [NOWN, D])
        k.norm_mix_g = dram_in("norm_mix_g", [2, D])
        k.norm_mlp_g = dram_in("norm_mlp_g", [2, D])
        k.w_in = dram_in("hyb_w_in", [D, INCOLS])
        k.conv_w = dram_in("conv_w", [3, 1024])
        k.wg2 = dram_in("gla_w_gate2", [16, 512])
        k.bg = dram_in("gla_b_gate", [1, 512])
        k.gng = dram_in("gla_norm_g", [1, 256])
        k.w_out = dram_in("hyb_w_out", [D, D])
        k.w_qkv = dram_in("attn_w_qkv", [D, 3 * D])
        k.w1_0 = dram_in("mlp_w1_0", [D, DFF])
        k.w2_0 = dram_in("mlp_w2_0", [DFF, D])
    if mode == 'A':
        k.X1 = dram_out("X1", [NOWN, D])
        k.QT = dram_out("QT", [D, NOWN], BF16)
        k.KT = dram_out("KT", [D, NOWN], BF16)
        k.V = dram_out("V", [NOWN, D], BF16)
    elif mode == 'B':
        k.X1 = dram_in("X1", [NOWN, D])
        k.QT = dram_in("QT", [D, NOWN], BF16)
        k.KT = dram_in("KT", [D, NOWN], BF16)
        k.V = dram_in("V", [NOWN, D], BF16)
        k.KTp = dram_in("KTp", [D, NOWN], BF16)
        k.Vp = dram_in("Vp", [NOWN, D], BF16)
    else:
        k.X1 = dram_int("X1", [NOWN, D])
        k.QT = dram_int("QT", [D, NOWN], BF16)
        k.KT = dram_int("KT", [D, NOWN], BF16)
        k.V = dram_int("V", [NOWN, D], BF16)
        k.KTp = dram_int("KTp", [D, NOWN], BF16)
        k.Vp = dram_int("Vp", [NOWN, D], BF16)
    if doB:
        k.flag = dram_in("flag", [128, 1])
        k.norm_mlp_g1 = k.norm_mlp_g if doA else dram_in("norm_mlp_g", [2, D])
        k.final_g = dram_in("final_norm_g", [1, D])
        k.w_o = dram_in("attn_w_o", [D, D])
        k.w1_1 = dram_in("mlp_w1_1", [D, DFF])
        k.w2_1 = dram_in("mlp_w2_1", [DFF, D])
        k.OT = dram_int("OT", [D, NOWN], BF16)
        k.out = dram_out("out", [NOWN, D])

    k.banks = [(nc.alloc_psum_tensor("bank%d" % i, [128, 512], F32), Buf("bank%d" % i)) for i in range(8)]
    k.bank_i = 0

    def bank():
        b = k.banks[k.bank_i % 8]
        k.bank_i += 1
        return b
    k.bank = bank

    k.ident = nc.alloc_sbuf_tensor("ident", [128, 128], BF16)
    k.b_const = Buf("consts")
    cb = k.b_const
    S.op('pool', lambda e: e.memset(k.ident[:, :], 1.0), writes=[cb])
    S.op('pool', lambda e: e.affine_select(k.ident[:, :], k.ident[:, :], [[-1, 128]], ALU.is_equal, 0.0,
                                           base=0, channel_multiplier=1), reads=[cb], writes=[cb])

    k.ident32 = nc.alloc_sbuf_tensor("ident32", [16, 16], F32)
    S.op('pool', lambda e: e.memset(k.ident32[:, :], 1.0), writes=[cb])
    S.op('pool', lambda e: e.affine_select(k.ident32[:, :], k.ident32[:, :], [[-1, 16]], ALU.is_equal, 0.0,
                                           base=0, channel_multiplier=1), reads=[cb], writes=[cb])
    k.gstage = (nc.alloc_sbuf_tensor("gstage", [16, 128], F32), Buf("gstage"))
    k.epsc = nc.alloc_sbuf_tensor("epsc", [128, 2], F32)
    S.op('pool', lambda e: e.memset(k.epsc[:, 0:1], EPS), writes=[cb])
    S.op('pool', lambda e: e.memset(k.epsc[:, 1:2], 1.0), writes=[cb])
    if doA:
        phaseA(k)
    if doB:
        S.barrier()
        phaseB(k)
    S.barrier()
    print("built mode", mode, "ops", S.nops, "waits", S.nwaits)
    return nc


def load_gT(k, name, src_row_ap, nchunk=16, dst=None):
    nc, S = k.nc, k.S
    b = k.b_const
    if dst is None:
        dst = nc.alloc_sbuf_tensor(name, [128, nchunk], F32)[:, :]
    st, bst = k.gstage
    S.dma('sp', st[0:nchunk, :], src_row_ap.rearrange("o (c p) -> (o c) p", p=128), bst, writes=[bst])
    pt, bpt = k.bank()
    S.mm(lambda e: e.matmul(pt[:, 0:nchunk], st[0:nchunk, :], k.ident32[0:nchunk, 0:nchunk], start=True, stop=True),
         reads=[bst, b], writes=[bpt])
    S.op('dve', lambda e: e.tensor_copy(dst, pt[:, 0:nchunk]), reads=[bpt], writes=[b])
    return dst


def norm_transpose(k, xres, bx, gT, hT, bhT, nsub):
    nc, S = k.nc, k.S
    for s in range(nsub):
        hb, bhb = k.hb[s % 2]
        S.op('act', lambda e: e.activation(hb[:, :], xres[:, s, :], AF.Square, accum_out=k.stat[:, s:s + 1]),
             reads=[bx[s]], writes=[bhb, k.bstat])
    S.op('act', lambda e: e.activation(k.stat[:, 8:8 + nsub], k.stat[:, 0:nsub], AF.Ln, scale=1.0 / D, bias=k.epsc[:, 0:1]),
         reads=[k.bstat, k.b_const], writes=[k.bstat])
    S.op('act', lambda e: e.activation(k.stat[:, 16:16 + nsub], k.stat[:, 8:8 + nsub], AF.Exp, scale=-0.5),
         reads=[k.bstat], writes=[k.bstat])
    for s in range(nsub):
        hb, bhb = k.hb[s % 2]
        rstd = k.stat[:, 16 + s:17 + s]
        if s % 2 == 0:
            S.op('act', lambda e: e.activation(hb[:, :], xres[:, s, :], AF.Copy, scale=rstd),
                 reads=[bx[s], k.bstat], writes=[bhb])
        else:
            S.op('dve', lambda e: e.tensor_scalar(hb[:, :], xres[:, s, :], rstd, None, ALU.mult),
                 reads=[bx[s], k.bstat], writes=[bhb])
        for cg in range(2):
            pt, bpt = k.bank()
            pv = pt[:, :].bitcast(BF16)
            for c in range(8):
                cc = cg * 8 + c
                S.mm(lambda e: e.transpose(pv[:, c * 128:(c + 1) * 128], hb[:, cc * 128:(cc + 1) * 128], k.ident[:, :]),
                     reads=[bhb, k.b_const], writes=[bpt], sig=(c == 7))
            pv3 = pv.rearrange("p (c t) -> p c t", c=8)
            S.op('dve', lambda e: e.tensor_tensor(hT[:, cg * 8:(cg + 1) * 8, s * 128:(s + 1) * 128], pv3,
                                                  bc_last(gT[:, cg * 8:(cg + 1) * 8], 128), ALU.mult),
                 reads=[bpt, k.b_const], writes=[bhT])


def wblock_loader(k, W, r0, segs, nk=16):
    S = k.S
    ncols = sum(n for _, n in segs)

    def load(slot):
        wb, bwb = slot
        v = wb[:, 0:nk * ncols].rearrange("p (k c) -> p k c", k=nk)
        off = 0
        for (c0, n) in segs:
            src = W[r0:r0 + nk * 128, c0:c0 + n].rearrange("(kc p) c -> p kc c", p=128)
            S.dma('pool', v[:, :, off:off + n], src, bwb, writes=[bwb])
            off += n
        return v
    return load


def gemm_f(k, wv, bwb, chunks, rhsT, brhs, halves, evac):
    S = k.S
    nk = wv.shape[1]
    for ci, (off, width) in enumerate(chunks):
        for hi, (t0, n) in enumerate(halves):
            pt, bpt = k.bank()
            for kk in range(nk):
                S.mm(lambda e: e.matmul(pt[0:width, 0:n], wv[:, kk, off:off + width], rhsT[:, kk, t0:t0 + n],
                                        start=(kk == 0), stop=(kk == nk - 1)),
                     reads=[bwb, brhs], writes=[bpt], sig=(kk == nk - 1))
                if kk == nk // 2 - 1:
                    bg_tick(k)
            evac(ci, hi, pt[0:width, 0:n], bpt)
            bg_tick(k)


def gemm_t(k, wv, bwb, ncols, lhsT, blhs, subtiles, evac, t_base=0):
    S = k.S
    nk = wv.shape[1]
    for m in subtiles:
        pt, bpt = k.bank()
        t0 = t_base + m * 128
        for kk in range(nk):
            S.mm(lambda e: e.matmul(pt[:, 0:ncols], lhsT[:, kk, t0:t0 + 128], wv[:, kk, 0:ncols],
                                    start=(kk == 0), stop=(kk == nk - 1)),
                 reads=[bwb, blhs[kk] if isinstance(blhs, list) else blhs], writes=[bpt], sig=(kk == nk - 1))
        evac(m, pt[:, 0:ncols], bpt)


def bg_tick(k):
    g = getattr(k, 'bgen', None)
    if g is not None:
        try:
            next(g)
        except StopIteration:
            k.bgen = None


def bg_drain(k):
    while getattr(k, 'bgen', None) is not None:
        bg_tick(k)


class Pipe:
    def __init__(self, k):
        self.k = k
        self.steps = []

    def add(self, loader, compute):
        self.steps.append((loader, compute))

    def run(self):
        k = self.k
        slots = k.wslots
        ns = len(slots)
        views = {}
        li = 0
        wi = 0
        import os
        sk = os.environ.get("KSKIP")
        if sk:
            rngs = [[int(v) for v in r.split(":")] for r in sk.split(",")]
            self.steps = [st for i, st in enumerate(self.steps) if not any(a <= i < b for a, b in rngs)]
        order = [i for i, (l, c) in enumerate(self.steps) if l is not None]
        slot_of = {i: slots[j % ns] for j, i in enumerate(order)}
        pos = 0

        def issue_next():
            nonlocal pos
            if pos < len(order):
                i = order[pos]
                views[i] = self.steps[i][0](slot_of[i])
                pos += 1
        for _ in range(ns - 1):
            issue_next()
        import os
        lim = int(os.environ.get("KSTEPS", "1000000"))
        for i, (l, c) in enumerate(self.steps):
            if i >= lim:
                break
            if l is not None:
                while i not in views:
                    issue_next()
                c(views[i], slot_of[i][1])
                issue_next()
                del views[i]
            else:
                c(None, None)
        self.steps = []


def phaseA(k):
    from contextlib import ExitStack
    with ExitStack() as es:
        _phaseA(k, es)
        k.S.barrier()


def _phaseA(k, es):
    nc, S = k.nc, k.S
    cb = k.b_const
    A = lambda name, shape, dt: es.enter_context(nc.sbuf_tensor(name, shape, dt))
    k.xres = A("xres", [128, 8, D], F32); k.bx = [Buf("xres%d" % i) for i in range(8)]; k.bxd = [Buf("xres_dma0"), Buf("xres_dma1")]
    k.hT = A("hT", [128, 16, TT], BF16); k.bhT = Buf("hT")
    k.actT = A("actT", [128, 16, TT], BF16); k.bactT = [Buf("actT%d" % c) for c in range(16)]
    k.wslots = [(A("wb%d" % i, [128, 4096], BF16), Buf("wb%d" % i)) for i in range(2)]
    k.hb = [(A("hb%d" % i, [128, D], BF16), Buf("hb%d" % i)) for i in range(2)]
    k.stat = A("stat", [128, 32], F32); k.bstat = Buf("stat")
    k.gmix0 = load_gT(k, "gmix0", k.norm_mix_g[0:1, :], dst=A("gmix0", [128, 16], F32)[:, :])
    k.gmix1 = load_gT(k, "gmix1", k.norm_mix_g[1:2, :], dst=A("gmix1", [128, 16], F32)[:, :])
    k.gmlp0 = load_gT(k, "gmlp0", k.norm_mlp_g[0:1, :], dst=A("gmlp0", [128, 16], F32)[:, :])
    k.convw = A("convw", [128, 8, 3], F32)
    for kk in range(3):
        load_gT(k, None, k.conv_w[kk:kk + 1, :], nchunk=8, dst=k.convw[:, :, kk])
    k.wg2e = A("wg2e", [32, 512], BF16)
    k.b_wg2 = Buf("wg2e")
    S.dma('pool', k.wg2e[0:16, :], k.wg2[:, :], k.b_wg2, writes=[k.b_wg2])
    S.dma('pool', k.wg2e[16:17, :], k.bg[:, :], k.b_wg2, writes=[k.b_wg2])
    k.gngB = A("gngB", [128, 256], F32)
    S.dma('sp', k.gngB[:, :], k.gng[0:1, :].partition_broadcast(128), cb, writes=[cb])
    k.triG = A("triG", [128, 128], BF16)
    k.triD = A("triD", [128, 128], BF16)
    k.cmask = A("cmask", [128, 128], F32)
    S.op('pool', lambda e: e.memset(k.triG[:, :], -1.0 / 16), writes=[cb])
    S.op('pool', lambda e: e.affine_select(k.triG[:, :], k.triG[:, :], [[1, 128]], ALU.is_ge, 0.0, base=0,
                                           channel_multiplier=-1), reads=[cb], writes=[cb])
    S.op('pool', lambda e: e.memset(k.triD[:, :], -1.0 / 16), writes=[cb])
    S.op('pool', lambda e: e.affine_select(k.triD[:, :], k.triD[:, :], [[-1, 128]], ALU.is_gt, 0.0, base=0,
                                           channel_multiplier=1), reads=[cb], writes=[cb])
    S.op('pool', lambda e: e.memset(k.cmask[:, :], 1.0), writes=[cb])
    S.op('pool', lambda e: e.affine_select(k.cmask[:, :], k.cmask[:, :], [[1, 128]], ALU.is_ge, 0.0, base=0,
                                           channel_multiplier=-1), reads=[cb], writes=[cb])
    k.arena = A("arena", [128, 8192], BF16)
    ar = k.arena
    k.convst = ar[:, 0:4096].rearrange("p (c t) -> p c t", c=8); k.bconvst = [Buf("convst%d" % c) for c in range(8)]
    k.U = ar[:, 4096:5632].bitcast(F32)[:, 0:ST + 2]; k.bU = Buf("U")
    k.actmp = ar[:, 5632:6656].bitcast(F32); k.bactmp = Buf("actmp")
    k.t1 = k.actmp; k.bt1 = k.bactmp
    k.halo = A("halo", [128, 8, 2], F32); k.bhalo = Buf("halo")
    k.glowT = A("glowT", [32, ST], BF16); k.bglow = Buf("glowT")
    k.sp_tok = A("sp_tok", [128, 4, 512], BF16); k.bsp = Buf("sp_tok")
    k.qT = A("qT", [128, ST], BF16); k.bqT = Buf("qT")
    k.kT = A("kT", [128, ST], BF16); k.bkT = Buf("kT")
    k.k_tok = A("k_tok", [128, 4, 128], BF16); k.bktok = Buf("k_tok")
    k.v_tok = A("v_tok", [128, 4, 256], BF16); k.bvtok = Buf("v_tok")
    k.rg = A("rg", [128, 4, 256], F32); k.brg = Buf("rg")
    k.Sst = A("Sst", [128, 4, 256], F32); k.bSst = [Buf("Sst%d" % h) for h in range(4)]
    k.Sbf = A("Sbf", [128, 4, 256], BF16); k.bSbf = [Buf("Sbf%d" % h) for h in range(4)]
    k.ch = []
    for i in range(2):
        d = {}
        for nm, shp, dt in [("Eg", [128, 128], F32), ("Eng", [128, 128], F32), ("Dd", [128, 128], F32),
                            ("qg", [128, 128], BF16), ("kg", [128, 128], BF16), ("kd", [128, 128], BF16),
                            ("sm", [128, 128], BF16), ("of", [128, 256], BF16), ("osq", [128, 256], BF16),
                            ("cst", [128, 4], F32)]:
            d[nm] = (A("%s%d" % (nm, i), shp, dt), Buf("%s%d" % (nm, i)))
        k.ch.append(d)
    k.chi = 0
    k.rtmp = [(A("rtmp%d" % i, [128, 512], F32), Buf("rtmp%d" % i)) for i in range(2)]
    k.rti = 0
    k.sptmp, k.bsptmp = k.rtmp[0]
    k.qstage = [(ar[:, i * 2048:(i + 1) * 2048].rearrange("p (c t) -> p c t", c=2), Buf("qstage%d" % i)) for i in range(2)]
    k.vstage = [(ar[:, 4096 + i * 2048:4096 + (i + 1) * 2048].rearrange("p (m c) -> p m c", m=8), Buf("vstage%d" % i)) for i in range(2)]
    k.sti = 0
    k.dX1 = Buf("dX1"); k.dX1s = [Buf("dX1a"), Buf("dX1b")]; k.dQT = Buf("dQT"); k.dKT = Buf("dKT"); k.dV = Buf("dV")

    import os
    if os.environ.get("KDEBUG_INIT"):
        for c in range(16):
            S.op('dve', lambda e: e.memset(k.actT[:, c, :], 0.0), writes=[k.bactT[c]])
    for h in range(4):
        S.op('dve', lambda e: e.memset(k.Sst[:, h, :], 0.0), writes=[k.bSst[h]])
        S.op('dve', lambda e: e.memset(k.Sbf[:, h, :], 0.0), writes=[k.bSbf[h]])
    S.op('dve', lambda e: e.memset(k.halo[:, :, :], 0.0), writes=[k.bhalo])
    S.op('dve', lambda e: e.memset(k.glowT[:, :], 1.0), writes=[k.bglow])

    pipe = Pipe(k)
    if k.mode == 'F':
        seq = [(k.x_prev, t, True) for t in range(NT)] + [(k.x_own, t, False) for t in range(NT)]
        for i, (src, t, prev) in enumerate(seq):
            mixer_tile(k, pipe, src, t, state_only=False, last=False, preloaded=(i > 0))
            mlp_tile(k, pipe, k.gmlp0, k.w1_0, k.w2_0)
            nxt = seq[i + 1][:2] if i + 1 < len(seq) else None
            qkv_tile(k, pipe, t, prev=prev, next_x=nxt)
        pipe.run()
        return
    else:
        for t in range(NT):
            mixer_tile(k, pipe, k.x_prev, t, state_only=True, last=(t == NT - 1))
    for t in range(NT):
        mixer_tile(k, pipe, k.x_own, t, state_only=False, last=False)
        mlp_tile(k, pipe, k.gmlp0, k.w1_0, k.w2_0)
        qkv_tile(k, pipe, t)
    pipe.run()


def load_x_tile(k, src, t):
    S = k.S
    v = src[t * TT:(t + 1) * TT, :].rearrange("(s p) d -> p s d", p=128)
    for s0 in range(0, 8, 4):
        S.dma('sp', k.xres[:, s0:s0 + 4, :], v[:, s0:s0 + 4, :], k.bxd[s0 // 4], writes=k.bx[s0:s0 + 4])


def mixer_tile(k, pipe, xsrc, t, state_only, last, preloaded=False):
    nc, S = k.nc, k.S
    cb = k.b_const
    W = k.w_in

    def c_load(wv, bwb):
        if not preloaded:
            load_x_tile(k, xsrc, t)
        norm_transpose(k, k.xres, k.bx, k.gmix0, k.hT, k.bhT, 8)
    pipe.add(None, c_load)

    def c_bar(wv, bwb):
        S.barrier()
    pipe.add(None, c_bar)
    for sub in range(TT // ST):
        mixer_sub(k, pipe, sub, state_only, last)

    if not state_only:
        for n in range(8):
            def wout(wv, bwb, n=n):
                def evac_t(m, ps, bps):
                    S.op('dve', lambda e: e.tensor_tensor(k.xres[:, m, n * 256:(n + 1) * 256], ps,
                                                          k.xres[:, m, n * 256:(n + 1) * 256], ALU.add),
                         reads=[bps, k.bx[m]], writes=[k.bx[m]])
                gemm_t(k, wv, bwb, 256, k.actT, k.bactT, range(8), evac_t)
            pipe.add(wblock_loader(k, k.w_out, 0, [(n * 256, 256)]), wout)


def mixer_sub(k, pipe, sub, state_only, last):
    nc, S = k.nc, k.S
    cb = k.b_const
    W = k.w_in
    tb = sub * ST
    hTs = k.hT
    halves = [(tb, ST)]

    conv_steps = []
    if not state_only:
        def mk_conv_c(c):
            def conv_c(wv, bwb):
                def evac(ci, hi, ps, bps):
                    if ci == 0:
                        S.op('act', lambda e: e.copy(k.actmp[:, :], ps), reads=[bps], writes=[k.bactmp])
                    else:
                        S.op('dve', lambda e: e.tensor_copy(k.U[:, 0:2], k.halo[:, c, :]), reads=[k.bhalo], writes=[k.bU])
                        S.op('dve', lambda e: e.tensor_tensor(k.U[:, 2:2 + ST], ps, k.actmp[:, :], ALU.mult),
                             reads=[bps, k.bactmp], writes=[k.bU])
                        S.op('dve', lambda e: e.tensor_copy(k.halo[:, c, :], k.U[:, ST:ST + 2]), reads=[k.bU], writes=[k.bhalo])
                        S.op('act', lambda e: e.activation(k.t1[:, :], k.U[:, 2:2 + ST], AF.Copy, scale=k.convw[:, c, 2:3]),
                             reads=[k.bU, cb], writes=[k.bt1])
                        S.op('dve', lambda e: e.scalar_tensor_tensor(k.t1[:, :], k.U[:, 1:1 + ST], k.convw[:, c, 1:2], k.t1[:, :],
                                                                     ALU.mult, ALU.add),
                             reads=[k.bU, k.bt1, cb], writes=[k.bt1])
                        S.op('dve', lambda e: e.scalar_tensor_tensor(k.convst[:, c, :], k.U[:, 0:ST], k.convw[:, c, 0:1], k.t1[:, :],
                                                                     ALU.mult, ALU.add),
                             reads=[k.bU, k.bt1, cb], writes=[k.bconvst[c]])
                gemm_f(k, wv, bwb, [(0, 128), (128, 128)], hTs, k.bhT, halves, evac)
            return (wblock_loader(k, W, 0, [(1024 + c * 128, 128), (2048 + c * 128, 128)]), conv_c)

        def mk_convb(cp):
            def convb(wv, bwb):
                def evac(ci, hi, ps, bps):
                    c = 2 * cp + ci
                    S.op('dve', lambda e: e.tensor_tensor(k.actT[:, c, tb:tb + ST], ps, k.convst[:, c, :], ALU.mult),
                         reads=[bps, k.bconvst[c]], writes=[k.bactT[c]])
                gemm_f(k, wv, bwb, [(0, 128), (128, 128)], hTs, k.bhT, halves, evac)
                bg_drain(k)
            return (wblock_loader(k, W, 0, [(cp * 256, 256)]), convb)
        for cp in range(4):
            conv_steps.append([mk_conv_c(2 * cp), mk_conv_c(2 * cp + 1), mk_convb(cp)])
    elif last and sub == TT // ST - 1:
        for c in range(8):
            def halo_c(wv, bwb, c=c):
                def evac(ci, hi, ps, bps):
                    if ci == 0:
                        S.op('act', lambda e: e.copy(k.actmp[:, 0:2], ps), reads=[bps], writes=[k.bactmp])
                    else:
                        S.op('dve', lambda e: e.tensor_tensor(k.halo[:, c, :], ps, k.actmp[:, 0:2], ALU.mult),
                             reads=[bps, k.bactmp], writes=[k.bhalo])
                gemm_f(k, wv, bwb, [(0, 128), (128, 128)], hTs, k.bhT, [(TT - 2, 2)], evac)
            pipe.add(wblock_loader(k, W, 0, [(1024 + c * 128, 128), (2048 + c * 128, 128)]), halo_c)

    def gate(wv, bwb):
        def evac(ci, hi, ps, bps):
            S.op('act', lambda e: e.copy(k.glowT[0:16, :], ps), reads=[bps], writes=[k.bglow])
        gemm_f(k, wv, bwb, [(0, 16)], hTs, k.bhT, halves, evac)
        for m in range(4):
            pt, bpt = k.bank()
            S.mm(lambda e: e.matmul(pt[:, 0:512], k.glowT[0:17, m * 128:(m + 1) * 128], k.wg2e[0:17, :],
                                    start=True, stop=True), reads=[k.bglow, k.b_wg2], writes=[bpt])
            S.op('act', lambda e: e.activation(k.sptmp[:, :], pt[:, 0:512], AF.Exp, scale=-1.0),
                 reads=[bpt], writes=[k.bsptmp])
            S.op('act', lambda e: e.activation(k.sp_tok[:, m, :], k.sptmp[:, :], AF.Ln, bias=k.epsc[:, 1:2]),
                 reads=[k.bsptmp, cb], writes=[k.bsp])
    pipe.add(wblock_loader(k, W, 0, [(6144, 16)]), gate)

    for h in range(4):
        if not state_only:
            def qk(wv, bwb, h=h):
                def evac(ci, hi, ps, bps):
                    if ci == 0:
                        S.op('act', lambda e: e.activation(k.qT[:, :], ps, AF.Copy, scale=float(128 ** -0.5)),
                             reads=[bps], writes=[k.bqT])
                    else:
                        S.op('act', lambda e: e.copy(k.kT[:, :], ps), reads=[bps], writes=[k.bkT])
                gemm_f(k, wv, bwb, [(0, 128), (128, 128)], hTs, k.bhT, halves, evac)

                def evac_t(m, ps, bps):
                    S.op('dve', lambda e: e.tensor_copy(k.k_tok[:, m, :], ps), reads=[bps], writes=[k.bktok])
                gemm_t(k, wv[:, :, 128:256], bwb, 128, hTs, k.bhT, range(4), evac_t, t_base=tb)
            pipe.add(wblock_loader(k, W, 0, [(3072 + h * 128, 128), (3584 + h * 128, 128)]), qk)
        else:
            def konly(wv, bwb, h=h):
                def evac_t(m, ps, bps):
                    S.op('dve', lambda e: e.tensor_copy(k.k_tok[:, m, :], ps), reads=[bps], writes=[k.bktok])
                gemm_t(k, wv, bwb, 128, hTs, k.bhT, range(4), evac_t, t_base=tb)
            pipe.add(wblock_loader(k, W, 0, [(3584 + h * 128, 128)]), konly)

        def vproj(wv, bwb, h=h):
            def evac_t(m, ps, bps):
                S.op('act', lambda e: e.copy(k.v_tok[:, m, :], ps), reads=[bps], writes=[k.bvtok])
            gemm_t(k, wv, bwb, 256, hTs, k.bhT, range(4), evac_t, t_base=tb)
            if state_only:
                k.bgen = gla_gen(k, h, tb, state_only=True)
                bg_drain(k)
        pipe.add(wblock_loader(k, W, 0, [(4096 + h * 256, 256)]), vproj)

        if not state_only:
            def rproj(wv, bwb, h=h):
                def evac_t(m, ps, bps):
                    rt, brt = k.rtmp[k.rti % 2]; k.rti += 1
                    S.op('act', lambda e: e.activation(rt[:, 0:256], ps, AF.Silu), reads=[bps], writes=[brt])
                    S.op('dve', lambda e: e.tensor_tensor(k.rg[:, m, :], rt[:, 0:256], k.gngB[:, :], ALU.mult),
                         reads=[brt, cb], writes=[k.brg])
                gemm_t(k, wv, bwb, 256, hTs, k.bhT, range(4), evac_t, t_base=tb)
                bg_drain(k)
                k.bgen = gla_gen(k, h, tb, state_only=False)
            pipe.add(wblock_loader(k, W, 0, [(5120 + h * 256, 256)]), rproj)
            for st in conv_steps[h]:
                pipe.add(*st)


def gla_gen(k, h, tb, state_only):
    nc, S = k.nc, k.S
    cb = k.b_const
    hs = slice(h * 128, (h + 1) * 128)
    Ts = {}

    def fa(m):
        T = k.ch[k.chi % 2]; k.chi += 1
        Ts[m] = T
        Eg, bEg = T["Eg"]; Eng, bEng = T["Eng"]; Dd, bDd = T["Dd"]
        qg, bqg = T["qg"]; kg, bkg = T["kg"]; kd, bkd = T["kd"]
        cst, bcst = T["cst"]
        ms = slice(m * 128, (m + 1) * 128)
        p2, bp2 = k.bank()
        S.mm(lambda e: e.matmul(p2[:, 0:128], k.triD[:, :], k.sp_tok[:, m, hs], start=True, stop=True),
             reads=[cb, k.bsp], writes=[bp2])
        if not state_only:
            p1, bp1 = k.bank()
            S.mm(lambda e: e.matmul(p1[:, 0:128], k.sp_tok[:, m, hs], k.triG[:, :], start=True, stop=True),
                 reads=[cb, k.bsp], writes=[bp1])
            S.op('act', lambda e: e.activation(Eg[:, :], p1[:, 0:128], AF.Exp), reads=[bp1], writes=[bEg])
            S.op('act', lambda e: e.activation(Eng[:, :], p1[:, 0:128], AF.Exp, scale=-1.0), reads=[bp1], writes=[bEng])
            S.op('dve', lambda e: e.tensor_tensor(qg[:, :], k.qT[:, ms], Eg[:, :], ALU.mult),
                 reads=[k.bqT, bEg], writes=[bqg])
            S.op('dve', lambda e: e.tensor_tensor(kg[:, :], k.kT[:, ms], Eng[:, :], ALU.mult),
                 reads=[k.bkT, bEng], writes=[bkg])
        else:
            p1, bp1 = k.bank()
            S.mm(lambda e: e.matmul(p1[:, 0:1], k.sp_tok[:, m, hs], k.triG[:, 127:128], start=True, stop=True),
                 reads=[cb, k.bsp], writes=[bp1])
            S.op('act', lambda e: e.activation(cst[:, 2:3], p1[:, 0:1], AF.Exp), reads=[bp1], writes=[bcst])
        S.op('act', lambda e: e.activation(Dd[:, :], p2[:, 0:128], AF.Exp), reads=[bp2], writes=[bDd])
        S.op('dve', lambda e: e.tensor_tensor(kd[:, :], k.k_tok[:, m, :], Dd[:, :], ALU.mult),
             reads=[k.bktok, bDd], writes=[bkd])

    def fb(m):
        if state_only:
            return
        T = Ts[m]
        qg, bqg = T["qg"]; kg, bkg = T["kg"]; sm, bsm = T["sm"]
        p3, bp3 = k.bank()
        S.mm(lambda e: e.matmul(p3[:, 0:128], kg[:, :], qg[:, :], start=True, stop=True),
             reads=[bkg, bqg], writes=[bp3])
        S.op('dve', lambda e: e.tensor_tensor(sm[:, :], p3[:, 0:128], k.cmask[:, :], ALU.mult),
             reads=[bp3, cb], writes=[bsm])

    def ba(m):
        T = Ts[m]
        Eg, bEg = T["Eg"]
        qg, bqg = T["qg"]; kd, bkd = T["kd"]
        sm, bsm = T["sm"]; of, bof = T["of"]; osq, bosq = T["osq"]; cst, bcst = T["cst"]
        if not state_only:
            egl, begl = Eg[:, 127:128], bEg
            p4, bp4 = k.bank()
            S.mm(lambda e: e.matmul(p4[:, 0:256], sm[:, :], k.v_tok[:, m, :], start=True, stop=False),
                 reads=[bsm, k.bvtok], writes=[bp4], sig=False)
            S.mm(lambda e: e.matmul(p4[:, 0:256], qg[:, :], k.Sbf[:, h, :], start=False, stop=True),
                 reads=[bqg, k.bSbf[h]], writes=[bp4])
        else:
            egl, begl = cst[:, 2:3], bcst
        p6, bp6 = k.bank()
        S.mm(lambda e: e.matmul(p6[:, 0:256], kd[:, :], k.v_tok[:, m, :], start=True, stop=True),
             reads=[bkd, k.bvtok], writes=[bp6])
        S.op('dve', lambda e: e.scalar_tensor_tensor(k.Sst[:, h, :], k.Sst[:, h, :], egl, p6[:, 0:256], ALU.mult, ALU.add),
             reads=[k.bSst[h], begl, bp6], writes=[k.bSst[h]])
        S.op('act', lambda e: e.copy(k.Sbf[:, h, :], k.Sst[:, h, :]), reads=[k.bSst[h]], writes=[k.bSbf[h]])
        if not state_only:
            S.op('act', lambda e: e.activation(osq[:, :], p4[:, 0:256], AF.Square, accum_out=cst[:, 0:1]),
                 reads=[bp4], writes=[bosq, bcst])
            S.op('act', lambda e: e.activation(cst[:, 3:4], cst[:, 0:1], AF.Ln, scale=1.0 / 256, bias=k.epsc[:, 0:1]),
                 reads=[bcst, cb], writes=[bcst])
            S.op('act', lambda e: e.activation(cst[:, 1:2], cst[:, 3:4], AF.Exp, scale=-0.5),
                 reads=[bcst], writes=[bcst])
            S.op('dve', lambda e: e.scalar_tensor_tensor(of[:, :], p4[:, 0:256], cst[:, 1:2], k.rg[:, m, :], ALU.mult, ALU.mult),
                 reads=[bp4, bcst, k.brg], writes=[bof])

    def bb(m):
        if state_only:
            return
        T = Ts[m]
        of, bof = T["of"]
        p5, bp5 = k.bank()
        pv = p5[:, :].bitcast(BF16)
        for j in range(2):
            S.mm(lambda e: e.transpose(pv[:, j * 128:(j + 1) * 128], of[:, j * 128:(j + 1) * 128], k.ident[:, :]),
                 reads=[bof, cb], writes=[bp5], sig=(j == 1))
        S.op('act', lambda e: e.copy(k.actT[:, 8 + 2 * h:10 + 2 * h, tb + m * 128:tb + (m + 1) * 128],
                                     pv[:, 0:256].rearrange("p (c t) -> p c t", c=2)),
             reads=[bp5], writes=[k.bactT[8 + 2 * h], k.bactT[9 + 2 * h]])

    order = [(fa, 0), (fa, 1), (fb, 0), (fb, 1), (ba, 0), (fa, 2), (bb, 0), (ba, 1), (fb, 2), (bb, 1),
             (fa, 3), (ba, 2), (fb, 3), (bb, 2), (ba, 3), (bb, 3)]
    for i, (fn, m) in enumerate(order):
        fn(m)
        if i + 1 < len(order) and not state_only:
            yield
    if state_only:
        yield


def tbs(ms):
    return ms


def mlp_tile(k, pipe, gT, W1, W2, final=None):
    S = k.S

    def c_norm(wv, bwb):
        norm_transpose(k, k.xres, k.bx, gT, k.hT, k.bhT, 8)
    pipe.add(None, c_norm)
    halves = [(0, 512), (512, 512)]
    for q in range(4):
        for j in range(8):
            def w1(wv, bwb, j=j):
                def evac(ci, hi, ps, bps):
                    rt, brt = k.rtmp[k.rti % 2]; k.rti += 1
                    S.op('dve', lambda e: e.tensor_scalar(rt[:, :], ps, 0.0, None, ALU.max), reads=[bps], writes=[brt])
                    S.op('act', lambda e: e.activation(k.actT[:, 2 * j + ci, hi * 512:(hi + 1) * 512], rt[:, :], AF.Square),
                         reads=[brt], writes=[k.bactT[2 * j + ci]])
                gemm_f(k, wv, bwb, [(0, 128), (128, 128)], k.hT, k.bhT, halves, evac)
            pipe.add(wblock_loader(k, W1, 0, [(q * 2048 + j * 256, 256)]), w1)
        for n in range(8):
            def w2(wv, bwb, n=n):
                def evac_t(m, ps, bps):
                    S.op('dve', lambda e: e.tensor_tensor(k.xres[:, m, n * 256:(n + 1) * 256], ps,
                                                          k.xres[:, m, n * 256:(n + 1) * 256], ALU.add),
                         reads=[bps, k.bx[m]], writes=[k.bx[m]])
                gemm_t(k, wv, bwb, 256, k.actT, k.bactT, range(8), evac_t)
            pipe.add(wblock_loader(k, W2, q * 2048, [(n * 256, 256)]), w2)


def qkv_tile(k, pipe, t, prev=False, next_x=None):
    S = k.S
    W = k.w_qkv

    def c_store(wv, bwb):
        S.barrier()
        if not prev:
            v = k.X1[t * TT:(t + 1) * TT, :].rearrange("(s p) d -> p s d", p=128)
            for s0 in range(0, 8, 4):
                S.dma('sp', v[:, s0:s0 + 4, :], k.xres[:, s0:s0 + 4, :], k.dX1s[s0 // 4], reads=k.bx[s0:s0 + 4], writes=[k.dX1])
        norm_transpose(k, k.xres, k.bx, k.gmix1, k.hT, k.bhT, 8)
        if next_x is not None:
            load_x_tile(k, next_x[0], next_x[1])
    pipe.add(None, c_store)
    halves = [(0, 512), (512, 512)]
    Vdst = k.Vp if prev else k.V
    for which, dst, dbuf in ((0, k.QT, k.dQT), (1, k.KTp if prev else k.KT, k.dKT)):
        if prev and which == 0:
            continue
        for j in range(8):
            def qk(wv, bwb, j=j, dst=dst, dbuf=dbuf):
                st, bst = k.qstage[k.sti % 2]; k.sti += 1

                def evac(ci, hi, ps, bps):
                    if hi == 0:
                        S.op('act', lambda e: e.copy(st[:, ci, hi * 512:(hi + 1) * 512], ps), reads=[bps], writes=[bst])
                    else:
                        S.op('dve', lambda e: e.tensor_copy(st[:, ci, hi * 512:(hi + 1) * 512], ps), reads=[bps], writes=[bst])
                gemm_f(k, wv, bwb, [(0, 128), (128, 128)], k.hT, k.bhT, halves, evac)
                for ci in range(2):
                    r0 = (2 * j + ci) * 128
                    S.dma('sp', dst[r0:r0 + 128, t * TT:(t + 1) * TT], st[:, ci, :], bst, reads=[bst], writes=[dbuf])
            pipe.add(wblock_loader(k, W, 0, [(which * D + j * 256, 256)]), qk)
    for n in range(8):
        def vp(wv, bwb, n=n):
            st, bst = k.vstage[k.sti % 2]; k.sti += 1

            def evac_t(m, ps, bps):
                if m % 2 == 0:
                    S.op('act', lambda e: e.copy(st[:, m, :], ps), reads=[bps], writes=[bst])
                else:
                    S.op('dve', lambda e: e.tensor_copy(st[:, m, :], ps), reads=[bps], writes=[bst])
            gemm_t(k, wv, bwb, 256, k.hT, k.bhT, range(8), evac_t)
            dv = Vdst[t * TT:(t + 1) * TT, n * 256:(n + 1) * 256].rearrange("(m p) c -> p m c", p=128)
            S.dma('sp', dv, st[:, :, :], bst, reads=[bst], writes=[k.dV])
        pipe.add(wblock_loader(k, W, 0, [(2 * D + n * 256, 256)]), vp)


def exchange(k):
    nc, S = k.nc, k.S
    S.barrier()
    groups = [[2 * i, 2 * i + 1] for i in range(k.ncores // 2)]
    bkv = Buf("kvall")
    S._deps('pool', [], [bkv])
    ins = nc.gpsimd.collective_compute("AllGather", ALU.bypass, groups,
                                       [k.KV.rearrange("a t d -> (a t) d")],
                                       [k.KVall.rearrange("r a t d -> (r a t) d")])
    bkv.dsem = nc.alloc_semaphore(name="d_kvall")
    S.allsems[id(bkv.dsem)] = bkv
    bkv.dcount = 16
    ins.then_inc(bkv.dsem, 16)


def slopes():
    return [2.0 ** (-8.0 * (h + 1) / NHEAD_ATT) for h in range(NHEAD_ATT)]


DILS = (1, 4, 16)


def phaseB(k):
    from contextlib import ExitStack
    nc, S = k.nc, k.S
    cb = k.b_const
    with ExitStack() as es:
        A = lambda name, shape, dt: es.enter_context(nc.sbuf_tensor(name, shape, dt))
        flag = A("flag_sb", [128, 1], F32)
        S.dma('sp', flag[:, :], k.flag[:, :], cb, writes=[cb])
        ones = A("ones_bf", [128, 128], BF16)
        S.op('pool', lambda e: e.memset(ones[:, :], 1.0), writes=[cb])
        Dm = A("Dm", [128, 256], F32)
        S.op('pool', lambda e: e.iota(Dm[:, :], [[1, 256]], base=0, channel_multiplier=-1, allow_small_or_imprecise_dtypes=True), writes=[cb])
        Dc = A("Dc", [128, 256], F32)
        S.op('dve', lambda e: e.tensor_scalar(Dc[:, :], Dm[:, :], 0.0, 128.0, ALU.max, ALU.min), reads=[cb], writes=[cb])
        EB = A("EB", [128, 48, 256], BF16)
        EBf = A("EBf", [128, 48, 128], BF16)
        M01 = A("M01", [128, 256], F32)
        S.op('pool', lambda e: e.memset(M01[:, :], 1.0), writes=[cb])
        S.op('pool', lambda e: e.affine_select(M01[:, :], M01[:, :], [[1, 256]], ALU.is_ge, 0.0, base=0,
                                               channel_multiplier=-1), reads=[cb], writes=[cb])
        S.op('pool', lambda e: e.affine_select(M01[:, :], M01[:, :], [[-1, 256]], ALU.is_ge, 0.0, base=128,
                                               channel_multiplier=1), reads=[cb], writes=[cb])
        M01f = A("M01f", [128, 128], F32)
        S.op('dve', lambda e: e.tensor_scalar(M01f[:, :], M01[:, 128:256], flag[:, 0:1], None, ALU.mult), reads=[cb], writes=[cb])
        ebts = [(A("ebt%d" % i, [128, 256], F32), Buf("ebt%d" % i)) for i in range(2)]
        sl = slopes()
        for h in range(NHEAD_ATT):
            for di, d in enumerate(DILS):
                idx = h * 3 + di
                ebt, bebt = ebts[idx % 2]
                S.op('act', lambda e: e.activation(ebt[:, :], Dc[:, :], AF.Exp, scale=-float(sl[h] * d)),
                     reads=[cb], writes=[bebt])
                S.op('dve', lambda e: e.tensor_tensor(EB[:, idx, :], ebt[:, :], M01[:, :], ALU.mult), reads=[bebt, cb], writes=[cb])
                S.op('pool', lambda e: e.tensor_tensor(EBf[:, idx, :], ebt[:, 128:256], M01f[:, :], ALU.mult),
                     reads=[bebt, cb], writes=[cb])
        HG = 2
        NG = NHEAD_ATT // HG
        LA = 2
        qks = [(A("qTa%d" % i, [128, HG, NOWN], BF16), Buf("qTa%d" % i),
                A("kTa%d" % i, [128, HG, 2 * NOWN], BF16), Buf("kTa%d" % i)) for i in range(2)]
        vts = [(A("vt%d" % i, [128, 32, HG * 128], BF16), Buf("vt%d" % i)) for i in range(3)]
        acc = A("acc", [128, HG, 2, NOWN], F32); bacc = [Buf("acc%d" % i) for i in range(HG)]
        NPB = LA + 2
        ptmp = [(A("ptmp%d" % i, [128, 256], F32), Buf("ptmp%d" % i)) for i in range(NPB)]
        pbf = [(A("pbf%d" % i, [128, 256], BF16), Buf("pbf%d" % i)) for i in range(NPB)]
        ostg = [(A("ostg%d" % i, [128, NOWN], BF16), Buf("ostg%d" % i)) for i in range(2)]
        rz = A("rz", [128, NOWN], F32); brz = Buf("rz")
        dOT = Buf("dOT")
        scale = float(DH ** -0.5)

        def load_qk(g):
            qT, bq, kT, bk = qks[g % 2]
            for hh in range(HG):
                r0 = (g * HG + hh) * 128
                S.dma('sp', qT[:, hh, :], k.QT[r0:r0 + 128, :], bq, writes=[bq])
                S.dma('sp', kT[:, hh, 0:NOWN], k.KTp[r0:r0 + 128, :], bk, writes=[bk])
                S.dma('sp', kT[:, hh, NOWN:2 * NOWN], k.KT[r0:r0 + 128, :], bk, writes=[bk])

        def load_v(g, di):
            d = DILS[di]
            vt, bvt = vts[di]
            nbh = 16 // d
            vt4 = vt[:, :, :].rearrange("p (r b) c -> p r b c", r=d)
            for half, src in enumerate((k.Vp, k.V)):
                sv = src[:, g * HG * 128:(g + 1) * HG * 128].rearrange("(b p r) c -> p r b c", p=128, r=d)
                for r in range(d):
                    S.dma('sp', vt4[:, r, half * nbh:(half + 1) * nbh, :], sv[:, r, :, :], bvt, writes=[bvt])

        def st_qk(b):
            qT, bq, kT, bk = qks[b['g'] % 2]
            d, hh, r, qb, nbh = b['d'], b['hh'], b['r'], b['qb'], b['nbh']
            kbq = nbh + qb
            pt, bpt = k.bank()
            b['pt'], b['bpt'] = pt, bpt
            q0 = r + d * 128 * qb
            b['q0'] = q0
            qsl = qT[:, hh, q0:q0 + d * 127 + 1:d]
            for ci, kb in enumerate((kbq, kbq - 1)):
                f0 = r + d * 128 * kb
                S.mm(lambda e: e.matmul(pt[:, ci * 128:(ci + 1) * 128], kT[:, hh, f0:f0 + d * 127 + 1:d], qsl,
                                        start=True, stop=True), reads=[bk, bq], writes=[bpt], sig=(ci == 1))
            pm, bpm = ptmp[b['i'] % NPB]; pb, bpb = pbf[b['i'] % NPB]
            b['pb'], b['bpb'] = pb, bpb
            idx = b['idx']
            S.op('act', lambda e: e.activation(pm[:, :], pt[:, 0:256], AF.Exp, scale=scale), reads=[bpt], writes=[bpm])
            me = 'dve'
            if qb == 0:
                S.op(me, lambda e: e.tensor_tensor(pb[:, 0:128], pm[:, 0:128], EB[:, idx, 0:128], ALU.mult),
                     reads=[bpm, cb], writes=[bpb])
                S.op(me, lambda e: e.tensor_tensor(pb[:, 128:256], pm[:, 128:256], EBf[:, idx, :], ALU.mult),
                     reads=[bpm, cb], writes=[bpb])
            else:
                S.op(me, lambda e: e.tensor_tensor(pb[:, :], pm[:, :], EB[:, idx, :], ALU.mult),
                     reads=[bpm, cb], writes=[bpb])

        def st_pv(b):
            d, hh, r, qb, nbh, di = b['d'], b['hh'], b['r'], b['qb'], b['nbh'], b['di']
            vt, bvt = vts[di]
            pb, bpb = b['pb'], b['bpb']
            kbq = nbh + qb
            po, bpo = k.bank()
            for ci, kb in enumerate((kbq, kbq - 1)):
                tile = r * (2 * nbh) + kb
                S.mm(lambda e: e.matmul(po[:, 0:128], vt[:, tile, hh * 128:(hh + 1) * 128], pb[:, ci * 128:(ci + 1) * 128],
                                        start=(ci == 0), stop=(ci == 1)), reads=[bvt, bpb], writes=[bpo], sig=False)
            for ci in range(2):
                S.mm(lambda e: e.matmul(po[:, 128:256], ones[:, :], pb[:, ci * 128:(ci + 1) * 128],
                                        start=(ci == 0), stop=(ci == 1)), reads=[cb, bpb], writes=[bpo], sig=(ci == 1))
            q0 = b['q0']
            asl = acc[:, hh, :, q0:q0 + d * 127 + 1:d]
            pv = po[:, 0:256].rearrange("p (a t) -> p a t", a=2)
            if di == 0:
                S.op('act', lambda e: e.copy(asl, pv), reads=[bpo], writes=[bacc[hh]])
            else:
                S.op('dve', lambda e: e.tensor_tensor(asl, pv, asl, ALU.add), reads=[bpo, bacc[hh]], writes=[bacc[hh]])

        load_qk(0)
        for di in range(3):
            load_v(0, di)
        bi = 0
        for g in range(NG):
            blocks = []
            marks = {}
            for di, d in enumerate(DILS):
                nbh = 16 // d
                for hh in range(HG):
                    for r in range(d):
                        for qb in range(nbh):
                            blocks.append(dict(g=g, di=di, d=d, hh=hh, r=r, qb=qb, nbh=nbh, i=bi,
                                               idx=(g * HG + hh) * 3 + di))
                            bi += 1
                marks[len(blocks) - 1] = di
            n = len(blocks)
            for i in range(n + LA):
                if i < n:
                    st_qk(blocks[i])
                if i - LA >= 0:
                    st_pv(blocks[i - LA])
                    bd = blocks[i - LA]
                    if bd['di'] == 2 and bd['r'] == 15 and bd['qb'] == bd['nbh'] - 1:
                        hh = bd['hh']
                        h = g * HG + hh
                        og, bog = ostg[h % 2]
                        S.op('act', lambda e: e.activation(rz[:, :], acc[:, hh, 1, :], AF.Ln), reads=[bacc[hh]], writes=[brz])
                        S.op('act', lambda e: e.activation(rz[:, :], rz[:, :], AF.Exp, scale=-1.0), reads=[brz], writes=[brz])
                        S.op('pool', lambda e: e.tensor_tensor(og[:, :], acc[:, hh, 0, :], rz[:, :], ALU.mult),
                             reads=[bacc[hh], brz], writes=[bog])
                        S.dma('sp', k.OT[h * 128:(h + 1) * 128, :], og[:, :], bog, reads=[bog], writes=[dOT])
                    if (i - LA) in marks and g + 1 < NG:
                        di_done = marks[i - LA]
                        if di_done == 0:
                            load_qk(g + 1)
                        load_v(g + 1, di_done)
        S.barrier()
    with ExitStack() as es:
        A = lambda name, shape, dt: es.enter_context(nc.sbuf_tensor(name, shape, dt))
        k.xres = A("xres3", [128, 8, D], F32); k.bx = [Buf("xres3_%d" % i) for i in range(8)]; k.bxd = [Buf("xres3_dma0"), Buf("xres3_dma1")]
        k.hT = A("hT3", [128, 16, TT], BF16); k.bhT = Buf("hT3")
        k.actT = A("actT3", [128, 16, TT], BF16); k.bactT = [Buf("actT3_%d" % c) for c in range(16)]
        k.wslots = [(A("wc%d" % i, [128, 4096], BF16), Buf("wc%d" % i)) for i in range(2)]
        k.hb = [(A("hc%d" % i, [128, D], BF16), Buf("hc%d" % i)) for i in range(2)]
        k.stat = A("stat3", [128, 32], F32); k.bstat = Buf("stat3")
        k.rtmp = [(A("rtmq%d" % i, [128, 512], F32), Buf("rtmq%d" % i)) for i in range(2)]
        k.rti = 0
        gfB = A("gfB", [128, D], F32)
        S.dma('sp', gfB[:, :], k.final_g[0:1, :].partition_broadcast(128), cb, writes=[cb])
        k.gmlp1 = load_gT(k, "gmlp1", k.norm_mlp_g1[1:2, :], dst=A("gmlp1", [128, 16], F32)[:, :])
        ost = [(A("ost%d" % i, [128, D], F32), Buf("ost%d" % i)) for i in range(2)]
        dout = Buf("dout")
        pipe = Pipe(k)
        for t in range(NT):
            def c_load(wv, bwb, t=t):
                v = k.X1[t * TT:(t + 1) * TT, :].rearrange("(s p) d -> p s d", p=128)
                for s0 in range(0, 8, 4):
                    S.dma('sp', k.xres[:, s0:s0 + 4, :], v[:, s0:s0 + 4, :], k.bxd[s0 // 4], writes=k.bx[s0:s0 + 4])
                S.dma('sp', k.actT[:, :, :], k.OT[:, t * TT:(t + 1) * TT].rearrange("(c p) t -> p c t", p=128), k.bactT[0],
                      writes=k.bactT)
            pipe.add(None, c_load)
            for n in range(8):
                def wo(wv, bwb, n=n):
                    def evac_t(m, ps, bps):
                        S.op('dve', lambda e: e.tensor_tensor(k.xres[:, m, n * 256:(n + 1) * 256], ps,
                                                              k.xres[:, m, n * 256:(n + 1) * 256], ALU.add),
                             reads=[bps, k.bx[m]], writes=[k.bx[m]])
                    gemm_t(k, wv, bwb, 256, k.actT, k.bactT, range(8), evac_t)
                pipe.add(wblock_loader(k, k.w_o, 0, [(n * 256, 256)]), wo)
            mlp_tile(k, pipe, k.gmlp1, k.w1_1, k.w2_1)

            def c_final(wv, bwb, t=t):
                for s in range(8):
                    hb, bhb = k.hb[s % 2]
                    S.op('act', lambda e: e.activation(hb[:, :], k.xres[:, s, :], AF.Square, accum_out=k.stat[:, s:s + 1]),
                         reads=[k.bx[s]], writes=[bhb, k.bstat])
                S.op('act', lambda e: e.activation(k.stat[:, 8:16], k.stat[:, 0:8], AF.Ln, scale=1.0 / D, bias=k.epsc[:, 0:1]),
                     reads=[k.bstat, cb], writes=[k.bstat])
                S.op('act', lambda e: e.activation(k.stat[:, 16:24], k.stat[:, 8:16], AF.Exp, scale=-0.5),
                     reads=[k.bstat], writes=[k.bstat])
                for s in range(8):
                    o, bo = ost[s % 2]
                    S.op('dve', lambda e: e.scalar_tensor_tensor(o[:, :], k.xres[:, s, :], k.stat[:, 16 + s:17 + s], gfB[:, :],
                                                                 ALU.mult, ALU.mult), reads=[k.bx[s], k.bstat, cb], writes=[bo])
                    r0 = t * TT + s * 128
                    S.dma('sp', k.out[r0:r0 + 128, :], o[:, :], bo, reads=[bo], writes=[dout])
            pipe.add(None, c_final)
        pipe.run()
        S.barrier()


FUSED = True
_CACHE = {}


def _get(mode, ncores=8):
    key = (mode, ncores)
    if key not in _CACHE:
        _CACHE[key] = build(mode, ncores)
    return _CACHE[key]


def _maps_A(inp, cores):
    x = inp['x']
    maps = []
    zeros = np.zeros((NOWN, D), np.float32)
    for c in cores:
        b, half = c // 2, c % 2
        maps.append({
            'x_own': np.ascontiguousarray(x[b, half * NOWN:(half + 1) * NOWN]),
            'x_prev': np.ascontiguousarray(x[b, 0:NOWN]) if half == 1 else zeros,
            'norm_mix_g': inp['norm_mix_g'], 'norm_mlp_g': inp['norm_mlp_g'],
            'hyb_w_in': inp['hyb_w_in'][0], 'conv_w': inp['conv_w'][0],
            'gla_w_gate2': inp['gla_w_gate2'][0], 'gla_b_gate': inp['gla_b_gate'],
            'gla_norm_g': inp['gla_norm_g'], 'hyb_w_out': inp['hyb_w_out'][0],
            'attn_w_qkv': inp['attn_w_qkv'][0],
            'mlp_w1_0': inp['mlp_w1'][0], 'mlp_w2_0': inp['mlp_w2'][0],
        })
    return maps


def _maps_B_extra(inp, cores):
    maps = []
    for c in cores:
        half = c % 2
        maps.append({
            'flag': np.full((128, 1), float(half), np.float32),
            'final_norm_g': inp['final_norm_g'].reshape(1, D),
            'attn_w_o': inp['attn_w_o'][0],
            'mlp_w1_1': inp['mlp_w1'][1], 'mlp_w2_1': inp['mlp_w2'][1],
        })
    return maps


def kernel(**inputs):
    inp = {k_: np.ascontiguousarray(np.asarray(v)) for k_, v in inputs.items()}
    B = inp['x'].shape[0]
    ncores = 2 * B
    cores = list(range(ncores))
    if FUSED:
        nc = _get('F', ncores)
        mA = _maps_A(inp, cores)
        mB = _maps_B_extra(inp, cores)
        maps = [dict(a, **b) for a, b in zip(mA, mB)]
        res = run_bass_kernel_spmd(nc, maps, core_ids=cores)
        outs = [np.asarray(r['out']) for r in res.results]
    else:
        ncA = _get('A', ncores)
        resA = run_bass_kernel_spmd(ncA, _maps_A(inp, cores), core_ids=cores).results
        ncB = _get('B', ncores)
        mB = _maps_B_extra(inp, cores)
        zk = None
        for c in cores:
            m = mB[c]
            m['norm_mlp_g'] = inp['norm_mlp_g']
            for nm in ('X1', 'QT', 'KT', 'V'):
                m[nm] = np.asarray(resA[c][nm])
            if c % 2 == 1:
                m['KTp'] = np.asarray(resA[c - 1]['KT'])
                m['Vp'] = np.asarray(resA[c - 1]['V'])
            else:
                if zk is None:
                    zk = np.zeros_like(np.asarray(resA[c]['KT']))
                m['KTp'] = zk
                m['Vp'] = zk
        res = run_bass_kernel_spmd(ncB, mB, core_ids=cores)
        outs = [np.asarray(r['out']) for r in res.results]
    out = np.stack([np.concatenate([outs[2 * b], outs[2 * b + 1]], axis=0) for b in range(B)], axis=0)
    return out.astype(np.float32)
```

```python
import numpy as np
import concourse.bass as bass
import concourse.mybir as mybir
from concourse.bass_utils import run_bass_kernel_spmd

F32 = mybir.dt.float32
BF16 = mybir.dt.bfloat16
AF = mybir.ActivationFunctionType
ALU = mybir.AluOpType
AX = mybir.AxisListType

D = 2048
NOWN = 2048
TT = 1024
NT = NOWN // TT
ST = 512
DFF = 8192
INCOLS = 6160
EPS = 1e-6
NHEAD_ATT = 16
DH = 128


class Buf:
    def __init__(self, name):
        self.name = name
        self.wt = None
        self.rts = []
        self.dsem = None
        self.dcount = 0


class Sched:
    def __init__(self, nc):
        self.nc = nc
        self.eng = {'pe': nc.tensor, 'act': nc.scalar, 'dve': nc.vector, 'pool': nc.gpsimd, 'sp': nc.sync}
        self.sem = {k: nc.alloc_semaphore(name="prog_" + k) for k in self.eng}
        self.cnt = {k: 0 for k in self.eng}
        self.waited = {k: {} for k in self.eng}
        self.nwaits = 0
        self.nops = 0
        self.allsems = {}

    def _deps(self, e, reads, writes, skip_waw_sem=None):
        need = {}

        def add(t):
            if t is None:
                return
            s, v = t
            k = id(s)
            if k not in need or need[k][1] < v:
                need[k] = (s, v)
        for r in reads:
            add(r.wt)
        for w in writes:
            if not (skip_waw_sem is not None and w.wt is not None and w.wt[0] is skip_waw_sem):
                add(w.wt)
            for t in w.rts:
                add(t)
        wd = self.waited[e]
        for k, (s, v) in need.items():
            if wd.get(k, 0) >= v:
                continue
            if e == 'pe' and s is self.sem['pe']:
                continue
            self.eng[e].wait_ge(s, v)
            wd[k] = v
            self.nwaits += 1

    def _done(self, t, reads, writes):
        for r in reads:
            r.rts = [x for x in r.rts if x[0] is not t[0]] + [t]
        for w in writes:
            w.wt = t
            w.rts = []

    def op(self, e, fn, reads=(), writes=()):
        self._deps(e, reads, writes)
        ins = fn(self.eng[e])
        self.cnt[e] += 1
        ins.then_inc(self.sem[e], 1)
        t = (self.sem[e], self.cnt[e])
        self._done(t, reads, writes)
        self.nops += 1
        return t

    def mm(self, fn, reads=(), writes=(), sig=True):
        self._deps('pe', reads, writes)
        ins = fn(self.eng['pe'])
        self.nops += 1
        if sig:
            self.cnt['pe'] += 1
            ins.then_inc(self.sem['pe'], 1)
        t = (self.sem['pe'], self.cnt['pe'] + (0 if sig else 1))
        self._done(t, reads, writes)
        return t

    def dma(self, q, out_ap, in_ap, semb, reads=(), writes=(), **kw):
        if semb.dsem is None:
            semb.dsem = self.nc.alloc_semaphore(name="d_" + semb.name)
            self.allsems[id(semb.dsem)] = semb
        self._deps(q, reads, writes, skip_waw_sem=semb.dsem)
        ins = self.eng[q].dma_start(out=out_ap, in_=in_ap, **kw)
        semb.dcount += 16
        ins.then_inc(semb.dsem, 16)
        t = (semb.dsem, semb.dcount)
        self._done(t, reads, writes)
        self.nops += 1
        return t

    def wait_bufs(self, e, bufs):
        self._deps(e, bufs, bufs)

    def barrier(self, extra_bufs=()):
        for e in self.eng:
            wd = self.waited[e]
            for e2 in self.eng:
                v = self.cnt[e2]
                if v > 0 and wd.get(id(self.sem[e2]), 0) < v:
                    self.eng[e].wait_ge(self.sem[e2], v)
                    wd[id(self.sem[e2])] = v
            for k, b in self.allsems.items():
                if b.dcount > 0 and wd.get(k, 0) < b.dcount:
                    self.eng[e].wait_ge(b.dsem, b.dcount)
                    wd[k] = b.dcount


class K:
    pass


def bc_last(ap, n):
    return ap.unsqueeze(2).broadcast_to([ap.shape[0], ap.shape[1], n])


def build(mode, ncores=8):
    nc = bass.Bass("TRN2", target_bir_lowering=False)
    S = Sched(nc)
    k = K()
    k.nc, k.S, k.mode = nc, S, mode
    k.ncores = ncores
    doA = mode in ('A', 'F')
    doB = mode in ('B', 'F')

    def dram_in(name, shape, dt=F32):
        return nc.dram_tensor(name, shape, dt, kind="ExternalInput").ap()

    def dram_out(name, shape, dt=F32):
        return nc.dram_tensor(name, shape, dt, kind="ExternalOutput").ap()

    def dram_int(name, shape, dt=F32):
        return nc.dram_tensor(name, shape, dt).ap()

    if doA:
        k.x_own = dram_in("x_own", [NOWN, D])
        k.x_prev = dram_in("x_prev", [NOWN, D])
        k.norm_mix_g = dram_in("norm_mix_g", [2, D])
        k.norm_mlp_g = dram_in("norm_mlp_g", [2, D])
        k.w_in = dram_in("hyb_w_in", [D, INCOLS])
        k.conv_w = dram_in("conv_w", [3, 1024])
        k.wg2 = dram_in("gla_w_gate2", [16, 512])
        k.bg = dram_in("gla_b_gate", [1, 512])
        k.gng = dram_in("gla_norm_g", [1, 256])
        k.w_out = dram_in("hyb_w_out", [D, D])
        k.w_qkv = dram_in("attn_w_qkv", [D, 3 * D])
        k.w1_0 = dram_in("mlp_w1_0", [D, DFF])
        k.w2_0 = dram_in("mlp_w2_0", [DFF, D])
    if mode == 'A':
        k.X1 = dram_out("X1", [NOWN, D])
        k.QT = dram_out("QT", [D, NOWN], BF16)
        k.KT = dram_out("KT", [D, NOWN], BF16)
        k.V = dram_out("V", [NOWN, D], BF16)
    elif mode == 'B':
        k.X1 = dram_in("X1", [NOWN, D])
        k.QT = dram_in("QT", [D, NOWN], BF16)
        k.KT = dram_in("KT", [D, NOWN], BF16)
        k.V = dram_in("V", [NOWN, D], BF16)
        k.KTp = dram_in("KTp", [D, NOWN], BF16)
        k.Vp = dram_in("Vp", [NOWN, D], BF16)
    else:
        k.X1 = dram_int("X1", [NOWN, D])
        k.QT = dram_int("QT", [D, NOWN], BF16)
        k.KT = dram_int("KT", [D, NOWN], BF16)
        k.V = dram_int("V", [NOWN, D], BF16)
        k.KTp = dram_int("KTp", [D, NOWN], BF16)
        k.Vp = dram_int("Vp", [NOWN, D], BF16)
    if doB:
        k.flag = dram_in("flag", [128, 1])
        k.norm_mlp_g1 = k.norm_mlp_g if doA else dram_in("norm_mlp_g", [2, D])
        k.final_g = dram_in("final_norm_g", [1, D])
        k.w_o = dram_in("attn_w_o", [D, D])
        k.w1_1 = dram_in("mlp_w1_1", [D, DFF])
        k.w2_1 = dram_in("mlp_w2_1", [DFF, D])
        k.OT = dram_int("OT", [D, NOWN], BF16)
        k.out = dram_out("out", [NOWN, D])

    k.banks = [(nc.alloc_psum_tensor("bank%d" % i, [128, 512], F32), Buf("bank%d" % i)) for i in range(8)]
    k.bank_i = 0

    def bank():
        b = k.banks[k.bank_i % 8]
        k.bank_i += 1
        return b
    k.bank = bank

    k.ident = nc.alloc_sbuf_tensor("ident", [128, 128], BF16)
    k.b_const = Buf("consts")
    cb = k.b_const
    S.op('pool', lambda e: e.memset(k.ident[:, :], 1.0), writes=[cb])
    S.op('pool', lambda e: e.affine_select(k.ident[:, :], k.ident[:, :], [[-1, 128]], ALU.is_equal, 0.0,
                                           base=0, channel_multiplier=1), reads=[cb], writes=[cb])

    k.ident32 = nc.alloc_sbuf_tensor("ident32", [16, 16], F32)
    S.op('pool', lambda e: e.memset(k.ident32[:, :], 1.0), writes=[cb])
    S.op('pool', lambda e: e.affine_select(k.ident32[:, :], k.ident32[:, :], [[-1, 16]], ALU.is_equal, 0.0,
                                           base=0, channel_multiplier=1), reads=[cb], writes=[cb])
    k.gstage = (nc.alloc_sbuf_tensor("gstage", [16, 128], F32), Buf("gstage"))
    k.epsc = nc.alloc_sbuf_tensor("epsc", [128, 2], F32)
    S.op('pool', lambda e: e.memset(k.epsc[:, 0:1], EPS), writes=[cb])
    S.op('pool', lambda e: e.memset(k.epsc[:, 1:2], 1.0), writes=[cb])
    if doA:
        phaseA(k)
    if doB:
        S.barrier()
        phaseB(k)
    S.barrier()
    print("built mode", mode, "ops", S.nops, "waits", S.nwaits)
    return nc


def load_gT(k, name, src_row_ap, nchunk=16, dst=None):
    nc, S = k.nc, k.S
    b = k.b_const
    if dst is None:
        dst = nc.alloc_sbuf_tensor(name, [128, nchunk], F32)[:, :]
    st, bst = k.gstage
    S.dma('sp', st[0:nchunk, :], src_row_ap.rearrange("o (c p) -> (o c) p", p=128), bst, writes=[bst])
    pt, bpt = k.bank()
    S.mm(lambda e: e.matmul(pt[:, 0:nchunk], st[0:nchunk, :], k.ident32[0:nchunk, 0:nchunk], start=True, stop=True),
         reads=[bst, b], writes=[bpt])
    S.op('dve', lambda e: e.tensor_copy(dst, pt[:, 0:nchunk]), reads=[bpt], writes=[b])
    return dst


def norm_transpose(k, xres, bx, gT, hT, bhT, nsub):
    nc, S = k.nc, k.S
    for s in range(nsub):
        hb, bhb = k.hb[s % 2]
        S.op('act', lambda e: e.activation(hb[:, :], xres[:, s, :], AF.Square, accum_out=k.stat[:, s:s + 1]),
             reads=[bx[s]], writes=[bhb, k.bstat])
    S.op('act', lambda e: e.activation(k.stat[:, 8:8 + nsub], k.stat[:, 0:nsub], AF.Ln, scale=1.0 / D, bias=k.epsc[:, 0:1]),
         reads=[k.bstat, k.b_const], writes=[k.bstat])
    S.op('act', lambda e: e.activation(k.stat[:, 16:16 + nsub], k.stat[:, 8:8 + nsub], AF.Exp, scale=-0.5),
         reads=[k.bstat], writes=[k.bstat])
    for s in range(nsub):
        hb, bhb = k.hb[s % 2]
        rstd = k.stat[:, 16 + s:17 + s]
        if s % 2 == 0:
            S.op('act', lambda e: e.activation(hb[:, :], xres[:, s, :], AF.Copy, scale=rstd),
                 reads=[bx[s], k.bstat], writes=[bhb])
        else:
            S.op('dve', lambda e: e.tensor_scalar(hb[:, :], xres[:, s, :], rstd, None, ALU.mult),
                 reads=[bx[s], k.bstat], writes=[bhb])
        for cg in range(2):
            pt, bpt = k.bank()
            pv = pt[:, :].bitcast(BF16)
            for c in range(8):
                cc = cg * 8 + c
                S.mm(lambda e: e.transpose(pv[:, c * 128:(c + 1) * 128], hb[:, cc * 128:(cc + 1) * 128], k.ident[:, :]),
                     reads=[bhb, k.b_const], writes=[bpt], sig=(c == 7))
            pv3 = pv.rearrange("p (c t) -> p c t", c=8)
            S.op('dve', lambda e: e.tensor_tensor(hT[:, cg * 8:(cg + 1) * 8, s * 128:(s + 1) * 128], pv3,
                                                  bc_last(gT[:, cg * 8:(cg + 1) * 8], 128), ALU.mult),
                 reads=[bpt, k.b_const], writes=[bhT])


def wblock_loader(k, W, r0, segs, nk=16):
    S = k.S
    ncols = sum(n for _, n in segs)

    def load(slot):
        wb, bwb = slot
        v = wb[:, 0:nk * ncols].rearrange("p (k c) -> p k c", k=nk)
        off = 0
        for (c0, n) in segs:
            src = W[r0:r0 + nk * 128, c0:c0 + n].rearrange("(kc p) c -> p kc c", p=128)
            S.dma('pool', v[:, :, off:off + n], src, bwb, writes=[bwb])
            off += n
        return v
    return load


def gemm_f(k, wv, bwb, chunks, rhsT, brhs, halves, evac):
    S = k.S
    nk = wv.shape[1]
    for ci, (off, width) in enumerate(chunks):
        for hi, (t0, n) in enumerate(halves):
            pt, bpt = k.bank()
            for kk in range(nk):
                S.mm(lambda e: e.matmul(pt[0:width, 0:n], wv[:, kk, off:off + width], rhsT[:, kk, t0:t0 + n],
                                        start=(kk == 0), stop=(kk == nk - 1)),
                     reads=[bwb, brhs], writes=[bpt], sig=(kk == nk - 1))
                if kk == nk // 2 - 1:
                    bg_tick(k)
            evac(ci, hi, pt[0:width, 0:n], bpt)
            bg_tick(k)


def gemm_t(k, wv, bwb, ncols, lhsT, blhs, subtiles, evac, t_base=0):
    S = k.S
    nk = wv.shape[1]
    for m in subtiles:
        pt, bpt = k.bank()
        t0 = t_base + m * 128
        for kk in range(nk):
            S.mm(lambda e: e.matmul(pt[:, 0:ncols], lhsT[:, kk, t0:t0 + 128], wv[:, kk, 0:ncols],
                                    start=(kk == 0), stop=(kk == nk - 1)),
                 reads=[bwb, blhs[kk] if isinstance(blhs, list) else blhs], writes=[bpt], sig=(kk == nk - 1))
        evac(m, pt[:, 0:ncols], bpt)


def bg_tick(k):
    g = getattr(k, 'bgen', None)
    if g is not None:
        try:
            next(g)
        except StopIteration:
            k.bgen = None


def bg_drain(k):
    while getattr(k, 'bgen', None) is not None:
        bg_tick(k)


class Pipe:
    def __init__(self, k):
        self.k = k
        self.steps = []

    def add(self, loader, compute):
        self.steps.append((loader, compute))

    def run(self):
        k = self.k
        slots = k.wslots
        ns = len(slots)
        views = {}
        li = 0
        wi = 0
        import os
        sk = os.environ.get("KSKIP")
        if sk:
            rngs = [[int(v) for v in r.split(":")] for r in sk.split(",")]
            self.steps = [st for i, st in enumerate(self.steps) if not any(a <= i < b for a, b in rngs)]
        order = [i for i, (l, c) in enumerate(self.steps) if l is not None]
        slot_of = {i: slots[j % ns] for j, i in enumerate(order)}
        pos = 0

        def issue_next():
            nonlocal pos
            if pos < len(order):
                i = order[pos]
                views[i] = self.steps[i][0](slot_of[i])
                pos += 1
        for _ in range(ns - 1):
            issue_next()
        import os
        lim = int(os.environ.get("KSTEPS", "1000000"))
        for i, (l, c) in enumerate(self.steps):
            if i >= lim:
                break
            if l is not None:
                while i not in views:
                    issue_next()
                c(views[i], slot_of[i][1])
                issue_next()
                del views[i]
            else:
                c(None, None)
        self.steps = []


def phaseA(k):
    from contextlib import ExitStack
    with ExitStack() as es:
        _phaseA(k, es)
        k.S.barrier()


def _phaseA(k, es):
    nc, S = k.nc, k.S
    cb = k.b_const
    A = lambda name, shape, dt: es.enter_context(nc.sbuf_tensor(name, shape, dt))
    k.xres = A("xres", [128, 8, D], F32); k.bx = [Buf("xres%d" % i) for i in range(8)]; k.bxd = [Buf("xres_dma0"), Buf("xres_dma1")]
    k.hT = A("hT", [128, 16, TT], BF16); k.bhT = Buf("hT")
    k.actT = A("actT", [128, 16, TT], BF16); k.bactT = [Buf("actT%d" % c) for c in range(16)]
    k.wslots = [(A("wb%d" % i, [128, 4096], BF16), Buf("wb%d" % i)) for i in range(2)]
    k.hb = [(A("hb%d" % i, [128, D], BF16), Buf("hb%d" % i)) for i in range(2)]
    k.stat = A("stat", [128, 32], F32); k.bstat = Buf("stat")
    k.gmix0 = load_gT(k, "gmix0", k.norm_mix_g[0:1, :], dst=A("gmix0", [128, 16], F32)[:, :])
    k.gmix1 = load_gT(k, "gmix1", k.norm_mix_g[1:2, :], dst=A("gmix1", [128, 16], F32)[:, :])
    k.gmlp0 = load_gT(k, "gmlp0", k.norm_mlp_g[0:1, :], dst=A("gmlp0", [128, 16], F32)[:, :])
    k.convw = A("convw", [128, 8, 3], F32)
    for kk in range(3):
        load_gT(k, None, k.conv_w[kk:kk + 1, :], nchunk=8, dst=k.convw[:, :, kk])
    k.wg2e = A("wg2e", [32, 512], BF16)
    k.b_wg2 = Buf("wg2e")
    S.dma('pool', k.wg2e[0:16, :], k.wg2[:, :], k.b_wg2, writes=[k.b_wg2])
    S.dma('pool', k.wg2e[16:17, :], k.bg[:, :], k.b_wg2, writes=[k.b_wg2])
    k.gngB = A("gngB", [128, 256], F32)
    S.dma('sp', k.gngB[:, :], k.gng[0:1, :].partition_broadcast(128), cb, writes=[cb])
    k.triG = A("triG", [128, 128], BF16)
    k.triD = A("triD", [128, 128], BF16)
    k.cmask = A("cmask", [128, 128], F32)
    S.op('pool', lambda e: e.memset(k.triG[:, :], -1.0 / 16), writes=[cb])
    S.op('pool', lambda e: e.affine_select(k.triG[:, :], k.triG[:, :], [[1, 128]], ALU.is_ge, 0.0, base=0,
                                           channel_multiplier=-1), reads=[cb], writes=[cb])
    S.op('pool', lambda e: e.memset(k.triD[:, :], -1.0 / 16), writes=[cb])
    S.op('pool', lambda e: e.affine_select(k.triD[:, :], k.triD[:, :], [[-1, 128]], ALU.is_gt, 0.0, base=0,
                                           channel_multiplier=1), reads=[cb], writes=[cb])
    S.op('pool', lambda e: e.memset(k.cmask[:, :], 1.0), writes=[cb])
    S.op('pool', lambda e: e.affine_select(k.cmask[:, :], k.cmask[:, :], [[1, 128]], ALU.is_ge, 0.0, base=0,
                                           channel_multiplier=-1), reads=[cb], writes=[cb])
    k.arena = A("arena", [128, 8192], BF16)
    ar = k.arena
    k.convst = ar[:, 0:4096].rearrange("p (c t) -> p c t", c=8); k.bconvst = [Buf("convst%d" % c) for c in range(8)]
    k.U = ar[:, 4096:5632].bitcast(F32)[:, 0:ST + 2]; k.bU = Buf("U")
    k.actmp = ar[:, 5632:6656].bitcast(F32); k.bactmp = Buf("actmp")
    k.t1 = k.actmp; k.bt1 = k.bactmp
    k.halo = A("halo", [128, 8, 2], F32); k.bhalo = Buf("halo")
    k.glowT = A("glowT", [32, ST], BF16); k.bglow = Buf("glowT")
    k.sp_tok = A("sp_tok", [128, 4, 512], BF16); k.bsp = Buf("sp_tok")
    k.qT = A("qT", [128, ST], BF16); k.bqT = Buf("qT")
    k.kT = A("kT", [128, ST], BF16); k.bkT = Buf("kT")
    k.k_tok = A("k_tok", [128, 4, 128], BF16); k.bktok = Buf("k_tok")
    k.v_tok = A("v_tok", [128, 4, 256], BF16); k.bvtok = Buf("v_tok")
    k.rg = A("rg", [128, 4, 256], F32); k.brg = Buf("rg")
    k.Sst = A("Sst", [128, 4, 256], F32); k.bSst = [Buf("Sst%d" % h) for h in range(4)]
    k.Sbf = A("Sbf", [128, 4, 256], BF16); k.bSbf = [Buf("Sbf%d" % h) for h in range(4)]
    k.ch = []
    for i in range(2):
        d = {}
        for nm, shp, dt in [("Eg", [128, 128], F32), ("Eng", [128, 128], F32), ("Dd", [128, 128], F32),
                            ("qg", [128, 128], BF16), ("kg", [128, 128], BF16), ("kd", [128, 128], BF16),
                            ("sm", [128, 128], BF16), ("of", [128, 256], BF16), ("osq", [128, 256], BF16),
                            ("cst", [128, 4], F32)]:
            d[nm] = (A("%s%d" % (nm, i), shp, dt), Buf("%s%d" % (nm, i)))
        k.ch.append(d)
    k.chi = 0
    k.rtmp = [(A("rtmp%d" % i, [128, 512], F32), Buf("rtmp%d" % i)) for i in range(2)]
    k.rti = 0
    k.sptmp, k.bsptmp = k.rtmp[0]
    k.qstage = [(ar[:, i * 2048:(i + 1) * 2048].rearrange("p (c t) -> p c t", c=2), Buf("qstage%d" % i)) for i in range(2)]
    k.vstage = [(ar[:, 4096 + i * 2048:4096 + (i + 1) * 2048].rearrange("p (m c) -> p m c", m=8), Buf("vstage%d" % i)) for i in range(2)]
    k.sti = 0
    k.dX1 = Buf("dX1"); k.dX1s = [Buf("dX1a"), Buf("dX1b")]; k.dQT = Buf("dQT"); k.dKT = Buf("dKT"); k.dV = Buf("dV")

    import os
    if os.environ.get("KDEBUG_INIT"):
        for c in range(16):
            S.op('dve', lambda e: e.memset(k.actT[:, c, :], 0.0), writes=[k.bactT[c]])
    for h in range(4):
        S.op('dve', lambda e: e.memset(k.Sst[:, h, :], 0.0), writes=[k.bSst[h]])
        S.op('dve', lambda e: e.memset(k.Sbf[:, h, :], 0.0), writes=[k.bSbf[h]])
    S.op('dve', lambda e: e.memset(k.halo[:, :, :], 0.0), writes=[k.bhalo])
    S.op('dve', lambda e: e.memset(k.glowT[:, :], 1.0), writes=[k.bglow])

    pipe = Pipe(k)
    if k.mode == 'F':
        seq = [(k.x_prev, t, True) for t in range(NT)] + [(k.x_own, t, False) for t in range(NT)]
        for i, (src, t, prev) in enumerate(seq):
            mixer_tile(k, pipe, src, t, state_only=False, last=False, preloaded=(i > 0))
            mlp_tile(k, pipe, k.gmlp0, k.w1_0, k.w2_0)
            nxt = seq[i + 1][:2] if i + 1 < len(seq) else None
            qkv_tile(k, pipe, t, prev=prev, next_x=nxt)
        pipe.run()
        return
    else:
        for t in range(NT):
            mixer_tile(k, pipe, k.x_prev, t, state_only=True, last=(t == NT - 1))
    for t in range(NT):
        mixer_tile(k, pipe, k.x_own, t, state_only=False, last=False)
        mlp_tile(k, pipe, k.gmlp0, k.w1_0, k.w2_0)
        qkv_tile(k, pipe, t)
    pipe.run()


def load_x_tile(k, src, t):
    S = k.S
    v = src[t * TT:(t + 1) * TT, :].rearrange("(s p) d -> p s d", p=128)
    for s0 in range(0, 8, 4):
        S.dma('sp', k.xres[:, s0:s0 + 4, :], v[:, s0:s0 + 4, :], k.bxd[s0 // 4], writes=k.bx[s0:s0 + 4])


def mixer_tile(k, pipe, xsrc, t, state_only, last, preloaded=False):
    nc, S = k.nc, k.S
    cb = k.b_const
    W = k.w_in

    def c_load(wv, bwb):
        if not preloaded:
            load_x_tile(k, xsrc, t)
        norm_transpose(k, k.xres, k.bx, k.gmix0, k.hT, k.bhT, 8)
    pipe.add(None, c_load)

    def c_bar(wv, bwb):
        S.barrier()
    pipe.add(None, c_bar)
    for sub in range(TT // ST):
        mixer_sub(k, pipe, sub, state_only, last)

    if not state_only:
        for n in range(8):
            def wout(wv, bwb, n=n):
                def evac_t(m, ps, bps):
                    S.op('dve', lambda e: e.tensor_tensor(k.xres[:, m, n * 256:(n + 1) * 256], ps,
                                                          k.xres[:, m, n * 256:(n + 1) * 256], ALU.add),
                         reads=[bps, k.bx[m]], writes=[k.bx[m]])
                gemm_t(k, wv, bwb, 256, k.actT, k.bactT, range(8), evac_t)
            pipe.add(wblock_loader(k, k.w_out, 0, [(n * 256, 256)]), wout)


def mixer_sub(k, pipe, sub, state_only, last):
    nc, S = k.nc, k.S
    cb = k.b_const
    W = k.w_in
    tb = sub * ST
    hTs = k.hT
    halves = [(tb, ST)]

    conv_steps = []
    if not state_only:
        def mk_conv_c(c):
            def conv_c(wv, bwb):
                def evac(ci, hi, ps, bps):
                    if ci == 0:
                        S.op('act', lambda e: e.copy(k.actmp[:, :], ps), reads=[bps], writes=[k.bactmp])
                    else:
                        S.op('dve', lambda e: e.tensor_copy(k.U[:, 0:2], k.halo[:, c, :]), reads=[k.bhalo], writes=[k.bU])
                        S.op('dve', lambda e: e.tensor_tensor(k.U[:, 2:2 + ST], ps, k.actmp[:, :], ALU.mult),
                             reads=[bps, k.bactmp], writes=[k.bU])
                        S.op('dve', lambda e: e.tensor_copy(k.halo[:, c, :], k.U[:, ST:ST + 2]), reads=[k.bU], writes=[k.bhalo])
                        S.op('act', lambda e: e.activation(k.t1[:, :], k.U[:, 2:2 + ST], AF.Copy, scale=k.convw[:, c, 2:3]),
                             reads=[k.bU, cb], writes=[k.bt1])
                        S.op('dve', lambda e: e.scalar_tensor_tensor(k.t1[:, :], k.U[:, 1:1 + ST], k.convw[:, c, 1:2], k.t1[:, :],
                                                                     ALU.mult, ALU.add),
                             reads=[k.bU, k.bt1, cb], writes=[k.bt1])
                        S.op('dve', lambda e: e.scalar_tensor_tensor(k.convst[:, c, :], k.U[:, 0:ST], k.convw[:, c, 0:1], k.t1[:, :],
                                                                     ALU.mult, ALU.add),
                             reads=[k.bU, k.bt1, cb], writes=[k.bconvst[c]])
                gemm_f(k, wv, bwb, [(0, 128), (128, 128)], hTs, k.bhT, halves, evac)
            return (wblock_loader(k, W, 0, [(1024 + c * 128, 128), (2048 + c * 128, 128)]), conv_c)

        def mk_convb(cp):
            def convb(wv, bwb):
                def evac(ci, hi, ps, bps):
                    c = 2 * cp + ci
                    S.op('dve', lambda e: e.tensor_tensor(k.actT[:, c, tb:tb + ST], ps, k.convst[:, c, :], ALU.mult),
                         reads=[bps, k.bconvst[c]], writes=[k.bactT[c]])
                gemm_f(k, wv, bwb, [(0, 128), (128, 128)], hTs, k.bhT, halves, evac)
                bg_drain(k)
            return (wblock_loader(k, W, 0, [(cp * 256, 256)]), convb)
        for cp in range(4):
            conv_steps.append([mk_conv_c(2 * cp), mk_conv_c(2 * cp + 1), mk_convb(cp)])
    elif last and sub == TT // ST - 1:
        for c in range(8):
            def halo_c(wv, bwb, c=c):
                def evac(ci, hi, ps, bps):
                    if ci == 0:
                        S.op('act', lambda e: e.copy(k.actmp[:, 0:2], ps), reads=[bps], writes=[k.bactmp])
                    else:
                        S.op('dve', lambda e: e.tensor_tensor(k.halo[:, c, :], ps, k.actmp[:, 0:2], ALU.mult),
                             reads=[bps, k.bactmp], writes=[k.bhalo])
                gemm_f(k, wv, bwb, [(0, 128), (128, 128)], hTs, k.bhT, [(TT - 2, 2)], evac)
            pipe.add(wblock_loader(k, W, 0, [(1024 + c * 128, 128), (2048 + c * 128, 128)]), halo_c)

    def gate(wv, bwb):
        def evac(ci, hi, ps, bps):
            S.op('act', lambda e: e.copy(k.glowT[0:16, :], ps), reads=[bps], writes=[k.bglow])
        gemm_f(k, wv, bwb, [(0, 16)], hTs, k.bhT, halves, evac)
        for m in range(4):
            pt, bpt = k.bank()
            S.mm(lambda e: e.matmul(pt[:, 0:512], k.glowT[0:17, m * 128:(m + 1) * 128], k.wg2e[0:17, :],
                                    start=True, stop=True), reads=[k.bglow, k.b_wg2], writes=[bpt])
            S.op('act', lambda e: e.activation(k.sptmp[:, :], pt[:, 0:512], AF.Exp, scale=-1.0),
                 reads=[bpt], writes=[k.bsptmp])
            S.op('act', lambda e: e.activation(k.sp_tok[:, m, :], k.sptmp[:, :], AF.Ln, bias=k.epsc[:, 1:2]),
                 reads=[k.bsptmp, cb], writes=[k.bsp])
    pipe.add(wblock_loader(k, W, 0, [(6144, 16)]), gate)

    for h in range(4):
        if not state_only:
            def qk(wv, bwb, h=h):
                def evac(ci, hi, ps, bps):
                    if ci == 0:
                        S.op('act', lambda e: e.activation(k.qT[:, :], ps, AF.Copy, scale=float(128 ** -0.5)),
                             reads=[bps], writes=[k.bqT])
                    else:
                        S.op('act', lambda e: e.copy(k.kT[:, :], ps), reads=[bps], writes=[k.bkT])
                gemm_f(k, wv, bwb, [(0, 128), (128, 128)], hTs, k.bhT, halves, evac)

                def evac_t(m, ps, bps):
                    S.op('dve', lambda e: e.tensor_copy(k.k_tok[:, m, :], ps), reads=[bps], writes=[k.bktok])
                gemm_t(k, wv[:, :, 128:256], bwb, 128, hTs, k.bhT, range(4), evac_t, t_base=tb)
            pipe.add(wblock_loader(k, W, 0, [(3072 + h * 128, 128), (3584 + h * 128, 128)]), qk)
        else:
            def konly(wv, bwb, h=h):
                def evac_t(m, ps, bps):
                    S.op('dve', lambda e: e.tensor_copy(k.k_tok[:, m, :], ps), reads=[bps], writes=[k.bktok])
                gemm_t(k, wv, bwb, 128, hTs, k.bhT, range(4), evac_t, t_base=tb)
            pipe.add(wblock_loader(k, W, 0, [(3584 + h * 128, 128)]), konly)

        def vproj(wv, bwb, h=h):
            def evac_t(m, ps, bps):
                S.op('act', lambda e: e.copy(k.v_tok[:, m, :], ps), reads=[bps], writes=[k.bvtok])
            gemm_t(k, wv, bwb, 256, hTs, k.bhT, range(4), evac_t, t_base=tb)
            if state_only:
                k.bgen = gla_gen(k, h, tb, state_only=True)
                bg_drain(k)
        pipe.add(wblock_loader(k, W, 0, [(4096 + h * 256, 256)]), vproj)

        if not state_only:
            def rproj(wv, bwb, h=h):
                def evac_t(m, ps, bps):
                    rt, brt = k.rtmp[k.rti % 2]; k.rti += 1
                    S.op('act', lambda e: e.activation(rt[:, 0:256], ps, AF.Silu), reads=[bps], writes=[brt])
                    S.op('dve', lambda e: e.tensor_tensor(k.rg[:, m, :], rt[:, 0:256], k.gngB[:, :], ALU.mult),
                         reads=[brt, cb], writes=[k.brg])
                gemm_t(k, wv, bwb, 256, hTs, k.bhT, range(4), evac_t, t_base=tb)
                bg_drain(k)
                k.bgen = gla_gen(k, h, tb, state_only=False)
            pipe.add(wblock_loader(k, W, 0, [(5120 + h * 256, 256)]), rproj)
            for st in conv_steps[h]:
                pipe.add(*st)


def gla_gen(k, h, tb, state_only):
    nc, S = k.nc, k.S
    cb = k.b_const
    hs = slice(h * 128, (h + 1) * 128)
    Ts = {}

    def fa(m):
        T = k.ch[k.chi % 2]; k.chi += 1
        Ts[m] = T
        Eg, bEg = T["Eg"]; Eng, bEng = T["Eng"]; Dd, bDd = T["Dd"]
        qg, bqg = T["qg"]; kg, bkg = T["kg"]; kd, bkd = T["kd"]
        cst, bcst = T["cst"]
        ms = slice(m * 128, (m + 1) * 128)
        p2, bp2 = k.bank()
        S.mm(lambda e: e.matmul(p2[:, 0:128], k.triD[:, :], k.sp_tok[:, m, hs], start=True, stop=True),
             reads=[cb, k.bsp], writes=[bp2])
        if not state_only:
            p1, bp1 = k.bank()
            S.mm(lambda e: e.matmul(p1[:, 0:128], k.sp_tok[:, m, hs], k.triG[:, :], start=True, stop=True),
                 reads=[cb, k.bsp], writes=[bp1])
            S.op('act', lambda e: e.activation(Eg[:, :], p1[:, 0:128], AF.Exp), reads=[bp1], writes=[bEg])
            S.op('act', lambda e: e.activation(Eng[:, :], p1[:, 0:128], AF.Exp, scale=-1.0), reads=[bp1], writes=[bEng])
            S.op('dve', lambda e: e.tensor_tensor(qg[:, :], k.qT[:, ms], Eg[:, :], ALU.mult),
                 reads=[k.bqT, bEg], writes=[bqg])
            S.op('dve', lambda e: e.tensor_tensor(kg[:, :], k.kT[:, ms], Eng[:, :], ALU.mult),
                 reads=[k.bkT, bEng], writes=[bkg])
        else:
            p1, bp1 = k.bank()
            S.mm(lambda e: e.matmul(p1[:, 0:1], k.sp_tok[:, m, hs], k.triG[:, 127:128], start=True, stop=True),
                 reads=[cb, k.bsp], writes=[bp1])
            S.op('act', lambda e: e.activation(cst[:, 2:3], p1[:, 0:1], AF.Exp), reads=[bp1], writes=[bcst])
        S.op('act', lambda e: e.activation(Dd[:, :], p2[:, 0:128], AF.Exp), reads=[bp2], writes=[bDd])
        S.op('dve', lambda e: e.tensor_tensor(kd[:, :], k.k_tok[:, m, :], Dd[:, :], ALU.mult),
             reads=[k.bktok, bDd], writes=[bkd])

    def fb(m):
        if state_only:
            return
        T = Ts[m]
        qg, bqg = T["qg"]; kg, bkg = T["kg"]; sm, bsm = T["sm"]
        p3, bp3 = k.bank()
        S.mm(lambda e: e.matmul(p3[:, 0:128], kg[:, :], qg[:, :], start=True, stop=True),
             reads=[bkg, bqg], writes=[bp3])
        S.op('dve', lambda e: e.tensor_tensor(sm[:, :], p3[:, 0:128], k.cmask[:, :], ALU.mult),
             reads=[bp3, cb], writes=[bsm])

    def ba(m):
        T = Ts[m]
        Eg, bEg = T["Eg"]
        qg, bqg = T["qg"]; kd, bkd = T["kd"]
        sm, bsm = T["sm"]; of, bof = T["of"]; osq, bosq = T["osq"]; cst, bcst = T["cst"]
        if not state_only:
            egl, begl = Eg[:, 127:128], bEg
            p4, bp4 = k.bank()
            S.mm(lambda e: e.matmul(p4[:, 0:256], sm[:, :], k.v_tok[:, m, :], start=True, stop=False),
                 reads=[bsm, k.bvtok], writes=[bp4], sig=False)
            S.mm(lambda e: e.matmul(p4[:, 0:256], qg[:, :], k.Sbf[:, h, :], start=False, stop=True),
                 reads=[bqg, k.bSbf[h]], writes=[bp4])
        else:
            egl, begl = cst[:, 2:3], bcst
        p6, bp6 = k.bank()
        S.mm(lambda e: e.matmul(p6[:, 0:256], kd[:, :], k.v_tok[:, m, :], start=True, stop=True),
             reads=[bkd, k.bvtok], writes=[bp6])
        S.op('dve', lambda e: e.scalar_tensor_tensor(k.Sst[:, h, :], k.Sst[:, h, :], egl, p6[:, 0:256], ALU.mult, ALU.add),
             reads=[k.bSst[h], begl, bp6], writes=[k.bSst[h]])
        S.op('act', lambda e: e.copy(k.Sbf[:, h, :], k.Sst[:, h, :]), reads=[k.bSst[h]], writes=[k.bSbf[h]])
        if not state_only:
            S.op('act', lambda e: e.activation(osq[:, :], p4[:, 0:256], AF.Square, accum_out=cst[:, 0:1]),
                 reads=[bp4], writes=[bosq, bcst])
            S.op('act', lambda e: e.activation(cst[:, 3:4], cst[:, 0:1], AF.Ln, scale=1.0 / 256, bias=k.epsc[:, 0:1]),
                 reads=[bcst, cb], writes=[bcst])
            S.op('act', lambda e: e.activation(cst[:, 1:2], cst[:, 3:4], AF.Exp, scale=-0.5),
                 reads=[bcst], writes=[bcst])
            S.op('dve', lambda e: e.scalar_tensor_tensor(of[:, :], p4[:, 0:256], cst[:, 1:2], k.rg[:, m, :], ALU.mult, ALU.mult),
                 reads=[bp4, bcst, k.brg], writes=[bof])

    def bb(m):
        if state_only:
            return
        T = Ts[m]
        of, bof = T["of"]
        p5, bp5 = k.bank()
        pv = p5[:, :].bitcast(BF16)
        for j in range(2):
            S.mm(lambda e: e.transpose(pv[:, j * 128:(j + 1) * 128], of[:, j * 128:(j + 1) * 128], k.ident[:, :]),
                 reads=[bof, cb], writes=[bp5], sig=(j == 1))
        S.op('act', lambda e: e.copy(k.actT[:, 8 + 2 * h:10 + 2 * h, tb + m * 128:tb + (m + 1) * 128],
                                     pv[:, 0:256].rearrange("p (c t) -> p c t", c=2)),
             reads=[bp5], writes=[k.bactT[8 + 2 * h], k.bactT[9 + 2 * h]])

    order = [(fa, 0), (fa, 1), (fb, 0), (fb, 1), (ba, 0), (fa, 2), (bb, 0), (ba, 1), (fb, 2), (bb, 1),
             (fa, 3), (ba, 2), (fb, 3), (bb, 2), (ba, 3), (bb, 3)]
    for i, (fn, m) in enumerate(order):
        fn(m)
        if i + 1 < len(order) and not state_only:
            yield
    if state_only:
        yield


def tbs(ms):
    return ms


def mlp_tile(k, pipe, gT, W1, W2, final=None):
    S = k.S

    def c_norm(wv, bwb):
        norm_transpose(k, k.xres, k.bx, gT, k.hT, k.bhT, 8)
    pipe.add(None, c_norm)
    halves = [(0, 512), (512, 512)]
    for q in range(4):
        for j in range(8):
            def w1(wv, bwb, j=j):
                def evac(ci, hi, ps, bps):
                    rt, brt = k.rtmp[k.rti % 2]; k.rti += 1
                    S.op('dve', lambda e: e.tensor_scalar(rt[:, :], ps, 0.0, None, ALU.max), reads=[bps], writes=[brt])
                    S.op('act', lambda e: e.activation(k.actT[:, 2 * j + ci, hi * 512:(hi + 1) * 512], rt[:, :], AF.Square),
                         reads=[brt], writes=[k.bactT[2 * j + ci]])
                gemm_f(k, wv, bwb, [(0, 128), (128, 128)], k.hT, k.bhT, halves, evac)
            pipe.add(wblock_loader(k, W1, 0, [(q * 2048 + j * 256, 256)]), w1)
        for n in range(8):
            def w2(wv, bwb, n=n):
                def evac_t(m, ps, bps):
                    S.op('dve', lambda e: e.tensor_tensor(k.xres[:, m, n * 256:(n + 1) * 256], ps,
                                                          k.xres[:, m, n * 256:(n + 1) * 256], ALU.add),
                         reads=[bps, k.bx[m]], writes=[k.bx[m]])
                gemm_t(k, wv, bwb, 256, k.actT, k.bactT, range(8), evac_t)
            pipe.add(wblock_loader(k, W2, q * 2048, [(n * 256, 256)]), w2)


def qkv_tile(k, pipe, t, prev=False, next_x=None):
    S = k.S
    W = k.w_qkv

    def c_store(wv, bwb):
        S.barrier()
        if not prev:
            v = k.X1[t * TT:(t + 1) * TT, :].rearrange("(s p) d -> p s d", p=128)
            for s0 in range(0, 8, 4):
                S.dma('sp', v[:, s0:s0 + 4, :], k.xres[:, s0:s0 + 4, :], k.dX1s[s0 // 4], reads=k.bx[s0:s0 + 4], writes=[k.dX1])
        norm_transpose(k, k.xres, k.bx, k.gmix1, k.hT, k.bhT, 8)
        if next_x is not None:
            load_x_tile(k, next_x[0], next_x[1])
    pipe.add(None, c_store)
    halves = [(0, 512), (512, 512)]
    Vdst = k.Vp if prev else k.V
    for which, dst, dbuf in ((0, k.QT, k.dQT), (1, k.KTp if prev else k.KT, k.dKT)):
        if prev and which == 0:
            continue
        for j in range(8):
            def qk(wv, bwb, j=j, dst=dst, dbuf=dbuf):
                st, bst = k.qstage[k.sti % 2]; k.sti += 1

                def evac(ci, hi, ps, bps):
                    if hi == 0:
                        S.op('act', lambda e: e.copy(st[:, ci, hi * 512:(hi + 1) * 512], ps), reads=[bps], writes=[bst])
                    else:
                        S.op('dve', lambda e: e.tensor_copy(st[:, ci, hi * 512:(hi + 1) * 512], ps), reads=[bps], writes=[bst])
                gemm_f(k, wv, bwb, [(0, 128), (128, 128)], k.hT, k.bhT, halves, evac)
                for ci in range(2):
                    r0 = (2 * j + ci) * 128
                    S.dma('sp', dst[r0:r0 + 128, t * TT:(t + 1) * TT], st[:, ci, :], bst, reads=[bst], writes=[dbuf])
            pipe.add(wblock_loader(k, W, 0, [(which * D + j * 256, 256)]), qk)
    for n in range(8):
        def vp(wv, bwb, n=n):
            st, bst = k.vstage[k.sti % 2]; k.sti += 1

            def evac_t(m, ps, bps):
                if m % 2 == 0:
                    S.op('act', lambda e: e.copy(st[:, m, :], ps), reads=[bps], writes=[bst])
                else:
                    S.op('dve', lambda e: e.tensor_copy(st[:, m, :], ps), reads=[bps], writes=[bst])
            gemm_t(k, wv, bwb, 256, k.hT, k.bhT, range(8), evac_t)
            dv = Vdst[t * TT:(t + 1) * TT, n * 256:(n + 1) * 256].rearrange("(m p) c -> p m c", p=128)
            S.dma('sp', dv, st[:, :, :], bst, reads=[bst], writes=[k.dV])
        pipe.add(wblock_loader(k, W, 0, [(2 * D + n * 256, 256)]), vp)


def exchange(k):
    nc, S = k.nc, k.S
    S.barrier()
    groups = [[2 * i, 2 * i + 1] for i in range(k.ncores // 2)]
    bkv = Buf("kvall")
    S._deps('pool', [], [bkv])
    ins = nc.gpsimd.collective_compute("AllGather", ALU.bypass, groups,
                                       [k.KV.rearrange("a t d -> (a t) d")],
                                       [k.KVall.rearrange("r a t d -> (r a t) d")])
    bkv.dsem = nc.alloc_semaphore(name="d_kvall")
    S.allsems[id(bkv.dsem)] = bkv
    bkv.dcount = 16
    ins.then_inc(bkv.dsem, 16)


def slopes():
    return [2.0 ** (-8.0 * (h + 1) / NHEAD_ATT) for h in range(NHEAD_ATT)]


DILS = (1, 4, 16)


def phaseB(k):
    from contextlib import ExitStack
    nc, S = k.nc, k.S
    cb = k.b_const
    with ExitStack() as es:
        A = lambda name, shape, dt: es.enter_context(nc.sbuf_tensor(name, shape, dt))
        flag = A("flag_sb", [128, 1], F32)
        S.dma('sp', flag[:, :], k.flag[:, :], cb, writes=[cb])
        ones = A("ones_bf", [128, 128], BF16)
        S.op('pool', lambda e: e.memset(ones[:, :], 1.0), writes=[cb])
        Dm = A("Dm", [128, 256], F32)
        S.op('pool', lambda e: e.iota(Dm[:, :], [[1, 256]], base=0, channel_multiplier=-1, allow_small_or_imprecise_dtypes=True), writes=[cb])
        Dc = A("Dc", [128, 256], F32)
        S.op('dve', lambda e: e.tensor_scalar(Dc[:, :], Dm[:, :], 0.0, 128.0, ALU.max, ALU.min), reads=[cb], writes=[cb])
        EB = A("EB", [128, 48, 256], BF16)
        EBf = A("EBf", [128, 48, 128], BF16)
        M01 = A("M01", [128, 256], F32)
        S.op('pool', lambda e: e.memset(M01[:, :], 1.0), writes=[cb])
        S.op('pool', lambda e: e.affine_select(M01[:, :], M01[:, :], [[1, 256]], ALU.is_ge, 0.0, base=0,
                                               channel_multiplier=-1), reads=[cb], writes=[cb])
        S.op('pool', lambda e: e.affine_select(M01[:, :], M01[:, :], [[-1, 256]], ALU.is_ge, 0.0, base=128,
                                               channel_multiplier=1), reads=[cb], writes=[cb])
        M01f = A("M01f", [128, 128], F32)
        S.op('dve', lambda e: e.tensor_scalar(M01f[:, :], M01[:, 128:256], flag[:, 0:1], None, ALU.mult), reads=[cb], writes=[cb])
        ebts = [(A("ebt%d" % i, [128, 256], F32), Buf("ebt%d" % i)) for i in range(2)]
        sl = slopes()
        for h in range(NHEAD_ATT):
            for di, d in enumerate(DILS):
                idx = h * 3 + di
                ebt, bebt = ebts[idx % 2]
                S.op('act', lambda e: e.activation(ebt[:, :], Dc[:, :], AF.Exp, scale=-float(sl[h] * d)),
                     reads=[cb], writes=[bebt])
                S.op('dve', lambda e: e.tensor_tensor(EB[:, idx, :], ebt[:, :], M01[:, :], ALU.mult), reads=[bebt, cb], writes=[cb])
                S.op('pool', lambda e: e.tensor_tensor(EBf[:, idx, :], ebt[:, 128:256], M01f[:, :], ALU.mult),
                     reads=[bebt, cb], writes=[cb])
        HG = 2
        NG = NHEAD_ATT // HG
        LA = 2
        qks = [(A("qTa%d" % i, [128, HG, NOWN], BF16), Buf("qTa%d" % i),
                A("kTa%d" % i, [128, HG, 2 * NOWN], BF16), Buf("kTa%d" % i)) for i in range(2)]
        vts = [(A("vt%d" % i, [128, 32, HG * 128], BF16), Buf("vt%d" % i)) for i in range(3)]
        acc = A("acc", [128, HG, 2, NOWN], F32); bacc = [Buf("acc%d" % i) for i in range(HG)]
        NPB = LA + 2
        ptmp = [(A("ptmp%d" % i, [128, 256], F32), Buf("ptmp%d" % i)) for i in range(NPB)]
        pbf = [(A("pbf%d" % i, [128, 256], BF16), Buf("pbf%d" % i)) for i in range(NPB)]
        ostg = [(A("ostg%d" % i, [128, NOWN], BF16), Buf("ostg%d" % i)) for i in range(2)]
        rz = A("rz", [128, NOWN], F32); brz = Buf("rz")
        dOT = Buf("dOT")
        scale = float(DH ** -0.5)

        def load_qk(g):
            qT, bq, kT, bk = qks[g % 2]
            for hh in range(HG):
                r0 = (g * HG + hh) * 128
                S.dma('sp', qT[:, hh, :], k.QT[r0:r0 + 128, :], bq, writes=[bq])
                S.dma('sp', kT[:, hh, 0:NOWN], k.KTp[r0:r0 + 128, :], bk, writes=[bk])
                S.dma('sp', kT[:, hh, NOWN:2 * NOWN], k.KT[r0:r0 + 128, :], bk, writes=[bk])

        def load_v(g, di):
            d = DILS[di]
            vt, bvt = vts[di]
            nbh = 16 // d
            vt4 = vt[:, :, :].rearrange("p (r b) c -> p r b c", r=d)
            for half, src in enumerate((k.Vp, k.V)):
                sv = src[:, g * HG * 128:(g + 1) * HG * 128].rearrange("(b p r) c -> p r b c", p=128, r=d)
                for r in range(d):
                    S.dma('sp', vt4[:, r, half * nbh:(half + 1) * nbh, :], sv[:, r, :, :], bvt, writes=[bvt])

        def st_qk(b):
            qT, bq, kT, bk = qks[b['g'] % 2]
            d, hh, r, qb, nbh = b['d'], b['hh'], b['r'], b['qb'], b['nbh']
            kbq = nbh + qb
            pt, bpt = k.bank()
            b['pt'], b['bpt'] = pt, bpt
            q0 = r + d * 128 * qb
            b['q0'] = q0
            qsl = qT[:, hh, q0:q0 + d * 127 + 1:d]
            for ci, kb in enumerate((kbq, kbq - 1)):
                f0 = r + d * 128 * kb
                S.mm(lambda e: e.matmul(pt[:, ci * 128:(ci + 1) * 128], kT[:, hh, f0:f0 + d * 127 + 1:d], qsl,
                                        start=True, stop=True), reads=[bk, bq], writes=[bpt], sig=(ci == 1))
            pm, bpm = ptmp[b['i'] % NPB]; pb, bpb = pbf[b['i'] % NPB]
            b['pb'], b['bpb'] = pb, bpb
            idx = b['idx']
            S.op('act', lambda e: e.activation(pm[:, :], pt[:, 0:256], AF.Exp, scale=scale), reads=[bpt], writes=[bpm])
            me = 'dve'
            if qb == 0:
                S.op(me, lambda e: e.tensor_tensor(pb[:, 0:128], pm[:, 0:128], EB[:, idx, 0:128], ALU.mult),
                     reads=[bpm, cb], writes=[bpb])
                S.op(me, lambda e: e.tensor_tensor(pb[:, 128:256], pm[:, 128:256], EBf[:, idx, :], ALU.mult),
                     reads=[bpm, cb], writes=[bpb])
            else:
                S.op(me, lambda e: e.tensor_tensor(pb[:, :], pm[:, :], EB[:, idx, :], ALU.mult),
                     reads=[bpm, cb], writes=[bpb])

        def st_pv(b):
            d, hh, r, qb, nbh, di = b['d'], b['hh'], b['r'], b['qb'], b['nbh'], b['di']
            vt, bvt = vts[di]
            pb, bpb = b['pb'], b['bpb']
            kbq = nbh + qb
            po, bpo = k.bank()
            for ci, kb in enumerate((kbq, kbq - 1)):
                tile = r * (2 * nbh) + kb
                S.mm(lambda e: e.matmul(po[:, 0:128], vt[:, tile, hh * 128:(hh + 1) * 128], pb[:, ci * 128:(ci + 1) * 128],
                                        start=(ci == 0), stop=(ci == 1)), reads=[bvt, bpb], writes=[bpo], sig=False)
            for ci in range(2):
                S.mm(lambda e: e.matmul(po[:, 128:256], ones[:, :], pb[:, ci * 128:(ci + 1) * 128],
                                        start=(ci == 0), stop=(ci == 1)), reads=[cb, bpb], writes=[bpo], sig=(ci == 1))
            q0 = b['q0']
            asl = acc[:, hh, :, q0:q0 + d * 127 + 1:d]
            pv = po[:, 0:256].rearrange("p (a t) -> p a t", a=2)
            if di == 0:
                S.op('act', lambda e: e.copy(asl, pv), reads=[bpo], writes=[bacc[hh]])
            else:
                S.op('dve', lambda e: e.tensor_tensor(asl, pv, asl, ALU.add), reads=[bpo, bacc[hh]], writes=[bacc[hh]])

        load_qk(0)
        for di in range(3):
            load_v(0, di)
        bi = 0
        for g in range(NG):
            blocks = []
            marks = {}
            for di, d in enumerate(DILS):
                nbh = 16 // d
                for hh in range(HG):
                    for r in range(d):
                        for qb in range(nbh):
                            blocks.append(dict(g=g, di=di, d=d, hh=hh, r=r, qb=qb, nbh=nbh, i=bi,
                                               idx=(g * HG + hh) * 3 + di))
                            bi += 1
                marks[len(blocks) - 1] = di
            n = len(blocks)
            for i in range(n + LA):
                if i < n:
                    st_qk(blocks[i])
                if i - LA >= 0:
                    st_pv(blocks[i - LA])
                    bd = blocks[i - LA]
                    if bd['di'] == 2 and bd['r'] == 15 and bd['qb'] == bd['nbh'] - 1:
                        hh = bd['hh']
                        h = g * HG + hh
                        og, bog = ostg[h % 2]
                        S.op('act', lambda e: e.activation(rz[:, :], acc[:, hh, 1, :], AF.Ln), reads=[bacc[hh]], writes=[brz])
                        S.op('act', lambda e: e.activation(rz[:, :], rz[:, :], AF.Exp, scale=-1.0), reads=[brz], writes=[brz])
                        S.op('pool', lambda e: e.tensor_tensor(og[:, :], acc[:, hh, 0, :], rz[:, :], ALU.mult),
                             reads=[bacc[hh], brz], writes=[bog])
                        S.dma('sp', k.OT[h * 128:(h + 1) * 128, :], og[:, :], bog, reads=[bog], writes=[dOT])
                    if (i - LA) in marks and g + 1 < NG:
                        di_done = marks[i - LA]
                        if di_done == 0:
                            load_qk(g + 1)
                        load_v(g + 1, di_done)
        S.barrier()
    with ExitStack() as es:
        A = lambda name, shape, dt: es.enter_context(nc.sbuf_tensor(name, shape, dt))
        k.xres = A("xres3", [128, 8, D], F32); k.bx = [Buf("xres3_%d" % i) for i in range(8)]; k.bxd = [Buf("xres3_dma0"), Buf("xres3_dma1")]
        k.hT = A("hT3", [128, 16, TT], BF16); k.bhT = Buf("hT3")
        k.actT = A("actT3", [128, 16, TT], BF16); k.bactT = [Buf("actT3_%d" % c) for c in range(16)]
        k.wslots = [(A("wc%d" % i, [128, 4096], BF16), Buf("wc%d" % i)) for i in range(2)]
        k.hb = [(A("hc%d" % i, [128, D], BF16), Buf("hc%d" % i)) for i in range(2)]
        k.stat = A("stat3", [128, 32], F32); k.bstat = Buf("stat3")
        k.rtmp = [(A("rtmq%d" % i, [128, 512], F32), Buf("rtmq%d" % i)) for i in range(2)]
        k.rti = 0
        gfB = A("gfB", [128, D], F32)
        S.dma('sp', gfB[:, :], k.final_g[0:1, :].partition_broadcast(128), cb, writes=[cb])
        k.gmlp1 = load_gT(k, "gmlp1", k.norm_mlp_g1[1:2, :], dst=A("gmlp1", [128, 16], F32)[:, :])
        ost = [(A("ost%d" % i, [128, D], F32), Buf("ost%d" % i)) for i in range(2)]
        dout = Buf("dout")
        pipe = Pipe(k)
        for t in range(NT):
            def c_load(wv, bwb, t=t):
                v = k.X1[t * TT:(t + 1) * TT, :].rearrange("(s p) d -> p s d", p=128)
                for s0 in range(0, 8, 4):
                    S.dma('sp', k.xres[:, s0:s0 + 4, :], v[:, s0:s0 + 4, :], k.bxd[s0 // 4], writes=k.bx[s0:s0 + 4])
                S.dma('sp', k.actT[:, :, :], k.OT[:, t * TT:(t + 1) * TT].rearrange("(c p) t -> p c t", p=128), k.bactT[0],
                      writes=k.bactT)
            pipe.add(None, c_load)
            for n in range(8):
                def wo(wv, bwb, n=n):
                    def evac_t(m, ps, bps):
                        S.op('dve', lambda e: e.tensor_tensor(k.xres[:, m, n * 256:(n + 1) * 256], ps,
                                                              k.xres[:, m, n * 256:(n + 1) * 256], ALU.add),
                             reads=[bps, k.bx[m]], writes=[k.bx[m]])
                    gemm_t(k, wv, bwb, 256, k.actT, k.bactT, range(8), evac_t)
                pipe.add(wblock_loader(k, k.w_o, 0, [(n * 256, 256)]), wo)
            mlp_tile(k, pipe, k.gmlp1, k.w1_1, k.w2_1)

            def c_final(wv, bwb, t=t):
                for s in range(8):
                    hb, bhb = k.hb[s % 2]
                    S.op('act', lambda e: e.activation(hb[:, :], k.xres[:, s, :], AF.Square, accum_out=k.stat[:, s:s + 1]),
                         reads=[k.bx[s]], writes=[bhb, k.bstat])
                S.op('act', lambda e: e.activation(k.stat[:, 8:16], k.stat[:, 0:8], AF.Ln, scale=1.0 / D, bias=k.epsc[:, 0:1]),
                     reads=[k.bstat, cb], writes=[k.bstat])
                S.op('act', lambda e: e.activation(k.stat[:, 16:24], k.stat[:, 8:16], AF.Exp, scale=-0.5),
                     reads=[k.bstat], writes=[k.bstat])
                for s in range(8):
                    o, bo = ost[s % 2]
                    S.op('dve', lambda e: e.scalar_tensor_tensor(o[:, :], k.xres[:, s, :], k.stat[:, 16 + s:17 + s], gfB[:, :],
                                                                 ALU.mult, ALU.mult), reads=[k.bx[s], k.bstat, cb], writes=[bo])
                    r0 = t * TT + s * 128
                    S.dma('sp', k.out[r0:r0 + 128, :], o[:, :], bo, reads=[bo], writes=[dout])
            pipe.add(None, c_final)
        pipe.run()
        S.barrier()


FUSED = True
_CACHE = {}


def _get(mode, ncores=8):
    key = (mode, ncores)
    if key not in _CACHE:
        _CACHE[key] = build(mode, ncores)
    return _CACHE[key]


def _maps_A(inp, cores):
    x = inp['x']
    maps = []
    zeros = np.zeros((NOWN, D), np.float32)
    for c in cores:
        b, half = c // 2, c % 2
        maps.append({
            'x_own': np.ascontiguousarray(x[b, half * NOWN:(half + 1) * NOWN]),
            'x_prev': np.ascontiguousarray(x[b, 0:NOWN]) if half == 1 else zeros,
            'norm_mix_g': inp['norm_mix_g'], 'norm_mlp_g': inp['norm_mlp_g'],
            'hyb_w_in': inp['hyb_w_in'][0], 'conv_w': inp['conv_w'][0],
            'gla_w_gate2': inp['gla_w_gate2'][0], 'gla_b_gate': inp['gla_b_gate'],
            'gla_norm_g': inp['gla_norm_g'], 'hyb_w_out': inp['hyb_w_out'][0],
            'attn_w_qkv': inp['attn_w_qkv'][0],
            'mlp_w1_0': inp['mlp_w1'][0], 'mlp_w2_0': inp['mlp_w2'][0],
        })
    return maps


def _maps_B_extra(inp, cores):
    maps = []
    for c in cores:
        half = c % 2
        maps.append({
            'flag': np.full((128, 1), float(half), np.float32),
            'final_norm_g': inp['final_norm_g'].reshape(1, D),
            'attn_w_o': inp['attn_w_o'][0],
            'mlp_w1_1': inp['mlp_w1'][1], 'mlp_w2_1': inp['mlp_w2'][1],
        })
    return maps


def kernel(**inputs):
    inp = {k_: np.ascontiguousarray(np.asarray(v)) for k_, v in inputs.items()}
    B = inp['x'].shape[0]
    ncores = 2 * B
    cores = list(range(ncores))
    if FUSED:
        nc = _get('F', ncores)
        mA = _maps_A(inp, cores)
        mB = _maps_B_extra(inp, cores)
        maps = [dict(a, **b) for a, b in zip(mA, mB)]
        res = run_bass_kernel_spmd(nc, maps, core_ids=cores)
        outs = [np.asarray(r['out']) for r in res.results]
    else:
        ncA = _get('A', ncores)
        resA = run_bass_kernel_spmd(ncA, _maps_A(inp, cores), core_ids=cores).results
        ncB = _get('B', ncores)
        mB = _maps_B_extra(inp, cores)
        zk = None
        for c in cores:
            m = mB[c]
            m['norm_mlp_g'] = inp['norm_mlp_g']
            for nm in ('X1', 'QT', 'KT', 'V'):
                m[nm] = np.asarray(resA[c][nm])
            if c % 2 == 1:
                m['KTp'] = np.asarray(resA[c - 1]['KT'])
                m['Vp'] = np.asarray(resA[c - 1]['V'])
            else:
                if zk is None:
                    zk = np.zeros_like(np.asarray(resA[c]['KT']))
                m['KTp'] = zk
                m['Vp'] = zk
        res = run_bass_kernel_spmd(ncB, mB, core_ids=cores)
        outs = [np.asarray(r['out']) for r in res.results]
    out = np.stack([np.concatenate([outs[2 * b], outs[2 * b + 1]], axis=0) for b in range(B)], axis=0)
    return out.astype(np.float32)
```
